# Optimizing a Trainium2 kernel written in Bass

```python
import math
import jax, jax.numpy as jnp
from jax import lax
import numpy as np

D_MODEL = 2048
BATCH = 16
SEQ = 2048
DEPTH = 2

RET_HEADS = 8
RET_HEAD_DIM = 128
RET_CHUNK = 128
MLSTM_HEADS = 4
MLSTM_QK_DIM = 128
MLSTM_V_DIM = 256
MLSTM_CHUNK = 128
MLSTM_CONV = 4
MOBA_HEADS = 8
MOBA_HEAD_DIM = 128
MOBA_BLOCK = 256
MOBA_TOPK = 3
MOBA_QCHUNK = 64
REL_BUCKETS = 32
REL_MAX_DIST = 128
D_FF = 4 * D_MODEL
ROPE_BASE = 10000.0
EPS = 1e-6

RET_W = RET_HEADS * RET_HEAD_DIM
MLSTM_QK_W = MLSTM_HEADS * MLSTM_QK_DIM
MLSTM_V_W = MLSTM_HEADS * MLSTM_V_DIM
MOBA_W = MOBA_HEADS * MOBA_HEAD_DIM
N_BRANCH = 3
IN_SPLITS = (RET_W, RET_W, RET_W, RET_W,
             MLSTM_QK_W, MLSTM_QK_W, MLSTM_V_W, MLSTM_V_W, MLSTM_HEADS, MLSTM_HEADS,
             MOBA_W, MOBA_W, MOBA_W,
             N_BRANCH * D_MODEL)
D_IN = sum(IN_SPLITS)

kernel_name = "hybrid_retention_mlstm_moba_gated_block"


def rms_norm(x, w):
    xf = x.astype(jnp.float32)
    y = xf * lax.rsqrt(jnp.mean(xf * xf, axis=-1, keepdims=True) + EPS)
    return (y * w.astype(jnp.float32)).astype(x.dtype)


def head_rms(x):
    return x * lax.rsqrt(jnp.mean(x * x, axis=-1, keepdims=True) + EPS)


def rotary(x, pos):
    half = x.shape[-1] // 2
    inv = ROPE_BASE ** (-jnp.arange(half, dtype=jnp.float32) / half)
    ang = pos.astype(jnp.float32)[:, None] * inv[None, :]
    cos, sin = jnp.cos(ang), jnp.sin(ang)
    x1, x2 = x[..., :half], x[..., half:]
    return jnp.concatenate([x1 * cos - x2 * sin, x1 * sin + x2 * cos], axis=-1)


def causal_dwconv(x, w, b):
    K = w.shape[0]
    S = x.shape[1]
    xp = jnp.pad(x, ((0, 0), (K - 1, 0), (0, 0)))
    y = xp[:, 0:S] * w[0]
    for j in range(1, K):
        y = y + xp[:, j:j + S] * w[j]
    return y + b


def retention(q, k, v, g):
    B, S, H, dh = q.shape
    L = RET_CHUNK
    N = S // L
    pos = jnp.arange(S)
    q = rotary(jnp.swapaxes(q, 1, 2).astype(jnp.float32), pos) * (dh ** -0.5)
    k = rotary(jnp.swapaxes(k, 1, 2).astype(jnp.float32), pos)
    v = jnp.swapaxes(v, 1, 2).astype(jnp.float32)
    q = q.reshape(B, H, N, L, dh)
    k = k.reshape(B, H, N, L, dh)
    v = v.reshape(B, H, N, L, dh)
    log_gamma = jnp.log1p(-jnp.exp2(-5.0 - jnp.arange(H, dtype=jnp.float32)))
    idx = jnp.arange(L, dtype=jnp.float32)
    diff = idx[:, None] - idx[None, :]
    decay = jnp.where(diff >= 0, jnp.exp(jnp.maximum(diff, 0.0) * log_gamma[:, None, None]), 0.0)
    scores = jnp.einsum('bhnld,bhnmd->bhnlm', q, k) * decay[:, None]
    intra = jnp.einsum('bhnlm,bhnme->bhnle', scores, v)
    zeta = jnp.exp((L - 1 - idx)[None, :] * log_gamma[:, None])
    xi = jnp.exp((idx + 1.0)[None, :] * log_gamma[:, None])
    kv = jnp.einsum('bhnld,bhnle->bhnde', k * zeta[:, None, :, None], v)
    gamma_l = jnp.exp(L * log_gamma)

    def step(R, kv_n):
        return R * gamma_l[:, None, None] + kv_n, R

    _, R_prev = lax.scan(step, jnp.zeros((B, H, dh, dh), jnp.float32), jnp.moveaxis(kv, 2, 0))
    R_prev = jnp.moveaxis(R_prev, 0, 2)
    inter = jnp.einsum('bhnld,bhnde->bhnle', q, R_prev) * xi[:, None, :, None]
    o = head_rms((intra + inter).reshape(B, H, S, dh))
    o = jnp.swapaxes(o, 1, 2).reshape(B, S, H * dh)
    return o * jax.nn.silu(g.astype(jnp.float32))


def mlstm(q, k, v, o, i_pre, f_pre, norm_w):
    B, S, H, dk = q.shape
    dv = v.shape[-1]
    L = MLSTM_CHUNK
    N = S // L

    def chunk(t):
        return jnp.swapaxes(t, 1, 2).astype(jnp.float32).reshape((B, H, N, L) + t.shape[3:])

    q = chunk(q) * (dk ** -0.5)
    k = chunk(k)
    v = chunk(v)
    li = chunk(i_pre)
    lf = jax.nn.log_sigmoid(chunk(f_pre))
    bcum = jnp.cumsum(lf, axis=-1)
    gtot = bcum[..., -1]
    causal = jnp.tril(jnp.ones((L, L), dtype=bool))
    dlog = jnp.where(causal, bcum[..., :, None] - bcum[..., None, :] + li[..., None, :], -jnp.inf)
    w_loc = gtot[..., None] - bcum + li
    m_loc = jnp.max(w_loc, axis=-1)
    e_loc = jnp.exp(w_loc - m_loc[..., None])
    C_loc = jnp.einsum('bhnl,bhnld,bhnle->bhnde', e_loc, k, v)
    n_loc = jnp.einsum('bhnl,bhnld->bhnd', e_loc, k)

    def step(carry, xs):
        C, n, m = carry
        Cl, nl, ml, g = xs
        m_new = jnp.maximum(g + m, ml)
        a = jnp.exp(g + m - m_new)
        bb = jnp.exp(ml - m_new)
        C_new = a[..., None, None] * C + bb[..., None, None] * Cl
        n_new = a[..., None] * n + bb[..., None] * nl
        return (C_new, n_new, m_new), (C, n, m)

    init = (jnp.zeros((B, H, dk, dv), jnp.float32), jnp.zeros((B, H, dk), jnp.float32),
            jnp.zeros((B, H), jnp.float32))
    xs = (jnp.moveaxis(C_loc, 2, 0), jnp.moveaxis(n_loc, 2, 0), jnp.moveaxis(m_loc, 2, 0),
          jnp.moveaxis(gtot, 2, 0))
    _, (C_prev, n_prev, m_prev) = lax.scan(step, init, xs)
    C_prev = jnp.moveaxis(C_prev, 0, 2)
    n_prev = jnp.moveaxis(n_prev, 0, 2)
    m_prev = jnp.moveaxis(m_prev, 0, 2)
    inter_log = bcum + m_prev[..., None]
    m_pos = jnp.maximum(inter_log, jnp.max(dlog, axis=-1))
    s_intra = jnp.einsum('bhnld,bhnmd->bhnlm', q, k) * jnp.exp(dlog - m_pos[..., None])
    inter_scale = jnp.exp(inter_log - m_pos)
    num = (jnp.einsum('bhnlm,bhnme->bhnle', s_intra, v)
           + inter_scale[..., None] * jnp.einsum('bhnld,bhnde->bhnle', q, C_prev))
    den = jnp.sum(s_intra, axis=-1) + inter_scale * jnp.einsum('bhnld,bhnd->bhnl', q, n_prev)
    h = num / jnp.maximum(jnp.abs(den), jnp.exp(-m_pos))[..., None]
    h = head_rms(h.reshape(B, H, S, dv))
    h = jnp.swapaxes(h, 1, 2).reshape(B, S, H * dv) * norm_w.astype(jnp.float32)
    return h * jax.nn.sigmoid(o.astype(jnp.float32))


def t5_bucket(dist):
    n = jnp.maximum(dist, 0)
    exact = REL_BUCKETS // 2
    nf = jnp.maximum(n, 1).astype(jnp.float32)
    large = exact + (jnp.log(nf / exact) / math.log(REL_MAX_DIST / exact)
                     * (REL_BUCKETS - exact)).astype(jnp.int32)
    large = jnp.minimum(large, REL_BUCKETS - 1)
    return jnp.where(n < exact, n, large)


def moba(q, k, v, rel_bias):
    B, S, H, dh = q.shape
    BS = MOBA_BLOCK
    QC = MOBA_QCHUNK
    NB = -(-S // BS)
    S_pad = NB * BS
    n_sel = min(MOBA_TOPK, NB - 1)
    nC = S // QC
    q = jnp.swapaxes(q, 1, 2).astype(jnp.float32) * (dh ** -0.5)
    pad = ((0, 0), (0, 0), (0, S_pad - S), (0, 0))
    kb = jnp.pad(jnp.swapaxes(k, 1, 2).astype(jnp.float32), pad).reshape(B, H, NB, BS, dh)
    vb = jnp.pad(jnp.swapaxes(v, 1, 2).astype(jnp.float32), pad).reshape(B, H, NB, BS, dh)
    table_t = rel_bias.T.astype(jnp.float32)
    hid = jnp.arange(H)[None, :, None, None]
    if n_sel > 0:
        kmean = jnp.mean(kb, axis=3)
        gate = jnp.einsum('bhsd,bhnd->bhsn', q, kmean)
        past = jnp.arange(NB)[None, :] < (jnp.arange(S) // BS)[:, None]
        gate = jnp.where(past, gate, -jnp.inf)
        _, sel = lax.top_k(gate, n_sel)
        sel = sel.astype(jnp.int32)
    else:
        sel = jnp.zeros((B, H, S, 0), jnp.int32)
    qc = jnp.moveaxis(q.reshape(B, H, nC, QC, dh), 2, 0)
    selc = jnp.moveaxis(sel.reshape(B, H, nC, QC, n_sel), 2, 0)
    gather = jax.vmap(jax.vmap(lambda blocks, i: blocks[i]))
    ar = jnp.arange(BS)

    def one_chunk(args):
        cid, q_c, sel_c = args
        qpos = cid * QC + jnp.arange(QC)
        own = (cid * QC) // BS
        logits = []
        for s in range(n_sel):
            blk = sel_c[..., s]
            k_s = gather(kb, blk)
            kpos = blk[..., None] * BS + ar
            bias = table_t[hid, t5_bucket(qpos[:, None] - kpos)]
            lg = jnp.einsum('bhqd,bhqjd->bhqj', q_c, k_s) + bias
            logits.append(jnp.where((blk < own)[..., None], lg, -jnp.inf))
        k_o = lax.dynamic_index_in_dim(kb, own, axis=2, keepdims=False)
        v_o = lax.dynamic_index_in_dim(vb, own, axis=2, keepdims=False)
        kpos_o = own * BS + ar
        bias_o = table_t[:, t5_bucket(qpos[:, None] - kpos_o[None, :])]
        lg_o = jnp.einsum('bhqd,bhjd->bhqj', q_c, k_o) + bias_o
        logits.append(jnp.where(kpos_o[None, :] <= qpos[:, None], lg_o, -jnp.inf))
        p = jax.nn.softmax(jnp.concatenate(logits, axis=-1), axis=-1)
        out = jnp.einsum('bhqj,bhje->bhqe', p[..., n_sel * BS:], v_o)
        for s in range(n_sel):
            v_s = gather(vb, sel_c[..., s])
            out = out + jnp.einsum('bhqj,bhqje->bhqe', p[..., s * BS:(s + 1) * BS], v_s)
        return out

    out = lax.map(one_chunk, (jnp.arange(nC, dtype=jnp.int32), qc, selc))
    out = jnp.moveaxis(out, 0, 2).reshape(B, H, S, dh)
    return jnp.swapaxes(out, 1, 2).reshape(B, S, H * dh)


def setup_inputs(seed: int = 0) -> dict:
    key = jax.random.key(seed)
    ks = jax.random.split(key, 20)

    def nrm(k, shape, scale):
        return jax.random.normal(k, shape, jnp.float32) * scale

    x = nrm(ks[0], (BATCH, SEQ, D_MODEL), 1.0)
    w_in = nrm(ks[1], (DEPTH, D_MODEL, D_IN), D_MODEL ** -0.5)
    i_b = nrm(ks[2], (DEPTH, 1, MLSTM_HEADS), 0.1)
    f_b = (jnp.linspace(3.0, 6.0, MLSTM_HEADS, dtype=jnp.float32)[None, None, :]
           + nrm(ks[3], (DEPTH, 1, MLSTM_HEADS), 0.1))
    mlstm_gate_b = jnp.concatenate([i_b, f_b], axis=1)
    mlstm_conv_w = nrm(ks[4], (DEPTH, MLSTM_CONV, 2 * MLSTM_QK_W), MLSTM_CONV ** -0.5)
    mlstm_conv_b = nrm(ks[5], (DEPTH, 2 * MLSTM_QK_W), 0.01)
    mlstm_norm_w = 1.0 + nrm(ks[6], (DEPTH, MLSTM_V_W), 0.02)
    w_branch_ret = nrm(ks[7], (DEPTH, RET_W, D_MODEL), RET_W ** -0.5)
    w_branch_mlstm = nrm(ks[8], (DEPTH, MLSTM_V_W, D_MODEL), MLSTM_V_W ** -0.5)
    w_branch_moba = nrm(ks[9], (DEPTH, MOBA_W, D_MODEL), MOBA_W ** -0.5)
    w_out = nrm(ks[10], (DEPTH, D_MODEL, D_MODEL), D_MODEL ** -0.5)
    norm_mix_w = 1.0 + nrm(ks[11], (DEPTH, D_MODEL), 0.02)
    norm_mlp_w = 1.0 + nrm(ks[12], (DEPTH, D_MODEL), 0.02)
    w_ff1 = nrm(ks[13], (DEPTH, D_MODEL, D_FF), D_MODEL ** -0.5)
    w_ff2 = nrm(ks[14], (DEPTH, D_FF, D_MODEL), D_FF ** -0.5)
    rel_bias = nrm(ks[15], (REL_BUCKETS, MOBA_HEADS), 0.5)
    final_norm_w = 1.0 + nrm(ks[16], (D_MODEL,), 0.02)
    return {"x": x, "w_in": w_in, "mlstm_gate_b": mlstm_gate_b, "mlstm_conv_w": mlstm_conv_w,
            "mlstm_conv_b": mlstm_conv_b, "mlstm_norm_w": mlstm_norm_w,
            "w_branch_ret": w_branch_ret, "w_branch_mlstm": w_branch_mlstm,
            "w_branch_moba": w_branch_moba, "w_out": w_out, "norm_mix_w": norm_mix_w,
            "norm_mlp_w": norm_mlp_w, "w_ff1": w_ff1, "w_ff2": w_ff2, "rel_bias": rel_bias,
            "final_norm_w": final_norm_w}


def reference(x, w_in, mlstm_gate_b, mlstm_conv_w, mlstm_conv_b, mlstm_norm_w,
              w_branch_ret, w_branch_mlstm, w_branch_moba, w_out, norm_mix_w,
              norm_mlp_w, w_ff1, w_ff2, rel_bias, final_norm_w):
    B, S, _ = x.shape
    offsets = np.cumsum(IN_SPLITS)[:-1].tolist()
    for l in range(DEPTH):
        h = rms_norm(x, norm_mix_w[l])
        proj = h @ w_in[l]
        (rq, rk, rv, rg, mq, mk, mv, mo, mi, mf, bq, bk, bv, gates) = jnp.split(proj, offsets, axis=-1)
        y_ret = retention(rq.reshape(B, S, RET_HEADS, RET_HEAD_DIM),
                          rk.reshape(B, S, RET_HEADS, RET_HEAD_DIM),
                          rv.reshape(B, S, RET_HEADS, RET_HEAD_DIM), rg)
        mqk = jax.nn.silu(causal_dwconv(jnp.concatenate([mq, mk], axis=-1), mlstm_conv_w[l], mlstm_conv_b[l]))
        mq_c, mk_c = jnp.split(mqk, 2, axis=-1)
        y_ml = mlstm(mq_c.reshape(B, S, MLSTM_HEADS, MLSTM_QK_DIM),
                     mk_c.reshape(B, S, MLSTM_HEADS, MLSTM_QK_DIM),
                     mv.reshape(B, S, MLSTM_HEADS, MLSTM_V_DIM), mo,
                     mi + mlstm_gate_b[l, 0], mf + mlstm_gate_b[l, 1], mlstm_norm_w[l])
        y_mb = moba(bq.reshape(B, S, MOBA_HEADS, MOBA_HEAD_DIM),
                    bk.reshape(B, S, MOBA_HEADS, MOBA_HEAD_DIM),
                    bv.reshape(B, S, MOBA_HEADS, MOBA_HEAD_DIM), rel_bias)
        g_ret, g_ml, g_mb = jnp.split(gates, 3, axis=-1)
        mixed = (jax.nn.sigmoid(g_ret) * (y_ret.astype(x.dtype) @ w_branch_ret[l])
                 + jax.nn.sigmoid(g_ml) * (y_ml.astype(x.dtype) @ w_branch_mlstm[l])
                 + jax.nn.sigmoid(g_mb) * (y_mb.astype(x.dtype) @ w_branch_moba[l]))
        x = x + mixed @ w_out[l]
        h2 = rms_norm(x, norm_mlp_w[l])
        x = x + jnp.square(jax.nn.relu(h2 @ w_ff1[l])) @ w_ff2[l]
    return rms_norm(x, final_norm_w)
```

```python
import math
from contextlib import ExitStack
import numpy as np
import concourse.bass as bass
import concourse.mybir as mybir
from concourse.bass_utils import run_bass_kernel_spmd

F32 = mybir.dt.float32
BF16 = mybir.dt.bfloat16
AF = mybir.ActivationFunctionType
ALU = mybir.AluOpType
AX = mybir.AxisListType

D = 2048
S = 2048
NCORES = 8
DEPTH = 2
D_IN = 16392
D_FF = 8192
EPS = 1e-6
O_RQ, O_RK, O_RV, O_RG = 0, 1024, 2048, 3072
O_MQ, O_MK, O_MV, O_MO, O_MI, O_MF = 4096, 4608, 5120, 6144, 7168, 7172
O_BQ, O_BK, O_BV, O_G = 7176, 8200, 9224, 10248
NEG = -30000.0


class Ctx:
    def __init__(self, nc):
        self.nc = nc
        self.es = ExitStack()
        self.eng = {"pe": nc.tensor, "act": nc.scalar, "dve": nc.vector, "pool": nc.gpsimd, "sp": nc.sync}
        self.sem = {}
        self.cnt = {}
        for k in self.eng:
            self.sem[k] = self.es.enter_context(nc.semaphore("s_" + k))
            self.cnt[k] = 0
        self.waited = {k: {} for k in self.eng}
        self.res = {}
        self.ninst = {k: 0 for k in self.eng}
        self._psf = []
        self._psb = []
        self._pi = 0
        self._pb = 0

    def sb(self, st, name, shape, dt):
        self._uid = getattr(self, "_uid", 0) + 1
        return st.enter_context(self.nc.sbuf_tensor(f"{name}_u{self._uid}", list(shape), dt))

    def dsem(self, key):
        if key not in self.sem:
            self.sem[key] = self.es.enter_context(self.nc.semaphore("d_" + key))
            self.cnt[key] = 0
            assert len(self.sem) <= 100, "too many semaphores"
        return key

    def _r(self, name):
        r = self.res.get(name)
        if r is None:
            r = {"w": {}, "r": {}}
            self.res[name] = r
        return r

    def _wait(self, e, deps):
        m = {}
        for d in deps:
            if d is None:
                continue
            k, v = d
            if v > m.get(k, 0):
                m[k] = v
        for k, v in m.items():
            if k == "pe" and e == "pe":
                continue
            if self.waited[e].get(k, 0) >= v:
                continue
            self.eng[e].wait_ge(self.sem[k], v)
            self.waited[e][k] = v

    def _deps(self, reads, writes, par=False, e=None):
        deps = []
        for r in reads:
            deps.extend(self._r(r)["w"].items())
            if r.startswith("ps"):
                deps.extend((k, v) for k, v in self._r(r)["r"].items() if k != e)
        for w in writes:
            rr = self._r(w)
            if not par:
                deps.extend(rr["w"].items())
            deps.extend(rr["r"].items())
        return deps

    def _commit(self, ticket, reads, writes, par=False):
        k, v = ticket
        for r in reads:
            rr = self._r(r)
            if rr["r"].get(k, 0) < v:
                rr["r"][k] = v
        for w in writes:
            rr = self._r(w)
            if par:
                if rr["w"].get(k, 0) < v:
                    rr["w"][k] = v
            else:
                rr["w"] = {k: v}
                rr["r"] = {}

    def op(self, e, fn, reads=(), writes=(), inc=True, par=False):
        self._wait(e, self._deps(reads, writes, par, e))
        ins = fn()
        self.ninst[e] += 1
        if inc:
            ins.then_inc(self.sem[e], 1)
            self.cnt[e] += 1
            ticket = (e, self.cnt[e])
        else:
            assert e == "pe"
            ticket = (e, self.cnt[e] + 1)
        self._commit(ticket, reads, writes, par)
        return ticket

    def dma(self, q, out, in_, reads=(), writes=(), sem=None, par=False, **kw):
        key = self.dsem(sem)
        deps = self._deps(reads, writes, par)
        if self.cnt[key] > 0:
            deps.append((key, self.cnt[key]))
        self._wait(q, deps)
        ins = self.eng[q].dma_start(out=out, in_=in_, **kw)
        ins.then_inc(self.sem[key], 16)
        self.ninst[q] += 1
        self.cnt[key] += 16
        ticket = (key, self.cnt[key])
        self._commit(ticket, reads, writes, par)
        return ticket

    def barrier(self, engines=("pe", "act", "dve", "sp")):
        deps = []
        for rr in self.res.values():
            deps.extend(rr["w"].items())
            deps.extend(rr["r"].items())
        deps = [d for d in deps if d is not None and not str(d[0]).startswith("cast")]
        for e in engines:
            self._wait(e, deps)
        for n in list(self.res.keys()):
            if not n.startswith("D:"):
                del self.res[n]

    def finish(self, e="sp"):
        deps = []
        for rr in self.res.values():
            deps.extend(rr["w"].items())
            deps.extend(rr["r"].items())
        self._wait(e, deps)

    def psf(self):
        while True:
            i = self._pi % len(self._psf)
            self._pi += 1
            if i not in getattr(self, "reserved", ()):
                return self._psf[i], f"psf{i}"

    def psf_at(self, i):
        return self._psf[i], f"psf{i}"

    def psb(self):
        i = self._pb % len(self._psb)
        self._pb += 1
        return self._psb[i], f"psb{i}"


class WStream:
    def __init__(self, c, st, name, shape, nbuf, groups):
        self.c = c
        self.name = name
        self.nbuf = nbuf
        self.groups = groups
        self.tiles = [c.sb(st, f"{name}{i}", shape, BF16) for i in range(nbuf)]
        self.il = 0
        self.iu = 0

    def _load(self):
        ap, deps = self.groups[self.il]
        slot = self.il % self.nbuf
        self.c.dma("sp", self.tiles[slot][:], ap, reads=deps, writes=[f"{self.name}{slot}"], sem=f"{self.name}{slot}")
        self.il += 1

    def next(self, ahead=None):
        if ahead is None:
            ahead = self.nbuf - 1
        while self.il < min(self.iu + 1 + ahead, len(self.groups)):
            self._load()
        slot = self.iu % self.nbuf
        self.iu += 1
        return self.tiles[slot], f"{self.name}{slot}"


def _t5_bucket(dist):
    n = np.maximum(dist, 0)
    exact = 16
    nf = np.maximum(n, 1).astype(np.float32)
    large = exact + (np.log(nf / np.float32(exact)) / np.float32(math.log(128 / exact)) * np.float32(16)).astype(np.int32)
    large = np.minimum(large, 31)
    return np.where(n < exact, n, large)


def _constants():
    cst = {}
    half = 64
    inv = (np.float32(10000.0) ** (-np.arange(half, dtype=np.float32) / np.float32(half))).astype(np.float32)
    pos = np.arange(S, dtype=np.float32)
    ang = (pos[:, None] * inv[None, :]).astype(np.float32)
    cos = np.cos(ang).astype(np.float32).T
    sin = np.sin(ang).astype(np.float32).T
    cst["c_cos"] = np.ascontiguousarray(np.concatenate([cos, cos], 0))
    cst["c_sin"] = np.ascontiguousarray(np.concatenate([-sin, sin], 0))
    H = 8
    L = 128
    lg = np.log1p(-np.exp2(-5.0 - np.arange(H, dtype=np.float64)))
    idx = np.arange(L, dtype=np.float64)
    scale = 128 ** -0.5
    diff = idx[None, :] - idx[:, None]
    dt = np.where(diff[None] >= 0, np.exp(np.maximum(diff[None], 0) * lg[:, None, None]), 0.0) * scale
    cst["c_dt"] = np.ascontiguousarray(dt.transpose(1, 0, 2)).astype(np.float32)
    xi = np.exp((idx + 1.0)[None, :] * lg[:, None]) * scale
    cst["c_xi"] = np.ascontiguousarray(np.broadcast_to(xi[None], (128, H, L))).astype(np.float32)
    zeta = np.exp((L - 1 - idx)[None, :] * lg[:, None])
    cst["c_zeta"] = np.ascontiguousarray(zeta.T).astype(np.float32)
    gl = np.exp(L * lg)
    cst["_gl"] = [float(np.float32(v)) for v in gl]
    g16 = np.zeros((128, H, 16), np.float32)
    g16[:, :, 1:] = gl.astype(np.float32)[None, :, None]
    cst["c_g16"] = g16
    cst["c_mask"] = (idx[:, None] <= idx[None, :]).astype(np.float32)
    cst["c_ident"] = np.eye(128, dtype=np.float32)
    pm = np.zeros((128, 16, 8), np.float32)
    pi = np.zeros((128, 16, 8), np.float32)
    for t in range(16):
        own = t // 2
        for j in range(8):
            if j < own:
                pi[:, t, j] = 1.0
            else:
                pm[:, t, j] = -1e30
    cst["c_pm"] = pm
    cst["c_pi"] = pi
    k = np.arange(128)[:, None]
    q = np.arange(128)[None, :]
    cst["_b0"] = _t5_bucket(q - k)
    cst["_m0"] = (k <= q)
    cst["_b1"] = _t5_bucket(q - k + 128)
    return cst


class _Stop(Exception):
    pass


class _SkipPhase(Exception):
    pass


class CleanStack(ExitStack):
    def __exit__(self, *exc):
        super().__exit__(None, None, None)
        return bool(exc and exc[0] is _SkipPhase)


def build_nc(n_seq=2, depth=DEPTH, dbg=False, gl=None, stop=None, skip=()):
    T = n_seq * S
    nc = bass.Bass("TRN2", target_bir_lowering=False)

    def din(name, shape, dt=F32):
        return nc.dram_tensor(name, list(shape), dt, kind="ExternalInput").ap()

    x_in = din("x", [T, D])
    w_in = din("w_in", [DEPTH, D, D_IN])
    w_br = [din("w_branch_ret", [DEPTH, 1024, D]), din("w_branch_mlstm", [DEPTH, 1024, D]),
            din("w_branch_moba", [DEPTH, 1024, D])]
    w_out = din("w_out", [DEPTH, D, D])
    w_ff1 = din("w_ff1", [DEPTH, D, D_FF])
    w_ff2 = din("w_ff2", [DEPTH, D_FF, D])
    gate_b = din("mlstm_gate_b", [DEPTH, 2, 4])
    conv_wT = din("conv_wT", [DEPTH, 1024, 4])
    conv_b = din("mlstm_conv_b", [DEPTH, 1024])
    ml_nw = din("mlstm_norm_w", [DEPTH, 1024])
    nmix = din("nmixT", [DEPTH, 128, 16])
    nmlp = din("nmlpT", [DEPTH, 128, 16])
    nfin = din("nfinT", [128, 16])
    rel_bias = din("rel_bias", [32, 8])
    mb_bias = din("mb_bias", [128, 8, 2, 128])
    c_cos = din("c_cos", [128, S])
    c_sin = din("c_sin", [128, S])
    c_dt = din("c_dt", [128, 8, 128])
    c_xi = din("c_xi", [128, 8, 128])
    c_zeta = din("c_zeta", [128, 8])
    c_g16 = din("c_g16", [128, 8, 16])
    c_mask = din("c_mask", [128, 128])
    c_ident = din("c_ident", [128, 128])
    c_pm = din("c_pm", [128, 16, 8])
    c_pi = din("c_pi", [128, 16, 8])
    out = nc.dram_tensor("out", [T, D], F32, kind="ExternalOutput").ap()

    def dscr(name, shape, dt):
        return nc.dram_tensor(name, list(shape), dt, kind="Internal").ap()

    xT = dscr("xT", [16, 128, T], F32)
    ybuf = dscr("ybuf", [3, 8, 128, S], BF16)
    gsc = dscr("gsc", [2, 4, S], F32)
    gs2 = dscr("gs2", [64, 2], F32)
    gs3 = dscr("gs3", [4, 16, 5], F32)
    gs4 = dscr("gs4", [2, 64, 128], F32)
    if dbg:
        dbg_y = nc.dram_tensor("dbg_y", [3, 8, 128, S], BF16, kind="ExternalOutput").ap()
        dbg_x1 = nc.dram_tensor("dbg_x1", [16, 128, S], F32, kind="ExternalOutput").ap()
        dbg_x2 = nc.dram_tensor("dbg_x2", [16, 128, S], F32, kind="ExternalOutput").ap()

    c = Ctx(nc)
    ncast = [0]

    def cast_family(name, src2d, K, ncols, gw, col0=0):
        kc = K // 128
        G = ncols // gw
        dst = dscr("wb_" + name, [G, 128, kc, gw], BF16)
        srcv = src2d.rearrange("(kc p) n -> p kc n", p=128)
        groups = []
        for g in range(G):
            deps = []
            for k0 in range(0, kc, 16):
                k1 = min(kc, k0 + 16)
                rn = f"D:wb_{name}_{g}_{k0}"
                c.dma("pool", dst[g][:, k0:k1, :], srcv[:, k0:k1, col0 + g * gw: col0 + (g + 1) * gw],
                      writes=[rn], sem=f"cast{ncast[0] % 16}")
                ncast[0] += 1
                deps.append(rn)
            groups.append((dst[g], deps))
        return groups

    with c.es:
        gst = c.es
        for i in range(6):
            c._psf.append(gst.enter_context(nc.psum_tensor(f"psf{i}", [128, 512], F32)))
        for i in range(2):
            c._psb.append(gst.enter_context(nc.psum_tensor(f"psb{i}", [128, 1024], BF16)))
        A = c.sb(gst, "A", [128, 16, S], BF16)
        identf = c.sb(gst, "identf", [128, 128], F32)
        identb = c.sb(gst, "identb", [128, 128], BF16)
        onesb = c.sb(gst, "onesb", [128, 128], BF16)
        nw_all = c.sb(gst, "nw_all", [128, 2 * DEPTH + 1, 16], F32)

        c.dma("sp", identf[:], c_ident, writes=["identf"], sem="k0")
        c.op("dve", lambda: nc.vector.tensor_copy(out=identb[:], in_=identf[:]), reads=["identf"], writes=["identb"])
        c.op("dve", lambda: nc.vector.memset(onesb[:], 1.0), writes=["onesb"])
        for l in range(DEPTH):
            c.dma("sp", nw_all[:, 2 * l, :], nmix[l], writes=["nw_all"], sem="k0")
            c.dma("sp", nw_all[:, 2 * l + 1, :], nmlp[l], writes=["nw_all"], sem="k0")
        c.dma("sp", nw_all[:, 2 * DEPTH, :], nfin, writes=["nw_all"], sem="k0")

        W = []
        for l in range(depth):
            wl = {}
            wi = w_in[l]
            wl["rv"] = cast_family(f"rv{l}", wi, D, 1024, 512, O_RV)
            wl["rg"] = cast_family(f"rg{l}", wi, D, 1024, 512, O_RG)
            wl["rq"] = cast_family(f"rq{l}", wi, D, 1024, 128, O_RQ)
            wl["rk"] = cast_family(f"rk{l}", wi, D, 1024, 128, O_RK)
            wl["mi"] = cast_family(f"mi{l}", wi, D, 4, 4, O_MI)
            wl["mf"] = cast_family(f"mf{l}", wi, D, 4, 4, O_MF)
            wl["mv"] = cast_family(f"mv{l}", wi, D, 1024, 512, O_MV)
            wl["mo"] = cast_family(f"mo{l}", wi, D, 1024, 512, O_MO)
            wl["mq"] = cast_family(f"mq{l}", wi, D, 512, 128, O_MQ)
            wl["mk"] = cast_family(f"mk{l}", wi, D, 512, 128, O_MK)
            wl["bv"] = cast_family(f"bv{l}", wi, D, 1024, 512, O_BV)
            wl["bq"] = cast_family(f"bq{l}", wi, D, 1024, 128, O_BQ)
            wl["bk"] = cast_family(f"bk{l}", wi, D, 1024, 128, O_BK)
            wl["g"] = cast_family(f"g{l}", wi, D, 6144, 128, O_G)
            for i in range(3):
                wl[f"br{i}"] = cast_family(f"br{i}_{l}", w_br[i][l], 1024, D, 128)
            wl["out"] = cast_family(f"out{l}", w_out[l], D, D, 128)
            wl["ff1"] = cast_family(f"ff1_{l}", w_ff1[l], D, D_FF, 128)
            wl["ff2"] = cast_family(f"ff2_{l}", w_ff2[l], D_FF, D, 128)
            W.append(wl)

        def mm(o, lhsT, rhs, start, stop, reads, writes):
            return c.op("pe", lambda: nc.tensor.matmul(o, lhsT, rhs, start=start, stop=stop),
                        reads=reads, writes=writes, inc=bool(stop))

        def proj_fm(wt, wn, t0, ps, pn, kc=16, src=None, srcn="A", m=128):
            srcT = A if src is None else src
            for k in range(kc):
                mm(ps[0:m, :], wt[:, k, 0:m], srcT[:, k, t0:t0 + 512], k == 0, k == kc - 1, [wn, srcn], [pn])

        def transpose_bf(dst_fn, src_fn, n, reads, dst_reads_writes):
            pb, pbn = c.psb()
            for j in range(n):
                c.op("pe", lambda j=j: nc.tensor.transpose(pb[:, j * 128:(j + 1) * 128], src_fn(j), identb[:]),
                     reads=list(reads) + ["identb"], writes=[pbn], inc=(j == n - 1))
            return pb, pbn

        def norm_block(st_tiles, xblk, xn, nwi, dst, dstn):
            sqb, rs = st_tiles
            c.op("act", lambda: nc.scalar.activation(out=sqb[:], in_=xblk[:], func=AF.Square), reads=[xn], writes=["sqb"])
            ps, pn = c.psf()
            for k in range(16):
                mm(ps[:], onesb[:], sqb[:, k, :], k == 0, k == 15, ["onesb", "sqb"], [pn])
            c.op("act", lambda: nc.scalar.activation(out=rs[:], in_=ps[:], func=AF.Sqrt, scale=1.0 / D, bias=EPS),
                 reads=[pn], writes=["rs"])
            c.op("dve", lambda: nc.vector.reciprocal(out=rs[:], in_=rs[:]), reads=["rs"], writes=["rs"])
            for k in range(16):
                c.op("dve", lambda k=k: nc.vector.scalar_tensor_tensor(
                    out=dst(k), in0=xblk[:, k, :], scalar=nw_all[:, nwi, k:k + 1], in1=rs[:],
                    op0=ALU.mult, op1=ALU.mult), reads=[xn, "rs", "nw_all"], writes=[dstn], par=True)

        xTv = xT.rearrange("dc p t -> p dc t")

        with CleanStack() as st:
            xt = [c.sb(st, f"xt{i}", [128, D], F32) for i in range(2)]
            xs = [c.sb(st, f"xs{i}", [128, 16, 128], F32) for i in range(2)]
            for tt in range(T // 128):
                b = tt % 2
                c.dma("sp", xt[b][:], x_in[tt * 128:(tt + 1) * 128, :], writes=[f"xt{b}"], sem=f"xt{b}")
                for q4 in range(4):
                    ps, pn = c.psf()
                    for j in range(4):
                        dcx = q4 * 4 + j
                        c.op("pe", lambda j=j, dcx=dcx, ps=ps: nc.tensor.transpose(
                            ps[:, j * 128:(j + 1) * 128], xt[b][:, dcx * 128:(dcx + 1) * 128], identf[:]),
                            reads=[f"xt{b}", "identf"], writes=[pn], inc=(j == 3))
                    eng = "act" if q4 % 2 == 0 else "dve"
                    dstap = xs[b][:, q4 * 4:(q4 + 1) * 4, :]
                    if eng == "act":
                        c.op("act", lambda ps=ps, dstap=dstap: nc.scalar.activation(
                            out=dstap, in_=ps[:].rearrange("p (j k) -> p j k", j=4), func=AF.Copy),
                            reads=[pn], writes=[f"xs{b}"], par=True)
                    else:
                        c.op("dve", lambda ps=ps, dstap=dstap: nc.vector.tensor_copy(
                            out=dstap, in_=ps[:].rearrange("p (j k) -> p j k", j=4)),
                            reads=[pn], writes=[f"xs{b}"], par=True)
                blk = tt // 4
                c.dma("sp", xTv[:, :, tt * 128:(tt + 1) * 128], xs[b][:], reads=[f"xs{b}"], writes=[f"D:xT{blk}"],
                      sem=f"xs{b}", par=True)
            c.barrier()

        gl = gl or _constants()["_gl"]

        def phase_done(name):
            if stop == name:
                raise _Stop()

        def body():
            phase_done("p0")
            for l in range(depth):
                for sq in range(n_seq):
                    tok0 = sq * S
                    with CleanStack() as st:
                        xb = [c.sb(st, f"xb{i}", [128, 16, 512], F32) for i in range(2)]
                        sqb = c.sb(st, "sqb", [128, 16, 512], BF16)
                        rs = c.sb(st, "rs", [128, 512], F32)
                        for tb in range(4):
                            b = tb % 2
                            blk = (tok0 + tb * 512) // 512
                            c.dma("sp", xb[b][:], xTv[:, :, tok0 + tb * 512: tok0 + (tb + 1) * 512],
                                  reads=[f"D:xT{blk}"], writes=[f"xb{b}"], sem=f"xb{b}")
                            norm_block((sqb, rs), xb[b], f"xb{b}", 2 * l,
                                       lambda k, tb=tb: A[:, k, tb * 512:(tb + 1) * 512], "A")
                        c.barrier()
                        phase_done("n1")

                    with CleanStack() as st:
                        if "ret" in skip:
                            raise _SkipPhase()
                        COS = c.sb(st, "COS", [128, S], F32)
                        SIN = c.sb(st, "SIN", [128, S], F32)
                        DT = c.sb(st, "DT", [128, 8, 128], F32)
                        XI = c.sb(st, "XI", [128, 8, 128], F32)
                        ZETA = c.sb(st, "ZETA", [128, 8], F32)
                        c.dma("sp", COS[:], c_cos, writes=["COS"], sem="k0")
                        c.dma("sp", SIN[:], c_sin, writes=["SIN"], sem="k1")
                        c.dma("sp", DT[:], c_dt, writes=["DT"], sem="k2")
                        c.dma("sp", XI[:], c_xi, writes=["XI"], sem="k3")
                        c.dma("sp", ZETA[:], c_zeta, writes=["ZETA"], sem="k4")
                        QT = c.sb(st, "QT", [128, S], BF16)
                        KT = c.sb(st, "KT", [128, S], BF16)
                        QX = c.sb(st, "QX", [128, S], BF16)
                        KZ = c.sb(st, "KZ", [128, 16, 128], BF16)
                        VV = c.sb(st, "VV", [128, 16, 512], BF16)
                        SG = c.sb(st, "SG", [128, 16, 512], BF16)
                        t1 = c.sb(st, "t1", [128, 512], F32)
                        t2 = c.sb(st, "t2", [128, 512], F32)
                        G16 = c.sb(st, "G16", [128, 8, 16], F32)
                        c.dma("sp", G16[:], c_g16, writes=["G16"], sem="k5")
                        KVs = c.sb(st, "KVs", [128, 128, 16], F32)
                        GMUL = c.sb(st, "GMUL", [128, 128, 16], F32)
                        Rbn = c.sb(st, "Rbn", [128, 16, 128], BF16)
                        c.op("dve", lambda: nc.vector.memset(KVs[:, :, 0:1], 0.0), writes=["KVs"])
                        PT = [c.sb(st, f"PT{i}", [128, 4, 128], BF16) for i in range(2)]
                        junk = c.sb(st, "junk", [128, 512], F32)
                        ss4 = c.sb(st, "ss4", [128, 4], F32)
                        yn = c.sb(st, "yn", [128, 4, 128], F32)
                        Yst = [c.sb(st, f"Yst{i}", [128, 4, 128], BF16) for i in range(2)]
                        YT = [c.sb(st, f"YT{i}", [128, S], BF16) for i in range(2)]
                        wtm = WStream(c, st, "wtm", [128, 16, 512], 1,
                                      [W[l]["rv"][0], W[l]["rg"][0], W[l]["rv"][1], W[l]["rg"][1]])
                        wfm = WStream(c, st, "wfm", [128, 16, 128], 3,
                                      [W[l][k][h] for h in range(8) for k in ("rq", "rk")])
                        ret_pend = []

                        def ret_epilogue(cg, pO, pOn, h, hc, yt, ytn):
                            c.op("act", lambda pO=pO: nc.scalar.activation(out=junk[:], in_=pO[:], func=AF.Square),
                                 reads=[pOn], writes=["junk"])
                            c.op("dve", lambda: nc.vector.reduce_sum(out=ss4[:], in_=junk[:].rearrange("p (j k) -> p j k", j=4), axis=AX.X),
                                 reads=["junk"], writes=["ss4"])
                            c.op("act", lambda: nc.scalar.activation(out=ss4[:], in_=ss4[:], func=AF.Sqrt, scale=1.0 / 128, bias=EPS),
                                 reads=["ss4"], writes=["ss4"])
                            c.op("dve", lambda: nc.vector.reciprocal(out=ss4[:], in_=ss4[:]), reads=["ss4"], writes=["ss4"])
                            c.op("dve", lambda pO=pO: nc.vector.tensor_tensor(
                                out=yn[:], in0=pO[:].rearrange("p (j k) -> p j k", j=4),
                                in1=ss4[:].unsqueeze(2).to_broadcast([128, 4, 128]), op=ALU.mult),
                                reads=[pOn, "ss4"], writes=["yn"])
                            ys = Yst[cg % 2]
                            ysn = f"Yst{cg % 2}"
                            c.op("dve", lambda ys=ys, cg=cg: nc.vector.tensor_tensor(
                                out=ys[:], in0=yn[:], in1=SG[:, cg * 4:(cg + 1) * 4, hc], op=ALU.mult),
                                reads=["yn", "SG"], writes=[ysn])
                            pb, pbn = transpose_bf(None, lambda j, ys=ys: ys[:, j, :], 4, [ysn], None)
                            c.op("act", lambda pb=pb, cg=cg, yt=yt: nc.scalar.activation(
                                out=yt[:, cg * 512:(cg + 1) * 512], in_=pb[:, 0:512], func=AF.Copy),
                                reads=[pbn], writes=[ytn])
                            if cg == 3:
                                c.dma("sp", ybuf[0, h], yt[:], reads=[ytn], writes=[f"D:yb0_{h}"], sem=ytn)

                        c.reserved = {2, 3}
                        for hg in range(2):
                            if ret_pend:
                                ret_pend.pop()()
                            wv, wvn = wtm.next(ahead=0)
                            for tl in range(16):
                                ps, pn = c.psf()
                                for k in range(16):
                                    mm(ps[:], A[:, k, tl * 128:(tl + 1) * 128], wv[:, k, :], k == 0, k == 15, ["A", wvn], [pn])
                                c.op("act", lambda ps=ps, tl=tl: nc.scalar.activation(out=VV[:, tl, :], in_=ps[:], func=AF.Copy),
                                     reads=[pn], writes=["VV"])
                            wg, wgn = wtm.next(ahead=0)
                            for tl in range(16):
                                ps, pn = c.psf()
                                for k in range(16):
                                    mm(ps[:], A[:, k, tl * 128:(tl + 1) * 128], wg[:, k, :], k == 0, k == 15, ["A", wgn], [pn])
                                c.op("act", lambda ps=ps, tl=tl: nc.scalar.activation(out=SG[:, tl, :], in_=ps[:], func=AF.Silu),
                                     reads=[pn], writes=["SG"])
                            for hl in range(4):
                                h = hg * 4 + hl
                                hc = slice(hl * 128, (hl + 1) * 128)
                                for dstT, dn in ((QT, "QT"), (KT, "KT")):
                                    wt, wn = wfm.next()
                                    for tb in range(4):
                                        ts_ = slice(tb * 512, (tb + 1) * 512)
                                        ps, pn = c.psf()
                                        proj_fm(wt, wn, tb * 512, ps, pn)
                                        c.op("dve", lambda ps=ps, ts_=ts_: nc.vector.tensor_tensor(
                                            out=t1[0:64, :], in0=ps[64:128, :], in1=SIN[0:64, ts_], op=ALU.mult),
                                            reads=[pn, "SIN"], writes=["t1"])
                                        c.op("dve", lambda ps=ps, ts_=ts_: nc.vector.tensor_tensor(
                                            out=t1[64:128, :], in0=ps[0:64, :], in1=SIN[64:128, ts_], op=ALU.mult),
                                            reads=[pn, "SIN"], writes=["t1"])
                                        c.op("dve", lambda ps=ps, ts_=ts_: nc.vector.tensor_tensor(
                                            out=t2[:], in0=ps[:], in1=COS[:, ts_], op=ALU.mult),
                                            reads=[pn, "COS"], writes=["t2"])
                                        c.op("dve", lambda dstT=dstT, ts_=ts_: nc.vector.tensor_tensor(
                                            out=dstT[:, ts_], in0=t1[:], in1=t2[:], op=ALU.add),
                                            reads=["t1", "t2"], writes=[dn])
                                c.op("dve", lambda h=h: nc.vector.tensor_tensor(
                                    out=QX[:].rearrange("p (n l) -> p n l", n=16), in0=QT[:].rearrange("p (n l) -> p n l", n=16),
                                    in1=XI[:, h:h + 1, :].to_broadcast([128, 16, 128]), op=ALU.mult),
                                    reads=["QT", "XI"], writes=["QX"])
                                for cg in range(4):
                                    pb, pbn = transpose_bf(None, lambda j, cg=cg: KT[:, (cg * 4 + j) * 128:(cg * 4 + j + 1) * 128], 4, ["KT"], None)
                                    c.op("act", lambda pb=pb, cg=cg, h=h: nc.scalar.activation(
                                        out=KZ[:, cg * 4:(cg + 1) * 4, :], in_=pb[:, 0:512].rearrange("p (j k) -> p j k", j=4),
                                        func=AF.Copy, scale=ZETA[:, h:h + 1]), reads=[pbn, "ZETA"], writes=["KZ"])
                                for cgk in range(4):
                                    nk = 4 if cgk < 3 else 3
                                    pK, pKn = c.psf()
                                    for j in range(nk):
                                        n = cgk * 4 + j
                                        c.op("pe", lambda j=j, n=n, pK=pK: nc.tensor.matmul(
                                            pK[:, j * 128:(j + 1) * 128], KZ[:, n, :], VV[:, n, hc], start=True, stop=True),
                                            reads=["KZ", "VV"], writes=[pKn], inc=(j == nk - 1))
                                    c.op("act", lambda pK=pK, cgk=cgk, nk=nk: nc.scalar.activation(
                                        out=KVs[:, :, cgk * 4 + 1: cgk * 4 + 1 + nk],
                                        in_=pK[:, 0:nk * 128].rearrange("p (j e) -> p e j", j=nk), func=AF.Copy),
                                        reads=[pKn], writes=["KVs"])
                                c.op("dve", lambda h=h: nc.vector.tensor_copy(
                                    out=GMUL[:], in_=G16[:, h:h + 1, :].to_broadcast([128, 128, 16])), reads=["G16"], writes=["GMUL"])
                                c.op("dve", lambda: nc.vector.tensor_tensor_scan(
                                    out=KVs[:].rearrange("p e n -> p (e n)"), data0=GMUL[:].rearrange("p e n -> p (e n)"),
                                    data1=KVs[:].rearrange("p e n -> p (e n)"),
                                    initial=0.0, op0=ALU.mult, op1=ALU.add), reads=["KVs", "GMUL"], writes=["KVs"])
                                c.op("dve", lambda: nc.vector.tensor_copy(out=Rbn[:], in_=KVs[:].rearrange("p e n -> p n e")),
                                     reads=["KVs"], writes=["Rbn"])
                                yt = YT[h % 2]
                                ytn = f"YT{h % 2}"
                                c.reserved = {2, 3}
                                for cg in range(4):
                                    pS, pSn = c.psf()
                                    for j in range(4):
                                        n = cg * 4 + j
                                        cs = slice(n * 128, (n + 1) * 128)
                                        c.op("pe", lambda j=j, cs=cs, pS=pS: nc.tensor.matmul(
                                            pS[:, j * 128:(j + 1) * 128], KT[:, cs], QT[:, cs], start=True, stop=True),
                                            reads=["KT", "QT"], writes=[pSn], inc=(j == 3))
                                    pt = PT[cg % 2]
                                    ptn = f"PT{cg % 2}"
                                    c.op("dve", lambda pS=pS, pt=pt, h=h: nc.vector.tensor_tensor(
                                        out=pt[:], in0=pS[:].rearrange("p (j k) -> p j k", j=4),
                                        in1=DT[:, h:h + 1, :].to_broadcast([128, 4, 128]), op=ALU.mult),
                                        reads=[pSn, "DT"], writes=[ptn])
                                    pO, pOn = c.psf_at(2 + cg % 2)
                                    for j in range(4):
                                        n = cg * 4 + j
                                        cs = slice(n * 128, (n + 1) * 128)
                                        mm(pO[:, j * 128:(j + 1) * 128], pt[:, j, :], VV[:, n, hc], True, n == 0, [ptn, "VV"], [pOn])
                                        if n > 0:
                                            mm(pO[:, j * 128:(j + 1) * 128], QX[:, cs], Rbn[:, n, :], False, True, ["QX", "Rbn"], [pOn])
                                    if ret_pend:
                                        ret_pend.pop()()
                                    ret_pend.append(lambda cg=cg, pO=pO, pOn=pOn, h=h, hc=hc, yt=yt, ytn=ytn: ret_epilogue(cg, pO, pOn, h, hc, yt, ytn))
                        if ret_pend:
                            ret_pend.pop()()
                        c.reserved = set()
                        c.barrier()
                        phase_done("ret")

                    with CleanStack() as st:
                        if "ml" in skip:
                            raise _SkipPhase()
                        MASK = c.sb(st, "MASK", [128, 128], F32)
                        c.dma("sp", MASK[:], c_mask, writes=["MASK"], sem="k0")
                        gb = c.sb(st, "gb", [4, 2], F32)
                        c.dma("sp", gb[:, 0:1], gate_b[l, 0, :].rearrange("(h o) -> h o", o=1), writes=["gb"], sem="k1")
                        c.dma("sp", gb[:, 1:2], gate_b[l, 1, :].rearrange("(h o) -> h o", o=1), writes=["gb"], sem="k1")
                        c.op("dve", lambda: nc.vector.tensor_scalar(out=gb[:, 1:2], in0=gb[:, 1:2], scalar1=-1.0, scalar2=None, op0=ALU.mult),
                             reads=["gb"], writes=["gb"])
                        NWB = c.sb(st, "NWB", [128, 1024], F32)
                        c.dma("sp", NWB[:], ml_nw[l:l + 1, :].partition_broadcast(128), writes=["NWB"], sem="k2")
                        EMT = c.sb(st, "EMT", [128, 64], F32)
                        SCb = c.sb(st, "SCb", [128, 64, 5], F32)
                        stg = ExitStack()
                        st_outer = st
                        st = stg
                        wif = [c.sb(st, f"wif{i}", [128, 16, 4], BF16) for i in range(2)]
                        c.dma("sp", wif[0][:], W[l]["mi"][0][0], reads=W[l]["mi"][0][1], writes=["wif0"], sem="k3")
                        c.dma("sp", wif[1][:], W[l]["mf"][0][0], reads=W[l]["mf"][0][1], writes=["wif1"], sem="k4")
                        LI = c.sb(st, "LI", [4, S], F32)
                        LF = c.sb(st, "LF", [4, S], F32)
                        for tb in range(4):
                            ts_ = slice(tb * 512, (tb + 1) * 512)
                            ps, pn = c.psf()
                            proj_fm(wif[0], "wif0", tb * 512, ps, pn, m=4)
                            c.op("act", lambda ps=ps, ts_=ts_: nc.scalar.activation(out=LI[:, ts_], in_=ps[0:4, :], func=AF.Identity, bias=gb[:, 0:1]),
                                 reads=[pn, "gb"], writes=["LI"])
                            ps, pn = c.psf()
                            proj_fm(wif[1], "wif1", tb * 512, ps, pn, m=4)
                            c.op("act", lambda ps=ps, ts_=ts_: nc.scalar.activation(out=LF[:, ts_], in_=ps[0:4, :], func=AF.Exp, scale=-1.0, bias=gb[:, 1:2]),
                                 reads=[pn, "gb"], writes=["LF"])
                        c.op("act", lambda: nc.scalar.activation(out=LF[:], in_=LF[:], func=AF.Ln, bias=1.0), reads=["LF"], writes=["LF"])
                        c.dma("sp", gsc[0], LI[:], reads=["LI"], writes=["D:gsc0"], sem="k5")
                        c.dma("sp", gsc[1], LF[:], reads=["LF"], writes=["D:gsc1"], sem="k6")
                        LIf = c.sb(st, "LIf", [64, 128], F32)
                        CS = c.sb(st, "CS", [64, 128], F32)
                        c.dma("sp", LIf[:], gsc[0].rearrange("h (n l) -> (h n) l", l=128), reads=["D:gsc0"], writes=["LIf"], sem="k5")
                        c.dma("sp", CS[:], gsc[1].rearrange("h (n l) -> (h n) l", l=128), reads=["D:gsc1"], writes=["CS"], sem="k6")
                        c.op("dve", lambda: nc.vector.tensor_tensor_scan(out=CS[:], data0=CS[:], data1=CS[:], initial=0.0, op0=ALU.add, op1=ALU.bypass),
                             reads=["CS"], writes=["CS"])
                        Afm = c.sb(st, "Afm", [64, 128], F32)
                        CM = c.sb(st, "CM", [64, 128], F32)
                        c.op("dve", lambda: nc.vector.tensor_tensor(out=Afm[:], in0=LIf[:], in1=CS[:], op=ALU.add), reads=["LIf", "CS"], writes=["Afm"])
                        c.op("dve", lambda: nc.vector.tensor_tensor_scan(out=CM[:], data0=Afm[:], data1=Afm[:], initial=-1e30, op0=ALU.max, op1=ALU.bypass),
                             reads=["Afm"], writes=["CM"])
                        st2 = c.sb(st, "st2", [64, 2], F32)
                        c.op("dve", lambda: nc.vector.tensor_scalar(out=st2[:, 0:1], in0=CS[:, 127:128], scalar1=-1.0, scalar2=None, op0=ALU.mult),
                             reads=["CS"], writes=["st2"])
                        c.op("dve", lambda: nc.vector.tensor_copy(out=st2[:, 1:2], in_=CM[:, 127:128]), reads=["CM"], writes=["st2"])
                        c.dma("sp", gs2, st2[:], reads=["st2"], writes=["D:gs2"], sem="k5")
                        GT = c.sb(st, "GT", [4, 16, 2], F32)
                        c.dma("sp", GT[:], gs2.rearrange("(h n) k -> h n k", n=16), reads=["D:gs2"], writes=["GT"], sem="k5")
                        ML = c.sb(st, "ML", [4, 16], F32)
                        MP = c.sb(st, "MP", [4, 17], F32)
                        c.op("dve", lambda: nc.vector.tensor_tensor(out=ML[:], in0=GT[:, :, 0], in1=GT[:, :, 1], op=ALU.add), reads=["GT"], writes=["ML"])
                        c.op("dve", lambda: nc.vector.memset(MP[:], 0.0), writes=["MP"])
                        for n in range(16):
                            c.op("dve", lambda n=n: nc.vector.scalar_tensor_tensor(
                                out=MP[:, n + 1:n + 2], in0=MP[:, n:n + 1], scalar=GT[:, n, 0:1], in1=ML[:, n:n + 1],
                                op0=ALU.add, op1=ALU.max), reads=["MP", "GT", "ML"], writes=["MP"])
                        SC = c.sb(st, "SC", [4, 16, 5], F32)
                        tq = c.sb(st, "tq", [4, 16], F32)
                        c.op("dve", lambda: nc.vector.tensor_tensor(out=tq[:], in0=GT[:, :, 0], in1=MP[:, 0:16], op=ALU.add), reads=["GT", "MP"], writes=["tq"])
                        c.op("dve", lambda: nc.vector.tensor_tensor(out=tq[:], in0=tq[:], in1=MP[:, 1:17], op=ALU.subtract), reads=["tq", "MP"], writes=["tq"])
                        c.op("act", lambda: nc.scalar.activation(out=SC[:, :, 0], in_=tq[:], func=AF.Exp), reads=["tq"], writes=["SC"])
                        tq2 = c.sb(st, "tq2", [4, 16], F32)
                        c.op("dve", lambda: nc.vector.tensor_tensor(out=tq2[:], in0=ML[:], in1=MP[:, 1:17], op=ALU.subtract), reads=["ML", "MP"], writes=["tq2"])
                        c.op("act", lambda: nc.scalar.activation(out=SC[:, :, 1], in_=tq2[:], func=AF.Exp), reads=["tq2"], writes=["SC"])
                        tq3 = c.sb(st, "tq3", [4, 16], F32)
                        c.op("dve", lambda: nc.vector.tensor_tensor(out=tq3[:], in0=MP[:, 0:16], in1=GT[:, :, 1], op=ALU.subtract), reads=["GT", "MP"], writes=["tq3"])
                        c.op("act", lambda: nc.scalar.activation(out=SC[:, :, 2], in_=tq3[:], func=AF.Exp), reads=["tq3"], writes=["SC"])
                        c.op("dve", lambda: nc.vector.tensor_copy(out=SC[:, :, 3], in_=GT[:, :, 1]), reads=["GT", "SC"], writes=["SC"])
                        c.op("dve", lambda: nc.vector.tensor_copy(out=SC[:, :, 4], in_=MP[:, 0:16]), reads=["MP", "SC"], writes=["SC"])
                        c.dma("sp", gs3, SC[:], reads=["SC"], writes=["D:gs3"], sem="k6")
                        SCc = c.sb(st, "SCc", [64, 5], F32)
                        c.dma("sp", SCc[:], gs3.rearrange("h n k -> (h n) k"), reads=["D:gs3"], writes=["SCc"], sem="k6")
                        c.dma("sp", SCb[:].rearrange("p j k -> p (j k)"),
                              gs3.rearrange("h n k -> (h n k)").rearrange("(o f) -> o f", o=1).partition_broadcast(128),
                              reads=["D:gs3"], writes=["SCb"], sem="k5")
                        Mfm = c.sb(st, "Mfm", [64, 128], F32)
                        c.op("dve", lambda: nc.vector.tensor_scalar(out=Mfm[:], in0=CM[:], scalar1=SCc[:, 4:5], scalar2=None, op0=ALU.max),
                             reads=["CM", "SCc"], writes=["Mfm"])
                        bcol = c.sb(st, "bcol", [64, 2], F32)
                        c.op("dve", lambda: nc.vector.tensor_scalar(out=bcol[:, 0:1], in0=SCc[:, 3:4], scalar1=float(math.log(128 ** -0.5)), scalar2=None, op0=ALU.add),
                             reads=["SCc"], writes=["bcol"])
                        c.op("dve", lambda: nc.vector.tensor_scalar(out=bcol[:, 1:2], in0=SCc[:, 3:4], scalar1=-1.0, scalar2=None, op0=ALU.mult),
                             reads=["SCc", "bcol"], writes=["bcol"])
                        W1f = c.sb(st, "W1f", [64, 128], F32)
                        ELf = c.sb(st, "ELf", [64, 128], F32)
                        EMf = c.sb(st, "EMf", [64, 128], F32)
                        c.op("act", lambda: nc.scalar.activation(out=W1f[:], in_=Mfm[:], func=AF.Exp, scale=-1.0, bias=bcol[:, 0:1]),
                             reads=["Mfm", "bcol"], writes=["W1f"])
                        c.op("act", lambda: nc.scalar.activation(out=ELf[:], in_=Afm[:], func=AF.Exp, bias=bcol[:, 1:2]),
                             reads=["Afm", "bcol"], writes=["ELf"])
                        c.op("dve", lambda: nc.vector.tensor_tensor(out=EMf[:], in0=CS[:], in1=Mfm[:], op=ALU.subtract), reads=["CS", "Mfm"], writes=["EMf"])
                        c.op("act", lambda: nc.scalar.activation(out=EMf[:], in_=EMf[:], func=AF.Exp), reads=["EMf"], writes=["EMf"])
                        c.dma("sp", gs4[0], W1f[:], reads=["W1f"], writes=["D:gs40"], sem="k5")
                        c.dma("sp", gs4[1], ELf[:], reads=["ELf"], writes=["D:gs41"], sem="k6")
                        ps, pn = c.psf()
                        mm(ps[:, 0:64], EMf[:], identf[0:64, 0:64], True, True, ["EMf", "identf"], [pn])
                        c.op("dve", lambda ps=ps: nc.vector.tensor_copy(out=EMT[:], in_=ps[:, 0:64]), reads=[pn], writes=["EMT"])

                        c.barrier()
                        stg.close()
                        st = st_outer
                        phase_done("mlg")
                        XP = c.sb(st, "XP", [128, 3 + S], F32)
                        acc = c.sb(st, "acc", [128, S], F32)
                        CW = c.sb(st, "CW", [128, 8, 5], F32)
                        for qk in range(2):
                            for h in range(4):
                                c0 = qk * 512 + h * 128
                                c.dma("sp", CW[:, qk * 4 + h, 0:4], conv_wT[l, c0:c0 + 128, :], writes=["CW"], sem="k0")
                                c.dma("sp", CW[:, qk * 4 + h, 4:5], conv_b[l, c0:c0 + 128].rearrange("(p o) -> p o", o=1), writes=["CW"], sem="k0")
                        c.op("dve", lambda: nc.vector.memset(XP[:, 0:3], 0.0), writes=["XP"])
                        QcT = c.sb(st, "QcT", [128, S], BF16)
                        KcT = c.sb(st, "KcT", [128, S], BF16)
                        QS = c.sb(st, "QS", [128, S], BF16)
                        KST = c.sb(st, "KST", [128, S], BF16)
                        KSm = c.sb(st, "KSm", [128, 16, 128], BF16)
                        VA = c.sb(st, "VA", [128, 16, 2, 257], BF16)
                        SGO = c.sb(st, "SGO", [128, 16, 512], BF16)
                        sgt = c.sb(st, "sgt", [128, 512], F32)
                        CA = c.sb(st, "CA", [128, 257], F32)
                        CAb = [c.sb(st, f"CAb{i}", [128, 257], BF16) for i in range(2)]
                        PTm = [c.sb(st, f"PTm{i}", [128, 4, 128], BF16) for i in range(2)]
                        sc1 = c.sb(st, "sc1", [128, 4], F32)
                        junk2 = c.sb(st, "junk2", [128, 256], F32)
                        Ym = [c.sb(st, f"Ym{i}", [128, 256], BF16) for i in range(2)]
                        YTm = [c.sb(st, "YTm0", [128, 2, S], BF16)]
                        c.op("dve", lambda: nc.vector.memset(VA[:, :, :, 256:257], 1.0), writes=["VA"])
                        wtm = WStream(c, st, "wtm", [128, 16, 512], 2,
                                      [W[l]["mv"][0], W[l]["mo"][0], W[l]["mv"][1], W[l]["mo"][1]])
                        wfm = WStream(c, st, "wfm", [128, 16, 128], 3,
                                      [W[l][k][h] for h in range(4) for k in ("mq", "mk")])
                        ml_pend = []

                        def ml_epilogue(n, pN, pNn, hn, h, hl, cs, ytm, ytmn):
                            c.op("act", lambda pN=pN: nc.scalar.activation(
                                out=sc1[:, 0:1], in_=pN[:, 256:257], func=AF.Abs), reads=[pNn], writes=["sc1"])
                            c.op("dve", lambda hn=hn: nc.vector.tensor_tensor(
                                out=sc1[:, 0:1], in0=sc1[:, 0:1], in1=EMT[:, hn:hn + 1], op=ALU.max), reads=["sc1", "EMT"], writes=["sc1"])
                            c.op("dve", lambda: nc.vector.reciprocal(out=sc1[:, 0:1], in_=sc1[:, 0:1]), reads=["sc1"], writes=["sc1"])
                            c.op("act", lambda pN=pN: nc.scalar.activation(out=junk2[:], in_=pN[:, 0:256], func=AF.Square, accum_out=sc1[:, 1:2]),
                                 reads=[pNn, "sc1"], writes=["junk2", "sc1"])
                            c.op("dve", lambda: nc.vector.scalar_tensor_tensor(
                                out=sc1[:, 2:3], in0=sc1[:, 0:1], scalar=sc1[:, 0:1], in1=sc1[:, 1:2], op0=ALU.mult, op1=ALU.mult),
                                reads=["sc1"], writes=["sc1"])
                            c.op("act", lambda: nc.scalar.activation(out=sc1[:, 2:3], in_=sc1[:, 2:3], func=AF.Sqrt, scale=1.0 / 256, bias=EPS),
                                 reads=["sc1"], writes=["sc1"])
                            c.op("dve", lambda: nc.vector.reciprocal(out=sc1[:, 2:3], in_=sc1[:, 2:3]), reads=["sc1"], writes=["sc1"])
                            c.op("dve", lambda: nc.vector.tensor_tensor(out=sc1[:, 3:4], in0=sc1[:, 2:3], in1=sc1[:, 0:1], op=ALU.mult),
                                 reads=["sc1"], writes=["sc1"])
                            ym = Ym[n % 2]
                            ymn = f"Ym{n % 2}"
                            c.op("dve", lambda pN=pN, ym=ym, n=n, hl=hl: nc.vector.scalar_tensor_tensor(
                                out=ym[:], in0=pN[:, 0:256], scalar=sc1[:, 3:4], in1=SGO[:, n, hl * 256:(hl + 1) * 256],
                                op0=ALU.mult, op1=ALU.mult), reads=[pNn, "sc1", "SGO"], writes=[ymn])
                            pb, pbn = transpose_bf(None, lambda jj, ym=ym: ym[:, jj * 128:(jj + 1) * 128], 2, [ymn], None)
                            c.op("act", lambda pb=pb, ytm=ytm, cs=cs: nc.scalar.activation(
                                out=ytm[:, :, cs], in_=pb[:, 0:256].rearrange("p (a b) -> p a b", a=2), func=AF.Copy),
                                reads=[pbn], writes=[ytmn])
                            if n == 15:
                                c.dma("sp", ybuf[1, 2 * h:2 * h + 2].rearrange("e p t -> p e t"), ytm[:], reads=[ytmn],
                                      writes=[f"D:yb1_{2 * h}", f"D:yb1_{2 * h + 1}"], sem=ytmn)

                        c.reserved = {2, 3}
                        for hp in range(2):
                            if ml_pend:
                                ml_pend.pop()()
                            wv, wvn = wtm.next(ahead=1)
                            wo, won = wtm.next(ahead=0)
                            for tl in range(16):
                                ps, pn = c.psf()
                                for k in range(16):
                                    mm(ps[:], A[:, k, tl * 128:(tl + 1) * 128], wv[:, k, :], k == 0, k == 15, ["A", wvn], [pn])
                                c.op("act", lambda ps=ps, tl=tl: nc.scalar.activation(
                                    out=VA[:, tl, :, 0:256], in_=ps[:].rearrange("p (a b) -> p a b", a=2), func=AF.Copy),
                                    reads=[pn], writes=["VA"])
                                ps, pn = c.psf()
                                for k in range(16):
                                    mm(ps[:], A[:, k, tl * 128:(tl + 1) * 128], wo[:, k, :], k == 0, k == 15, ["A", won], [pn])
                                c.op("act", lambda ps=ps: nc.scalar.activation(out=sgt[:], in_=ps[:], func=AF.Sigmoid),
                                     reads=[pn], writes=["sgt"])
                                c.op("dve", lambda tl=tl, hp=hp: nc.vector.tensor_tensor(
                                    out=SGO[:, tl, :], in0=sgt[:], in1=NWB[:, hp * 512:(hp + 1) * 512], op=ALU.mult),
                                    reads=["sgt", "NWB"], writes=["SGO"])
                            for hl in range(2):
                                h = hp * 2 + hl
                                for qk, dstT, dn in ((0, QcT, "QcT"), (1, KcT, "KcT")):
                                    wt, wn = wfm.next()
                                    ci = qk * 4 + h
                                    for tb in range(4):
                                        ps, pn = c.psf()
                                        proj_fm(wt, wn, tb * 512, ps, pn)
                                        c.op("act", lambda ps=ps, tb=tb: nc.scalar.activation(
                                            out=XP[:, 3 + tb * 512: 3 + (tb + 1) * 512], in_=ps[:], func=AF.Copy),
                                            reads=[pn], writes=["XP"])
                                    c.op("dve", lambda ci=ci: nc.vector.tensor_scalar(
                                        out=acc[:], in0=XP[:, 3:3 + S], scalar1=CW[:, ci, 3:4], scalar2=None, op0=ALU.mult),
                                        reads=["XP", "CW"], writes=["acc"])
                                    for j in (2, 1, 0):
                                        c.op("dve", lambda ci=ci, j=j: nc.vector.scalar_tensor_tensor(
                                            out=acc[:], in0=XP[:, j:j + S], scalar=CW[:, ci, j:j + 1], in1=acc[:],
                                            op0=ALU.mult, op1=ALU.add), reads=["XP", "CW", "acc"], writes=["acc"])
                                    c.op("act", lambda ci=ci, dstT=dstT: nc.scalar.activation(
                                        out=dstT[:], in_=acc[:], func=AF.Silu, bias=CW[:, ci, 4:5]),
                                        reads=["acc", "CW"], writes=[dn])
                                c.dma("sp", acc[:], gs4[0, h * 16:(h + 1) * 16, :].rearrange("n l -> (n l)").rearrange("(o f) -> o f", o=1).partition_broadcast(128),
                                      reads=["D:gs40"], writes=["acc"], sem="k1")
                                c.dma("sp", XP[:, 3:3 + S], gs4[1, h * 16:(h + 1) * 16, :].rearrange("n l -> (n l)").rearrange("(o f) -> o f", o=1).partition_broadcast(128),
                                      reads=["D:gs41"], writes=["XP"], sem="k2")
                                c.op("dve", lambda: nc.vector.tensor_tensor(out=QS[:], in0=QcT[:], in1=acc[:], op=ALU.mult),
                                     reads=["QcT", "acc"], writes=["QS"])
                                c.op("dve", lambda: nc.vector.tensor_tensor(out=KST[:], in0=KcT[:], in1=XP[:, 3:3 + S], op=ALU.mult),
                                     reads=["KcT", "XP"], writes=["KST"])
                                for cg in range(4):
                                    pb, pbn = transpose_bf(None, lambda j, cg=cg: KST[:, (cg * 4 + j) * 128:(cg * 4 + j + 1) * 128], 4, ["KST"], None)
                                    c.op("act", lambda pb=pb, cg=cg: nc.scalar.activation(
                                        out=KSm[:, cg * 4:(cg + 1) * 4, :], in_=pb[:, 0:512].rearrange("p (j k) -> p j k", j=4), func=AF.Copy),
                                        reads=[pbn], writes=["KSm"])
                                c.op("dve", lambda: nc.vector.memset(CA[:], 0.0), writes=["CA"])
                                ytm = YTm[0]
                                ytmn = "YTm0"
                                for cg in range(4):
                                    pS, pSn = c.psf()
                                    for j in range(4):
                                        n = cg * 4 + j
                                        cs = slice(n * 128, (n + 1) * 128)
                                        c.op("pe", lambda j=j, cs=cs, pS=pS: nc.tensor.matmul(
                                            pS[:, j * 128:(j + 1) * 128], KST[:, cs], QS[:, cs], start=True, stop=True),
                                            reads=["KST", "QS"], writes=[pSn], inc=(j == 3))
                                    pt = PTm[cg % 2]
                                    ptn = f"PTm{cg % 2}"
                                    c.op("dve", lambda pS=pS, pt=pt: nc.vector.tensor_tensor(
                                        out=pt[:], in0=pS[:].rearrange("p (j k) -> p j k", j=4),
                                        in1=MASK[:].unsqueeze(1).to_broadcast([128, 4, 128]), op=ALU.mult),
                                        reads=[pSn, "MASK"], writes=[ptn])
                                    for j in range(4):
                                        n = cg * 4 + j
                                        hn = h * 16 + n
                                        cs = slice(n * 128, (n + 1) * 128)
                                        pN, pNn = c.psf_at(2 + n % 2)
                                        mm(pN[:, 0:257], pt[:, j, :], VA[:, n, hl, :], True, n == 0, [ptn, "VA"], [pNn])
                                        if n > 0:
                                            mm(pN[:, 0:257], QS[:, cs], CAb[n % 2][:], False, True, ["QS", f"CAb{n % 2}"], [pNn])
                                        if n < 15:
                                            pC, pCn = c.psf()
                                            mm(pC[:, 0:257], KSm[:, n, :], VA[:, n, hl, :], True, True, ["KSm", "VA"], [pCn])
                                            c.op("dve", lambda hn=hn: nc.vector.tensor_scalar(
                                                out=CA[:], in0=CA[:], scalar1=SCb[:, hn, 0:1], scalar2=None, op0=ALU.mult),
                                                reads=["CA", "SCb"], writes=["CA"])
                                            c.op("dve", lambda hn=hn, pC=pC: nc.vector.scalar_tensor_tensor(
                                                out=CA[:], in0=pC[:, 0:257], scalar=SCb[:, hn, 1:2], in1=CA[:], op0=ALU.mult, op1=ALU.add),
                                                reads=["CA", "SCb", pCn], writes=["CA"])
                                            cb = CAb[(n + 1) % 2]
                                            c.op("act", lambda cb=cb, hn=hn: nc.scalar.activation(
                                                out=cb[:], in_=CA[:], func=AF.Copy, scale=SCb[:, hn + 1, 2:3]),
                                                reads=["CA", "SCb"], writes=[f"CAb{(n + 1) % 2}"])
                                        if ml_pend:
                                            ml_pend.pop()()
                                        ml_pend.append(lambda n=n, pN=pN, pNn=pNn, hn=hn, h=h, hl=hl, cs=cs, ytm=ytm, ytmn=ytmn:
                                                       ml_epilogue(n, pN, pNn, hn, h, hl, cs, ytm, ytmn))
                        if ml_pend:
                            ml_pend.pop()()
                        c.reserved = set()
                        c.barrier()
                        phase_done("ml")

                    with CleanStack() as st:
                        if "mb" in skip:
                            raise _SkipPhase()
                        BIAS = c.sb(st, "BIAS", [128, 8, 2, 128], F32)
                        CB = c.sb(st, "CB", [128, 8], F32)
                        PMK = c.sb(st, "PMK", [128, 16, 8], F32)
                        PIK = c.sb(st, "PIK", [128, 16, 8], F32)
                        c.dma("sp", BIAS[:], mb_bias, writes=["BIAS"], sem="k0")
                        c.dma("sp", CB[:], rel_bias[31:32, :].partition_broadcast(128), writes=["CB"], sem="k1")
                        c.dma("sp", PMK[:], c_pm, writes=["PMK"], sem="k2")
                        c.dma("sp", PIK[:], c_pi, writes=["PIK"], sem="k3")
                        QT = c.sb(st, "QT", [128, S], BF16)
                        QTf = c.sb(st, "QTf", [128, S], F32)
                        KT = c.sb(st, "KT", [128, S], BF16)
                        KM = c.sb(st, "KM", [128, 8], F32)
                        KMh = c.sb(st, "KMh", [128, 8], BF16)
                        KMl = c.sb(st, "KMl", [128, 8], BF16)
                        QL = c.sb(st, "QL", [128, S], BF16)
                        VB = c.sb(st, "VB", [128, 16, 4, 129], BF16)
                        GM = c.sb(st, "GM", [128, 16, 8], F32)
                        MX = c.sb(st, "MX", [128, 16, 8], F32)
                        SEL = c.sb(st, "SEL", [128, 16, 8], F32)
                        ACC = c.sb(st, "ACC", [128, 16, 129], F32)
                        REC = c.sb(st, "REC", [128, 16], F32)
                        PTb = [c.sb(st, f"PTb{i}", [128, 512], BF16) for i in range(4)]
                        tb_ = c.sb(st, "tb_", [128, 128], F32)
                        Yb = c.sb(st, "Yb", [128, 16, 128], BF16)
                        YTb = [c.sb(st, f"YTb{i}", [128, S], BF16) for i in range(2)]
                        c.op("dve", lambda: nc.vector.memset(VB[:, :, :, 128:129], 1.0), writes=["VB"])
                        wtm = WStream(c, st, "wtm", [128, 16, 512], 2, [W[l]["bv"][0], W[l]["bv"][1]])
                        wfm = WStream(c, st, "wfm", [128, 16, 128], 3,
                                      [W[l][k][h] for h in range(8) for k in ("bq", "bk")])
                        ipt = [0]
                        for hg in range(2):
                            wv, wvn = wtm.next()
                            for tl in range(16):
                                ps, pn = c.psf()
                                for k in range(16):
                                    mm(ps[:], A[:, k, tl * 128:(tl + 1) * 128], wv[:, k, :], k == 0, k == 15, ["A", wvn], [pn])
                                c.op("act", lambda ps=ps, tl=tl: nc.scalar.activation(
                                    out=VB[:, tl, :, 0:128], in_=ps[:].rearrange("p (a b) -> p a b", a=4), func=AF.Copy),
                                    reads=[pn], writes=["VB"])
                            if stop == "mb_v":
                                c.barrier()
                                phase_done("mb_v")
                            for hl in range(4):
                                h = hg * 4 + hl
                                wt, wn = wfm.next()
                                for tb in range(4):
                                    ts_ = slice(tb * 512, (tb + 1) * 512)
                                    ps, pn = c.psf()
                                    proj_fm(wt, wn, tb * 512, ps, pn)
                                    c.op("act", lambda ps=ps, ts_=ts_: nc.scalar.activation(out=QTf[:, ts_], in_=ps[:], func=AF.Copy, scale=float(128 ** -0.5)),
                                         reads=[pn], writes=["QTf"])
                                    c.op("dve", lambda ts_=ts_: nc.vector.tensor_copy(out=QT[:, ts_], in_=QTf[:, ts_]), reads=["QTf"], writes=["QT"])
                                wt, wn = wfm.next()
                                for tb in range(4):
                                    ts_ = slice(tb * 512, (tb + 1) * 512)
                                    ps, pn = c.psf()
                                    proj_fm(wt, wn, tb * 512, ps, pn)
                                    c.op("act", lambda ps=ps, ts_=ts_: nc.scalar.activation(out=KT[:, ts_], in_=ps[:], func=AF.Copy),
                                         reads=[pn], writes=["KT"])
                                    c.op("dve", lambda ps=ps, tb=tb: nc.vector.reduce_sum(
                                        out=KM[:, 2 * tb:2 * tb + 2], in_=ps[:].rearrange("p (a b) -> p a b", a=2), axis=AX.X),
                                        reads=[pn], writes=["KM"])
                                c.op("dve", lambda: nc.vector.tensor_scalar(out=KM[:], in0=KM[:], scalar1=1.0 / 256, scalar2=None, op0=ALU.mult),
                                     reads=["KM"], writes=["KM"])
                                if stop == "mb_km":
                                    c.barrier()
                                    phase_done("mb_km")
                                c.op("dve", lambda: nc.vector.tensor_tensor(out=QL[:], in0=QTf[:], in1=QT[:], op=ALU.subtract),
                                     reads=["QTf", "QT"], writes=["QL"])
                                c.op("dve", lambda: nc.vector.tensor_copy(out=KMh[:], in_=KM[:]), reads=["KM"], writes=["KMh"])
                                c.op("dve", lambda: nc.vector.tensor_tensor(out=KMl[:], in0=KM[:], in1=KMh[:], op=ALU.subtract),
                                     reads=["KM", "KMh"], writes=["KMl"])
                                pG, pGn = c.psf()
                                for t in range(16):
                                    tsl = slice(t * 128, (t + 1) * 128)
                                    mm(pG[:, t * 8:(t + 1) * 8], QT[:, tsl], KMh[:], True, False, ["QT", "KMh"], [pGn])
                                    mm(pG[:, t * 8:(t + 1) * 8], QT[:, tsl], KMl[:], False, False, ["QT", "KMl"], [pGn])
                                    mm(pG[:, t * 8:(t + 1) * 8], QL[:, tsl], KMh[:], False, True, ["QL", "KMh"], [pGn])
                                c.op("dve", lambda pG=pG: nc.vector.tensor_tensor(
                                    out=GM[:], in0=pG[:, 0:128].rearrange("p (t j) -> p t j", t=16), in1=PMK[:], op=ALU.add),
                                    reads=[pGn, "PMK"], writes=["GM"])
                                if stop == "mb_gm":
                                    c.barrier()
                                    phase_done("mb_gm")
                                for t in range(16):
                                    c.op("dve", lambda t=t: nc.vector.max(out=MX[:, t, :], in_=GM[:, t, :]), reads=["GM"], writes=["MX"])
                                for t in range(16):
                                    c.op("dve", lambda t=t: nc.vector.scalar_tensor_tensor(
                                        out=SEL[:, t, :], in0=GM[:, t, :], scalar=MX[:, t, 2:3], in1=PIK[:, t, :], op0=ALU.is_ge, op1=ALU.mult),
                                        reads=["GM", "MX", "PIK"], writes=["SEL"])
                                if stop == "mb_sel":
                                    c.barrier()
                                    phase_done("mb_sel")
                                c.op("dve", lambda: nc.vector.memset(ACC[:], 0.0), writes=["ACC"])
                                def mb_stage_a(g, j):
                                    pts = {}
                                    for kt in (2 * j, 2 * j + 1):
                                        t_lo = max(kt, 4 * g)
                                        if t_lo > 4 * g + 3:
                                            continue
                                        ncol = (4 * g + 4 - t_lo) * 128
                                        q0 = t_lo * 128
                                        pS, pSn = c.psf()
                                        mm(pS[:, 0:ncol], KT[:, kt * 128:(kt + 1) * 128], QT[:, q0:q0 + ncol], True, True, ["KT", "QT"], [pSn])
                                        pt = PTb[ipt[0] % 4]
                                        ptn = f"PTb{ipt[0] % 4}"
                                        ipt[0] += 1
                                        tcur = t_lo
                                        while tcur <= 4 * g + 3:
                                            o0 = (tcur - t_lo) * 128
                                            if tcur - kt <= 1:
                                                kind = tcur - kt
                                                c.op("dve", lambda pS=pS, o0=o0, kind=kind: nc.vector.tensor_tensor(
                                                    out=tb_[:], in0=pS[:, o0:o0 + 128], in1=BIAS[:, h, kind, :], op=ALU.add),
                                                    reads=[pSn, "BIAS"], writes=["tb_"])
                                                c.op("act", lambda pt=pt, o0=o0: nc.scalar.activation(out=pt[:, o0:o0 + 128], in_=tb_[:], func=AF.Exp),
                                                     reads=["tb_"], writes=[ptn])
                                                tcur += 1
                                            else:
                                                o1 = (4 * g + 4 - t_lo) * 128
                                                c.op("act", lambda pt=pt, pS=pS, o0=o0, o1=o1: nc.scalar.activation(
                                                    out=pt[:, o0:o1], in_=pS[:, o0:o1], func=AF.Exp, bias=CB[:, h:h + 1]),
                                                    reads=[pSn, "CB"], writes=[ptn])
                                                tcur = 4 * g + 4
                                        pts[kt] = (pt, ptn, t_lo)
                                    return pts

                                def mb_stage_b(g, j, pts):
                                    for half in range(2):
                                        tq_ = [t for t in (4 * g + 2 * half, 4 * g + 2 * half + 1) if t >= 2 * j]
                                        if not tq_:
                                            continue
                                        pO, pOn = c.psf()
                                        for ti, t in enumerate(tq_):
                                            kts = [kt for kt in pts if kt <= t]
                                            for ki, kt in enumerate(kts):
                                                pt, ptn, t_lo = pts[kt]
                                                o0 = (t - t_lo) * 128
                                                mm(pO[:, ti * 256:ti * 256 + 129], pt[:, o0:o0 + 128], VB[:, kt, hl, :],
                                                   ki == 0, ki == len(kts) - 1, [ptn, "VB"], [pOn])
                                        for ti, t in enumerate(tq_):
                                            own = (j == t // 2)
                                            sc = 1.0 if own else SEL[:, t, j:j + 1]
                                            c.op("dve", lambda pO=pO, ti=ti, t=t, sc=sc: nc.vector.scalar_tensor_tensor(
                                                out=ACC[:, t, :], in0=pO[:, ti * 256:ti * 256 + 129], scalar=sc, in1=ACC[:, t, :],
                                                op0=ALU.mult, op1=ALU.add), reads=[pOn, "SEL", "ACC"], writes=["ACC"])

                                blocks = [(g, j) for g in range(4) for j in range(2 * g + 2)]
                                nxt = mb_stage_a(*blocks[0])
                                for bi, (g, j) in enumerate(blocks):
                                    cur = nxt
                                    if bi + 1 < len(blocks):
                                        nxt = mb_stage_a(*blocks[bi + 1])
                                    mb_stage_b(g, j, cur)
                                if stop == "mb_att":
                                    c.barrier()
                                    phase_done("mb_att")
                                c.op("dve", lambda: nc.vector.reciprocal(out=REC[:], in_=ACC[:, :, 128]), reads=["ACC"], writes=["REC"])
                                c.op("dve", lambda: nc.vector.tensor_tensor(
                                    out=Yb[:], in0=ACC[:, :, 0:128], in1=REC[:].unsqueeze(2).to_broadcast([128, 16, 128]), op=ALU.mult),
                                    reads=["ACC", "REC"], writes=["Yb"])
                                ytb = YTb[h % 2]
                                ytbn = f"YTb{h % 2}"
                                for cg in range(4):
                                    pb, pbn = transpose_bf(None, lambda jj, cg=cg: Yb[:, cg * 4 + jj, :], 4, ["Yb"], None)
                                    c.op("act", lambda pb=pb, cg=cg, ytb=ytb: nc.scalar.activation(
                                        out=ytb[:, cg * 512:(cg + 1) * 512], in_=pb[:, 0:512], func=AF.Copy), reads=[pbn], writes=[ytbn])
                                c.dma("sp", ybuf[2, h], ytb[:], reads=[ytbn], writes=[f"D:yb2_{h}"], sem=ytbn)
                        c.barrier()
                        phase_done("mb")

                    if dbg and l == 0 and sq == 0:
                        c.dma("sp", dbg_y, ybuf, reads=[f"D:yb{i}_{e}" for i in range(3) for e in range(8)], writes=["D:dbg_y"], sem="k0")

                    with CleanStack() as st:
                        YB = c.sb(st, "YB", [128, 24, 512], BF16)
                        MXT = c.sb(st, "MXT", [128, 16, 512], BF16)
                        XB = c.sb(st, "XB", [128, 16, 512], F32)
                        sqb = c.sb(st, "sqb", [128, 16, 512], BF16)
                        rs = c.sb(st, "rs", [128, 512], F32)
                        sg = [c.sb(st, f"sg{i}", [128, 512], F32) for i in range(3)]
                        pr = [c.sb(st, f"pr{i}", [128, 512], F32) for i in range(2)]
                        ggroups = []
                        bgroups = []
                        ogroups = []
                        for tb in range(4):
                            for dc in range(16):
                                for i in range(3):
                                    ggroups.append(W[l]["g"][i * 16 + dc])
                                    bgroups.append(W[l][f"br{i}"][dc])
                            for dc in range(16):
                                ogroups.append(W[l]["out"][dc])
                        wg_s = WStream(c, st, "wg", [128, 16, 128], 3, ggroups)
                        wb_s = WStream(c, st, "wbr", [128, 8, 128], 3, bgroups)
                        wo_s = WStream(c, st, "wo", [128, 16, 128], 2, ogroups)
                        for tb in range(4):
                            t0 = tb * 512
                            blk = (tok0 + t0) // 512
                            for i in range(3):
                                c.dma("sp", YB[:, i * 8:(i + 1) * 8, :], ybuf[i, :, :, t0:t0 + 512].rearrange("e p t -> p e t"),
                                      reads=[f"D:yb{i}_{e}" for e in range(8)], writes=["YB"], sem=f"YB{i}", par=True)
                            c.dma("sp", XB[:], xTv[:, :, tok0 + t0: tok0 + t0 + 512], reads=[f"D:xT{blk}"], writes=["XB"], sem="XB")
                            for dc in range(16):
                                for i in range(3):
                                    wt, wn = wg_s.next()
                                    pg, pgn = c.psf()
                                    proj_fm(wt, wn, t0, pg, pgn)
                                    c.op("act", lambda pg=pg, i=i: nc.scalar.activation(out=sg[i][:], in_=pg[:], func=AF.Sigmoid),
                                         reads=[pgn], writes=[f"sg{i}"])
                                    wt, wn = wb_s.next()
                                    pbr, pbrn = c.psf()
                                    for k in range(8):
                                        mm(pbr[:], wt[:, k, :], YB[:, i * 8 + k, :], k == 0, k == 7, [wn, "YB"], [pbrn])
                                    if i == 0:
                                        c.op("dve", lambda pbr=pbr: nc.vector.tensor_tensor(out=pr[0][:], in0=pbr[:], in1=sg[0][:], op=ALU.mult),
                                             reads=[pbrn, "sg0"], writes=["pr0"])
                                    else:
                                        c.op("dve", lambda pbr=pbr, i=i: nc.vector.tensor_tensor(out=pr[1][:], in0=pbr[:], in1=sg[i][:], op=ALU.mult),
                                             reads=[pbrn, f"sg{i}"], writes=["pr1"])
                                        if i == 1:
                                            c.op("dve", lambda: nc.vector.tensor_tensor(out=pr[0][:], in0=pr[0][:], in1=pr[1][:], op=ALU.add),
                                                 reads=["pr0", "pr1"], writes=["pr0"])
                                        else:
                                            c.op("dve", lambda dc=dc: nc.vector.tensor_tensor(out=MXT[:, dc, :], in0=pr[0][:], in1=pr[1][:], op=ALU.add),
                                                 reads=["pr0", "pr1"], writes=["MXT"])
                            for dc in range(16):
                                wt, wn = wo_s.next()
                                po, pon = c.psf()
                                for k in range(16):
                                    mm(po[:], wt[:, k, :], MXT[:, k, :], k == 0, k == 15, [wn, "MXT"], [pon])
                                c.op("dve", lambda po=po, dc=dc: nc.vector.tensor_tensor(out=XB[:, dc, :], in0=XB[:, dc, :], in1=po[:], op=ALU.add),
                                     reads=[pon, "XB"], writes=["XB"])
                            c.dma("sp", xTv[:, :, tok0 + t0: tok0 + t0 + 512], XB[:], reads=["XB"], writes=[f"D:xT{blk}"], sem="XBo")
                            norm_block((sqb, rs), XB, "XB", 2 * l + 1, lambda k, t0=t0: A[:, k, t0:t0 + 512], "A")
                        c.barrier()
                        phase_done("c1")

                    if dbg and l == 0 and sq == 0:
                        c.dma("sp", dbg_x1, xT[:, :, 0:S], reads=[f"D:xT{b}" for b in range(4)], writes=["D:dbg_x1"], sem="k0")

                    with CleanStack() as st:
                        UT = c.sb(st, "UT", [128, 64, 512], BF16)
                        sq_ = [c.sb(st, f"sq_{i}", [128, 512], F32) for i in range(2)]
                        xr = [c.sb(st, f"xr{i}", [128, 512], F32) for i in range(2)]
                        g1 = []
                        g2 = []
                        for tb in range(4):
                            for fc in range(64):
                                g1.append(W[l]["ff1"][fc])
                            for dc in range(16):
                                g2.append(W[l]["ff2"][dc])
                        w1_s = WStream(c, st, "w1", [128, 16, 128], 3, g1)
                        w2_s = WStream(c, st, "w2", [128, 64, 128], 3, g2)
                        for tb in range(4):
                            t0 = tb * 512
                            blk = (tok0 + t0) // 512
                            for fc in range(64):
                                wt, wn = w1_s.next()
                                ps, pn = c.psf()
                                proj_fm(wt, wn, t0, ps, pn)
                                s_ = sq_[fc % 2]
                                sn = f"sq_{fc % 2}"
                                c.op("act", lambda ps=ps, s_=s_: nc.scalar.activation(out=s_[:], in_=ps[:], func=AF.Square), reads=[pn], writes=[sn])
                                c.op("dve", lambda ps=ps, s_=s_, fc=fc: nc.vector.scalar_tensor_tensor(
                                    out=UT[:, fc, :], in0=ps[:], scalar=0.0, in1=s_[:], op0=ALU.is_gt, op1=ALU.mult),
                                    reads=[pn, sn], writes=["UT"])
                            for dc in range(16):
                                wt, wn = w2_s.next()
                                b = dc % 2
                                c.dma("sp", xr[b][:], xT[dc, :, tok0 + t0: tok0 + t0 + 512], reads=[f"D:xT{blk}"], writes=[f"xr{b}"], sem=f"xr{b}")
                                ps, pn = c.psf()
                                for k in range(64):
                                    mm(ps[:], wt[:, k, :], UT[:, k, :], k == 0, k == 63, [wn, "UT"], [pn])
                                c.op("dve", lambda ps=ps, b=b: nc.vector.tensor_tensor(out=xr[b][:], in0=xr[b][:], in1=ps[:], op=ALU.add),
                                     reads=[pn, f"xr{b}"], writes=[f"xr{b}"])
                                c.dma("sp", xT[dc, :, tok0 + t0: tok0 + t0 + 512], xr[b][:], reads=[f"xr{b}"], writes=[f"D:xT{blk}"], sem=f"xr{b}", par=True)
                        c.barrier()
                        phase_done("ffn")

                    if dbg and l == 0 and sq == 0:
                        c.dma("sp", dbg_x2, xT[:, :, 0:S], reads=[f"D:xT{b}" for b in range(4)], writes=["D:dbg_x2"], sem="k0")

            with CleanStack() as st:
                XB = c.sb(st, "XB", [128, 16, 512], F32)
                XN = c.sb(st, "XN", [128, 16, 512], F32)
                sqb = c.sb(st, "sqb", [128, 16, 512], BF16)
                rs = c.sb(st, "rs", [128, 512], F32)
                ot = [c.sb(st, f"ot{i}", [128, D], F32) for i in range(2)]
                for blk in range(T // 512):
                    c.dma("sp", XB[:], xTv[:, :, blk * 512:(blk + 1) * 512], reads=[f"D:xT{blk}"], writes=["XB"], sem="XB")
                    norm_block((sqb, rs), XB, "XB", 2 * DEPTH, lambda k: XN[:, k, :], "XN")
                    for tl in range(4):
                        b = tl % 2
                        for q4 in range(4):
                            ps, pn = c.psf()
                            for j in range(4):
                                dcx = q4 * 4 + j
                                c.op("pe", lambda j=j, dcx=dcx, ps=ps, tl=tl: nc.tensor.transpose(
                                    ps[:, j * 128:(j + 1) * 128], XN[:, dcx, tl * 128:(tl + 1) * 128], identf[:]),
                                    reads=["XN", "identf"], writes=[pn], inc=(j == 3))
                            if q4 % 2 == 0:
                                c.op("act", lambda ps=ps, b=b, q4=q4: nc.scalar.activation(out=ot[b][:, q4 * 512:(q4 + 1) * 512], in_=ps[:], func=AF.Copy),
                                     reads=[pn], writes=[f"ot{b}"], par=True)
                            else:
                                c.op("dve", lambda ps=ps, b=b, q4=q4: nc.vector.tensor_copy(out=ot[b][:, q4 * 512:(q4 + 1) * 512], in_=ps[:]),
                                     reads=[pn], writes=[f"ot{b}"], par=True)
                        r0 = blk * 512 + tl * 128
                        c.dma("sp", out[r0:r0 + 128, :], ot[b][:], reads=[f"ot{b}"], writes=[f"D:out{blk}_{tl}"], sem=f"ot{b}")
        try:
            body()
        except _Stop:
            c.barrier()
            if dbg:
                c.dma("sp", dbg_y, ybuf, reads=[f"D:yb{i}_{e}" for i in range(3) for e in range(8)], writes=["D:dbg_y"], sem="k0")
                c.dma("sp", dbg_x1, xT[:, :, 0:S], reads=[f"D:xT{b}" for b in range(4)], writes=["D:dbg_x1"], sem="k1")
        c.finish("sp")
    print("instructions:", c.ninst, "sems:", len(c.sem), flush=True)
    return nc


def _host_inputs(inputs, cst):
    rb = np.asarray(inputs["rel_bias"], np.float32)
    b0 = np.where(cst["_m0"][None], rb[cst["_b0"]].transpose(2, 0, 1), np.float32(NEG))
    b1 = rb[cst["_b1"]].transpose(2, 0, 1)
    mb = np.stack([b0, b1], 1)
    shared = {
        "w_in": np.ascontiguousarray(inputs["w_in"], np.float32),
        "w_branch_ret": np.ascontiguousarray(inputs["w_branch_ret"], np.float32),
        "w_branch_mlstm": np.ascontiguousarray(inputs["w_branch_mlstm"], np.float32),
        "w_branch_moba": np.ascontiguousarray(inputs["w_branch_moba"], np.float32),
        "w_out": np.ascontiguousarray(inputs["w_out"], np.float32),
        "w_ff1": np.ascontiguousarray(inputs["w_ff1"], np.float32),
        "w_ff2": np.ascontiguousarray(inputs["w_ff2"], np.float32),
        "mlstm_gate_b": np.ascontiguousarray(inputs["mlstm_gate_b"], np.float32),
        "conv_wT": np.ascontiguousarray(np.asarray(inputs["mlstm_conv_w"], np.float32).transpose(0, 2, 1)),
        "mlstm_conv_b": np.ascontiguousarray(inputs["mlstm_conv_b"], np.float32),
        "mlstm_norm_w": np.ascontiguousarray(inputs["mlstm_norm_w"], np.float32),
        "nmixT": np.ascontiguousarray(np.asarray(inputs["norm_mix_w"], np.float32).reshape(DEPTH, 16, 128).transpose(0, 2, 1)),
        "nmlpT": np.ascontiguousarray(np.asarray(inputs["norm_mlp_w"], np.float32).reshape(DEPTH, 16, 128).transpose(0, 2, 1)),
        "nfinT": np.ascontiguousarray(np.asarray(inputs["final_norm_w"], np.float32).reshape(16, 128).T),
        "rel_bias": np.ascontiguousarray(rb),
        "mb_bias": np.ascontiguousarray(mb.transpose(2, 0, 1, 3)).astype(np.float32),
    }
    for k, v in cst.items():
        if not k.startswith("_"):
            shared[k] = v
    return shared


def kernel(**inputs):
    cst = _constants()
    shared = _host_inputs(inputs, cst)
    x = np.asarray(inputs["x"], np.float32)
    B = x.shape[0]
    per = B // NCORES
    nc = build_nc(n_seq=per, depth=DEPTH, gl=cst["_gl"])
    in_maps = []
    for i in range(NCORES):
        m = dict(shared)
        m["x"] = np.ascontiguousarray(x[i * per:(i + 1) * per].reshape(per * S, D))
        in_maps.append(m)
    res = run_bass_kernel_spmd(nc, in_maps, core_ids=list(range(NCORES)))
    outs = [np.asarray(r["out"], np.float32).reshape(per, S, D) for r in res.results]
    return np.concatenate(outs, axis=0)
```

```python
import math
from contextlib import ExitStack
import numpy as np
import concourse.bass as bass
import concourse.mybir as mybir
from concourse.bass_utils import run_bass_kernel_spmd

F32 = mybir.dt.float32
BF16 = mybir.dt.bfloat16
AF = mybir.ActivationFunctionType
ALU = mybir.AluOpType
AX = mybir.AxisListType

D = 2048
S = 2048
NCORES = 8
DEPTH = 2
D_IN = 16392
D_FF = 8192
EPS = 1e-6
O_RQ, O_RK, O_RV, O_RG = 0, 1024, 2048, 3072
O_MQ, O_MK, O_MV, O_MO, O_MI, O_MF = 4096, 4608, 5120, 6144, 7168, 7172
O_BQ, O_BK, O_BV, O_G = 7176, 8200, 9224, 10248
NEG = -30000.0


class Ctx:
    def __init__(self, nc):
        self.nc = nc
        self.es = ExitStack()
        self.eng = {"pe": nc.tensor, "act": nc.scalar, "dve": nc.vector, "pool": nc.gpsimd, "sp": nc.sync}
        self.sem = {}
        self.cnt = {}
        for k in self.eng:
            self.sem[k] = self.es.enter_context(nc.semaphore("s_" + k))
            self.cnt[k] = 0
        self.waited = {k: {} for k in self.eng}
        self.res = {}
        self.ninst = {k: 0 for k in self.eng}
        self._psf = []
        self._psb = []
        self._pi = 0
        self._pb = 0

    def sb(self, st, name, shape, dt):
        self._uid = getattr(self, "_uid", 0) + 1
        return st.enter_context(self.nc.sbuf_tensor(f"{name}_u{self._uid}", list(shape), dt))

    def dsem(self, key):
        if key not in self.sem:
            self.sem[key] = self.es.enter_context(self.nc.semaphore("d_" + key))
            self.cnt[key] = 0
            assert len(self.sem) <= 100, "too many semaphores"
        return key

    def _r(self, name):
        r = self.res.get(name)
        if r is None:
            r = {"w": {}, "r": {}}
            self.res[name] = r
        return r

    def _wait(self, e, deps):
        m = {}
        for d in deps:
            if d is None:
                continue
            k, v = d
            if v > m.get(k, 0):
                m[k] = v
        for k, v in m.items():
            if k == "pe" and e == "pe":
                continue
            if self.waited[e].get(k, 0) >= v:
                continue
            self.eng[e].wait_ge(self.sem[k], v)
            self.waited[e][k] = v

    def _deps(self, reads, writes, par=False, e=None):
        deps = []
        for r in reads:
            deps.extend(self._r(r)["w"].items())
            if r.startswith("ps"):
                deps.extend((k, v) for k, v in self._r(r)["r"].items() if k != e)
        for w in writes:
            rr = self._r(w)
            if not par:
                deps.extend(rr["w"].items())
            deps.extend(rr["r"].items())
        return deps

    def _commit(self, ticket, reads, writes, par=False):
        k, v = ticket
        for r in reads:
            rr = self._r(r)
            if rr["r"].get(k, 0) < v:
                rr["r"][k] = v
        for w in writes:
            rr = self._r(w)
            if par:
                if rr["w"].get(k, 0) < v:
                    rr["w"][k] = v
            else:
                rr["w"] = {k: v}
                rr["r"] = {}

    def op(self, e, fn, reads=(), writes=(), inc=True, par=False):
        self._wait(e, self._deps(reads, writes, par, e))
        ins = fn()
        self.ninst[e] += 1
        if inc:
            ins.then_inc(self.sem[e], 1)
            self.cnt[e] += 1
            ticket = (e, self.cnt[e])
        else:
            assert e == "pe"
            ticket = (e, self.cnt[e] + 1)
        self._commit(ticket, reads, writes, par)
        return ticket

    def dma(self, q, out, in_, reads=(), writes=(), sem=None, par=False, **kw):
        key = self.dsem(sem)
        deps = self._deps(reads, writes, par)
        if self.cnt[key] > 0:
            deps.append((key, self.cnt[key]))
        self._wait(q, deps)
        ins = self.eng[q].dma_start(out=out, in_=in_, **kw)
        ins.then_inc(self.sem[key], 16)
        self.ninst[q] += 1
        self.cnt[key] += 16
        ticket = (key, self.cnt[key])
        self._commit(ticket, reads, writes, par)
        return ticket

    def barrier(self, engines=("pe", "act", "dve", "sp")):
        deps = []
        for rr in self.res.values():
            deps.extend(rr["w"].items())
            deps.extend(rr["r"].items())
        deps = [d for d in deps if d is not None and not str(d[0]).startswith("cast")]
        for e in engines:
            self._wait(e, deps)
        for n in list(self.res.keys()):
            if not n.startswith("D:"):
                del self.res[n]

    def finish(self, e="sp"):
        deps = []
        for rr in self.res.values():
            deps.extend(rr["w"].items())
            deps.extend(rr["r"].items())
        self._wait(e, deps)

    def psf(self):
        while True:
            i = self._pi % len(self._psf)
            self._pi += 1
            if i not in getattr(self, "reserved", ()):
                return self._psf[i], f"psf{i}"

    def psf_at(self, i):
        return self._psf[i], f"psf{i}"

    def psb(self):
        i = self._pb % len(self._psb)
        self._pb += 1
        return self._psb[i], f"psb{i}"


class WStream:
    def __init__(self, c, st, name, shape, nbuf, groups):
        self.c = c
        self.name = name
        self.nbuf = nbuf
        self.groups = groups
        self.tiles = [c.sb(st, f"{name}{i}", shape, BF16) for i in range(nbuf)]
        self.il = 0
        self.iu = 0

    def _load(self):
        ap, deps = self.groups[self.il]
        slot = self.il % self.nbuf
        self.c.dma("sp", self.tiles[slot][:], ap, reads=deps, writes=[f"{self.name}{slot}"], sem=f"{self.name}{slot}")
        self.il += 1

    def next(self, ahead=None):
        if ahead is None:
            ahead = self.nbuf - 1
        while self.il < min(self.iu + 1 + ahead, len(self.groups)):
            self._load()
        slot = self.iu % self.nbuf
        self.iu += 1
        return self.tiles[slot], f"{self.name}{slot}"


def _t5_bucket(dist):
    n = np.maximum(dist, 0)
    exact = 16
    nf = np.maximum(n, 1).astype(np.float32)
    large = exact + (np.log(nf / np.float32(exact)) / np.float32(math.log(128 / exact)) * np.float32(16)).astype(np.int32)
    large = np.minimum(large, 31)
    return np.where(n < exact, n, large)


def _constants():
    cst = {}
    half = 64
    inv = (np.float32(10000.0) ** (-np.arange(half, dtype=np.float32) / np.float32(half))).astype(np.float32)
    pos = np.arange(S, dtype=np.float32)
    ang = (pos[:, None] * inv[None, :]).astype(np.float32)
    cos = np.cos(ang).astype(np.float32).T
    sin = np.sin(ang).astype(np.float32).T
    cst["c_cos"] = np.ascontiguousarray(np.concatenate([cos, cos], 0))
    cst["c_sin"] = np.ascontiguousarray(np.concatenate([-sin, sin], 0))
    H = 8
    L = 128
    lg = np.log1p(-np.exp2(-5.0 - np.arange(H, dtype=np.float64)))
    idx = np.arange(L, dtype=np.float64)
    scale = 128 ** -0.5
    diff = idx[None, :] - idx[:, None]
    dt = np.where(diff[None] >= 0, np.exp(np.maximum(diff[None], 0) * lg[:, None, None]), 0.0) * scale
    cst["c_dt"] = np.ascontiguousarray(dt.transpose(1, 0, 2)).astype(np.float32)
    xi = np.exp((idx + 1.0)[None, :] * lg[:, None]) * scale
    cst["c_xi"] = np.ascontiguousarray(np.broadcast_to(xi[None], (128, H, L))).astype(np.float32)
    zeta = np.exp((L - 1 - idx)[None, :] * lg[:, None])
    cst["c_zeta"] = np.ascontiguousarray(zeta.T).astype(np.float32)
    gl = np.exp(L * lg)
    cst["_gl"] = [float(np.float32(v)) for v in gl]
    g16 = np.zeros((128, H, 16), np.float32)
    g16[:, :, 1:] = gl.astype(np.float32)[None, :, None]
    cst["c_g16"] = g16
    cst["c_mask"] = (idx[:, None] <= idx[None, :]).astype(np.float32)
    cst["c_ident"] = np.eye(128, dtype=np.float32)
    pm = np.zeros((128, 16, 8), np.float32)
    pi = np.zeros((128, 16, 8), np.float32)
    for t in range(16):
        own = t // 2
        for j in range(8):
            if j < own:
                pi[:, t, j] = 1.0
            else:
                pm[:, t, j] = -1e30
    cst["c_pm"] = pm
    cst["c_pi"] = pi
    k = np.arange(128)[:, None]
    q = np.arange(128)[None, :]
    cst["_b0"] = _t5_bucket(q - k)
    cst["_m0"] = (k <= q)
    cst["_b1"] = _t5_bucket(q - k + 128)
    return cst


class _Stop(Exception):
    pass


class _SkipPhase(Exception):
    pass


class CleanStack(ExitStack):
    def __exit__(self, *exc):
        super().__exit__(None, None, None)
        return bool(exc and exc[0] is _SkipPhase)


def build_nc(n_seq=2, depth=DEPTH, dbg=False, gl=None, stop=None, skip=()):
    T = n_seq * S
    nc = bass.Bass("TRN2", target_bir_lowering=False)

    def din(name, shape, dt=F32):
        return nc.dram_tensor(name, list(shape), dt, kind="ExternalInput").ap()

    x_in = din("x", [T, D])
    w_in = din("w_in", [DEPTH, D, D_IN])
    w_br = [din("w_branch_ret", [DEPTH, 1024, D]), din("w_branch_mlstm", [DEPTH, 1024, D]),
            din("w_branch_moba", [DEPTH, 1024, D])]
    w_out = din("w_out", [DEPTH, D, D])
    w_ff1 = din("w_ff1", [DEPTH, D, D_FF])
    w_ff2 = din("w_ff2", [DEPTH, D_FF, D])
    gate_b = din("mlstm_gate_b", [DEPTH, 2, 4])
    conv_wT = din("conv_wT", [DEPTH, 1024, 4])
    conv_b = din("mlstm_conv_b", [DEPTH, 1024])
    ml_nw = din("mlstm_norm_w", [DEPTH, 1024])
    nmix = din("nmixT", [DEPTH, 128, 16])
    nmlp = din("nmlpT", [DEPTH, 128, 16])
    nfin = din("nfinT", [128, 16])
    rel_bias = din("rel_bias", [32, 8])
    mb_bias = din("mb_bias", [128, 8, 2, 128])
    c_cos = din("c_cos", [128, S])
    c_sin = din("c_sin", [128, S])
    c_dt = din("c_dt", [128, 8, 128])
    c_xi = din("c_xi", [128, 8, 128])
    c_zeta = din("c_zeta", [128, 8])
    c_g16 = din("c_g16", [128, 8, 16])
    c_mask = din("c_mask", [128, 128])
    c_ident = din("c_ident", [128, 128])
    c_pm = din("c_pm", [128, 16, 8])
    c_pi = din("c_pi", [128, 16, 8])
    out = nc.dram_tensor("out", [T, D], F32, kind="ExternalOutput").ap()

    def dscr(name, shape, dt):
        return nc.dram_tensor(name, list(shape), dt, kind="Internal").ap()

    xT = dscr("xT", [16, 128, T], F32)
    ybuf = dscr("ybuf", [3, 8, 128, S], BF16)
    gsc = dscr("gsc", [2, 4, S], F32)
    gs2 = dscr("gs2", [64, 2], F32)
    gs3 = dscr("gs3", [4, 16, 5], F32)
    gs4 = dscr("gs4", [2, 64, 128], F32)
    if dbg:
        dbg_y = nc.dram_tensor("dbg_y", [3, 8, 128, S], BF16, kind="ExternalOutput").ap()
        dbg_x1 = nc.dram_tensor("dbg_x1", [16, 128, S], F32, kind="ExternalOutput").ap()
        dbg_x2 = nc.dram_tensor("dbg_x2", [16, 128, S], F32, kind="ExternalOutput").ap()

    c = Ctx(nc)
    ncast = [0]
    cast_q = [[]]

    def flush_casts(wait_names=()):
        deps = []
        for n in wait_names:
            deps.extend(c._r(n)["w"].items())
        if deps:
            c._wait("pool", deps)
        for q in cast_q:
            for f in q:
                f()
        cast_q.clear()
        cast_q.append([])

    def cast_family(name, src2d, K, ncols, gw, col0=0):
        kc = K // 128
        G = ncols // gw
        dst = dscr("wb_" + name, [G, 128, kc, gw], BF16)
        srcv = src2d.rearrange("(kc p) n -> p kc n", p=128)
        groups = []
        for g in range(G):
            deps = []
            for k0 in range(0, kc, 16):
                k1 = min(kc, k0 + 16)
                rn = f"D:wb_{name}_{g}_{k0}"
                def emit(g=g, k0=k0, k1=k1, rn=rn):
                    c.dma("pool", dst[g][:, k0:k1, :], srcv[:, k0:k1, col0 + g * gw: col0 + (g + 1) * gw],
                          writes=[rn], sem=f"cast{ncast[0] % 16}")
                    ncast[0] += 1
                cast_q[-1].append(emit)
                deps.append(rn)
            groups.append((dst[g], deps))
        return groups

    with c.es:
        gst = c.es
        for i in range(6):
            c._psf.append(gst.enter_context(nc.psum_tensor(f"psf{i}", [128, 512], F32)))
        for i in range(2):
            c._psb.append(gst.enter_context(nc.psum_tensor(f"psb{i}", [128, 1024], BF16)))
        A = c.sb(gst, "A", [128, 16, S], BF16)
        identf = c.sb(gst, "identf", [128, 128], F32)
        identb = c.sb(gst, "identb", [128, 128], BF16)
        onesb = c.sb(gst, "onesb", [128, 128], BF16)
        nw_all = c.sb(gst, "nw_all", [128, 2 * DEPTH + 1, 16], F32)

        c.dma("sp", identf[:], c_ident, writes=["identf"], sem="k0")
        c.op("dve", lambda: nc.vector.tensor_copy(out=identb[:], in_=identf[:]), reads=["identf"], writes=["identb"])
        c.op("dve", lambda: nc.vector.memset(onesb[:], 1.0), writes=["onesb"])
        for l in range(DEPTH):
            c.dma("sp", nw_all[:, 2 * l, :], nmix[l], writes=["nw_all"], sem="k0")
            c.dma("sp", nw_all[:, 2 * l + 1, :], nmlp[l], writes=["nw_all"], sem="k0")
        c.dma("sp", nw_all[:, 2 * DEPTH, :], nfin, writes=["nw_all"], sem="k0")

        W = []
        for l in range(depth):
            wl = {}
            wi = w_in[l]
            wl["rv"] = cast_family(f"rv{l}", wi, D, 1024, 512, O_RV)
            wl["rg"] = cast_family(f"rg{l}", wi, D, 1024, 512, O_RG)
            wl["rq"] = cast_family(f"rq{l}", wi, D, 1024, 128, O_RQ)
            wl["rk"] = cast_family(f"rk{l}", wi, D, 1024, 128, O_RK)
            if l == 0:
                flush_casts()
            wl["mi"] = cast_family(f"mi{l}", wi, D, 4, 4, O_MI)
            wl["mf"] = cast_family(f"mf{l}", wi, D, 4, 4, O_MF)
            wl["mv"] = cast_family(f"mv{l}", wi, D, 1024, 512, O_MV)
            wl["mo"] = cast_family(f"mo{l}", wi, D, 1024, 512, O_MO)
            wl["mq"] = cast_family(f"mq{l}", wi, D, 512, 128, O_MQ)
            wl["mk"] = cast_family(f"mk{l}", wi, D, 512, 128, O_MK)
            wl["bv"] = cast_family(f"bv{l}", wi, D, 1024, 512, O_BV)
            wl["bq"] = cast_family(f"bq{l}", wi, D, 1024, 128, O_BQ)
            wl["bk"] = cast_family(f"bk{l}", wi, D, 1024, 128, O_BK)
            wl["g"] = cast_family(f"g{l}", wi, D, 6144, 128, O_G)
            for i in range(3):
                wl[f"br{i}"] = cast_family(f"br{i}_{l}", w_br[i][l], 1024, D, 128)
            wl["out"] = cast_family(f"out{l}", w_out[l], D, D, 128)
            wl["ff1"] = cast_family(f"ff1_{l}", w_ff1[l], D, D_FF, 128)
            wl["ff2"] = cast_family(f"ff2_{l}", w_ff2[l], D_FF, D, 128)
            W.append(wl)
            if l == 0:
                cast_q.append([])

        def mm(o, lhsT, rhs, start, stop, reads, writes):
            return c.op("pe", lambda: nc.tensor.matmul(o, lhsT, rhs, start=start, stop=stop),
                        reads=reads, writes=writes, inc=bool(stop))

        def proj_fm(wt, wn, t0, ps, pn, kc=16, src=None, srcn="A", m=128):
            srcT = A if src is None else src
            for k in range(kc):
                mm(ps[0:m, :], wt[:, k, 0:m], srcT[:, k, t0:t0 + 512], k == 0, k == kc - 1, [wn, srcn], [pn])

        def transpose_bf(dst_fn, src_fn, n, reads, dst_reads_writes):
            pb, pbn = c.psb()
            for j in range(n):
                c.op("pe", lambda j=j: nc.tensor.transpose(pb[:, j * 128:(j + 1) * 128], src_fn(j), identb[:]),
                     reads=list(reads) + ["identb"], writes=[pbn], inc=(j == n - 1))
            return pb, pbn

        def norm_block(st_tiles, xblk, xn, nwi, dst, dstn):
            sqb, rs = st_tiles
            c.op("act", lambda: nc.scalar.activation(out=sqb[:], in_=xblk[:], func=AF.Square), reads=[xn], writes=["sqb"])
            ps, pn = c.psf()
            for k in range(16):
                mm(ps[:], onesb[:], sqb[:, k, :], k == 0, k == 15, ["onesb", "sqb"], [pn])
            c.op("act", lambda: nc.scalar.activation(out=rs[:], in_=ps[:], func=AF.Sqrt, scale=1.0 / D, bias=EPS),
                 reads=[pn], writes=["rs"])
            c.op("dve", lambda: nc.vector.reciprocal(out=rs[:], in_=rs[:]), reads=["rs"], writes=["rs"])
            for k in range(16):
                c.op("dve", lambda k=k: nc.vector.scalar_tensor_tensor(
                    out=dst(k), in0=xblk[:, k, :], scalar=nw_all[:, nwi, k:k + 1], in1=rs[:],
                    op0=ALU.mult, op1=ALU.mult), reads=[xn, "rs", "nw_all"], writes=[dstn], par=True)

        xTv = xT.rearrange("dc p t -> p dc t")

        with CleanStack() as st:
            xt = [c.sb(st, f"xt{i}", [128, D], F32) for i in range(2)]
            xs = [c.sb(st, f"xs{i}", [128, 16, 128], F32) for i in range(2)]
            for tt in range(T // 128):
                b = tt % 2
                c.dma("sp", xt[b][:], x_in[tt * 128:(tt + 1) * 128, :], writes=[f"xt{b}"], sem=f"xt{b}")
                for q4 in range(4):
                    ps, pn = c.psf()
                    for j in range(4):
                        dcx = q4 * 4 + j
                        c.op("pe", lambda j=j, dcx=dcx, ps=ps: nc.tensor.transpose(
                            ps[:, j * 128:(j + 1) * 128], xt[b][:, dcx * 128:(dcx + 1) * 128], identf[:]),
                            reads=[f"xt{b}", "identf"], writes=[pn], inc=(j == 3))
                    eng = "act" if q4 % 2 == 0 else "dve"
                    dstap = xs[b][:, q4 * 4:(q4 + 1) * 4, :]
                    if eng == "act":
                        c.op("act", lambda ps=ps, dstap=dstap: nc.scalar.activation(
                            out=dstap, in_=ps[:].rearrange("p (j k) -> p j k", j=4), func=AF.Copy),
                            reads=[pn], writes=[f"xs{b}"], par=True)
                    else:
                        c.op("dve", lambda ps=ps, dstap=dstap: nc.vector.tensor_copy(
                            out=dstap, in_=ps[:].rearrange("p (j k) -> p j k", j=4)),
                            reads=[pn], writes=[f"xs{b}"], par=True)
                blk = tt // 4
                c.dma("sp", xTv[:, :, tt * 128:(tt + 1) * 128], xs[b][:], reads=[f"xs{b}"], writes=[f"D:xT{blk}"],
                      sem=f"xs{b}", par=True)
            c.barrier()

        gl = gl or _constants()["_gl"]

        def phase_done(name):
            if stop == name:
                raise _Stop()

        later = cast_q[1:] if len(cast_q) > 1 else []
        del cast_q[1:]
        flush_casts([f"D:xT{b}" for b in range(T // 512)])
        for q in later:
            cast_q[-1].extend(q)

        def body():
            phase_done("p0")
            for l in range(depth):
                for sq in range(n_seq):
                    tok0 = sq * S
                    with CleanStack() as st:
                        xb = [c.sb(st, f"xb{i}", [128, 16, 512], F32) for i in range(2)]
                        sqb = c.sb(st, "sqb", [128, 16, 512], BF16)
                        rs = c.sb(st, "rs", [128, 512], F32)
                        for tb in range(4):
                            b = tb % 2
                            blk = (tok0 + tb * 512) // 512
                            c.dma("sp", xb[b][:], xTv[:, :, tok0 + tb * 512: tok0 + (tb + 1) * 512],
                                  reads=[f"D:xT{blk}"], writes=[f"xb{b}"], sem=f"xb{b}")
                            norm_block((sqb, rs), xb[b], f"xb{b}", 2 * l,
                                       lambda k, tb=tb: A[:, k, tb * 512:(tb + 1) * 512], "A")
                        c.barrier()
                        phase_done("n1")

                    with CleanStack() as st:
                        if "ret" in skip:
                            raise _SkipPhase()
                        COS = c.sb(st, "COS", [128, S], F32)
                        SIN = c.sb(st, "SIN", [128, S], F32)
                        DT = c.sb(st, "DT", [128, 8, 128], F32)
                        XI = c.sb(st, "XI", [128, 8, 128], F32)
                        ZETA = c.sb(st, "ZETA", [128, 8], F32)
                        c.dma("sp", COS[:], c_cos, writes=["COS"], sem="k0")
                        c.dma("sp", SIN[:], c_sin, writes=["SIN"], sem="k1")
                        c.dma("sp", DT[:], c_dt, writes=["DT"], sem="k2")
                        c.dma("sp", XI[:], c_xi, writes=["XI"], sem="k3")
                        c.dma("sp", ZETA[:], c_zeta, writes=["ZETA"], sem="k4")
                        QT = c.sb(st, "QT", [128, S], BF16)
                        KT = c.sb(st, "KT", [128, S], BF16)
                        QX = c.sb(st, "QX", [128, S], BF16)
                        KZ = c.sb(st, "KZ", [128, 16, 128], BF16)
                        VV = c.sb(st, "VV", [128, 16, 512], BF16)
                        SG = c.sb(st, "SG", [128, 16, 512], BF16)
                        t1 = c.sb(st, "t1", [128, 512], F32)
                        t2 = c.sb(st, "t2", [128, 512], F32)
                        G16 = c.sb(st, "G16", [128, 8, 16], F32)
                        c.dma("sp", G16[:], c_g16, writes=["G16"], sem="k5")
                        KVs = c.sb(st, "KVs", [128, 128, 16], F32)
                        GMUL = c.sb(st, "GMUL", [128, 128, 16], F32)
                        Rbn = c.sb(st, "Rbn", [128, 16, 128], BF16)
                        c.op("dve", lambda: nc.vector.memset(KVs[:, :, 0:1], 0.0), writes=["KVs"])
                        PT = [c.sb(st, f"PT{i}", [128, 4, 128], BF16) for i in range(2)]
                        junk = c.sb(st, "junk", [128, 512], F32)
                        ss4 = c.sb(st, "ss4", [128, 4], F32)
                        yn = c.sb(st, "yn", [128, 4, 128], F32)
                        Yst = [c.sb(st, f"Yst{i}", [128, 4, 128], BF16) for i in range(2)]
                        YT = [c.sb(st, f"YT{i}", [128, S], BF16) for i in range(2)]
                        wtm = WStream(c, st, "wtm", [128, 16, 512], 1,
                                      [W[l]["rv"][0], W[l]["rg"][0], W[l]["rv"][1], W[l]["rg"][1]])
                        wfm = WStream(c, st, "wfm", [128, 16, 128], 3,
                                      [W[l][k][h] for h in range(8) for k in ("rq", "rk")])
                        ret_pend = []

                        def ret_epilogue(cg, pO, pOn, h, hc, yt, ytn):
                            c.op("act", lambda pO=pO: nc.scalar.activation(out=junk[:], in_=pO[:], func=AF.Square),
                                 reads=[pOn], writes=["junk"])
                            c.op("dve", lambda: nc.vector.reduce_sum(out=ss4[:], in_=junk[:].rearrange("p (j k) -> p j k", j=4), axis=AX.X),
                                 reads=["junk"], writes=["ss4"])
                            c.op("act", lambda: nc.scalar.activation(out=ss4[:], in_=ss4[:], func=AF.Sqrt, scale=1.0 / 128, bias=EPS),
                                 reads=["ss4"], writes=["ss4"])
                            c.op("dve", lambda: nc.vector.reciprocal(out=ss4[:], in_=ss4[:]), reads=["ss4"], writes=["ss4"])
                            c.op("dve", lambda pO=pO: nc.vector.tensor_tensor(
                                out=yn[:], in0=pO[:].rearrange("p (j k) -> p j k", j=4),
                                in1=ss4[:].unsqueeze(2).to_broadcast([128, 4, 128]), op=ALU.mult),
                                reads=[pOn, "ss4"], writes=["yn"])
                            ys = Yst[cg % 2]
                            ysn = f"Yst{cg % 2}"
                            c.op("dve", lambda ys=ys, cg=cg: nc.vector.tensor_tensor(
                                out=ys[:], in0=yn[:], in1=SG[:, cg * 4:(cg + 1) * 4, hc], op=ALU.mult),
                                reads=["yn", "SG"], writes=[ysn])
                            pb, pbn = transpose_bf(None, lambda j, ys=ys: ys[:, j, :], 4, [ysn], None)
                            c.op("act", lambda pb=pb, cg=cg, yt=yt: nc.scalar.activation(
                                out=yt[:, cg * 512:(cg + 1) * 512], in_=pb[:, 0:512], func=AF.Copy),
                                reads=[pbn], writes=[ytn])
                            if cg == 3:
                                c.dma("sp", ybuf[0, h], yt[:], reads=[ytn], writes=[f"D:yb0_{h}"], sem=ytn)

                        c.reserved = {2, 3}
                        for hg in range(2):
                            if ret_pend:
                                ret_pend.pop()()
                            wv, wvn = wtm.next(ahead=0)
                            for tl in range(16):
                                ps, pn = c.psf()
                                for k in range(16):
                                    mm(ps[:], A[:, k, tl * 128:(tl + 1) * 128], wv[:, k, :], k == 0, k == 15, ["A", wvn], [pn])
                                c.op("act", lambda ps=ps, tl=tl: nc.scalar.activation(out=VV[:, tl, :], in_=ps[:], func=AF.Copy),
                                     reads=[pn], writes=["VV"])
                            wg, wgn = wtm.next(ahead=0)
                            for tl in range(16):
                                ps, pn = c.psf()
                                for k in range(16):
                                    mm(ps[:], A[:, k, tl * 128:(tl + 1) * 128], wg[:, k, :], k == 0, k == 15, ["A", wgn], [pn])
                                c.op("act", lambda ps=ps, tl=tl: nc.scalar.activation(out=SG[:, tl, :], in_=ps[:], func=AF.Silu),
                                     reads=[pn], writes=["SG"])
                            for hl in range(4):
                                h = hg * 4 + hl
                                hc = slice(hl * 128, (hl + 1) * 128)
                                for dstT, dn in ((QT, "QT"), (KT, "KT")):
                                    wt, wn = wfm.next()
                                    for tb in range(4):
                                        ts_ = slice(tb * 512, (tb + 1) * 512)
                                        ps, pn = c.psf()
                                        proj_fm(wt, wn, tb * 512, ps, pn)
                                        c.op("dve", lambda ps=ps, ts_=ts_: nc.vector.tensor_tensor(
                                            out=t1[0:64, :], in0=ps[64:128, :], in1=SIN[0:64, ts_], op=ALU.mult),
                                            reads=[pn, "SIN"], writes=["t1"])
                                        c.op("dve", lambda ps=ps, ts_=ts_: nc.vector.tensor_tensor(
                                            out=t1[64:128, :], in0=ps[0:64, :], in1=SIN[64:128, ts_], op=ALU.mult),
                                            reads=[pn, "SIN"], writes=["t1"])
                                        c.op("dve", lambda ps=ps, ts_=ts_: nc.vector.tensor_tensor(
                                            out=t2[:], in0=ps[:], in1=COS[:, ts_], op=ALU.mult),
                                            reads=[pn, "COS"], writes=["t2"])
                                        c.op("dve", lambda dstT=dstT, ts_=ts_: nc.vector.tensor_tensor(
                                            out=dstT[:, ts_], in0=t1[:], in1=t2[:], op=ALU.add),
                                            reads=["t1", "t2"], writes=[dn])
                                c.op("dve", lambda h=h: nc.vector.tensor_tensor(
                                    out=QX[:].rearrange("p (n l) -> p n l", n=16), in0=QT[:].rearrange("p (n l) -> p n l", n=16),
                                    in1=XI[:, h:h + 1, :].to_broadcast([128, 16, 128]), op=ALU.mult),
                                    reads=["QT", "XI"], writes=["QX"])
                                for cg in range(4):
                                    pb, pbn = transpose_bf(None, lambda j, cg=cg: KT[:, (cg * 4 + j) * 128:(cg * 4 + j + 1) * 128], 4, ["KT"], None)
                                    c.op("act", lambda pb=pb, cg=cg, h=h: nc.scalar.activation(
                                        out=KZ[:, cg * 4:(cg + 1) * 4, :], in_=pb[:, 0:512].rearrange("p (j k) -> p j k", j=4),
                                        func=AF.Copy, scale=ZETA[:, h:h + 1]), reads=[pbn, "ZETA"], writes=["KZ"])
                                for cgk in range(4):
                                    nk = 4 if cgk < 3 else 3
                                    pK, pKn = c.psf()
                                    for j in range(nk):
                                        n = cgk * 4 + j
                                        c.op("pe", lambda j=j, n=n, pK=pK: nc.tensor.matmul(
                                            pK[:, j * 128:(j + 1) * 128], KZ[:, n, :], VV[:, n, hc], start=True, stop=True),
                                            reads=["KZ", "VV"], writes=[pKn], inc=(j == nk - 1))
                                    c.op("act", lambda pK=pK, cgk=cgk, nk=nk: nc.scalar.activation(
                                        out=KVs[:, :, cgk * 4 + 1: cgk * 4 + 1 + nk],
                                        in_=pK[:, 0:nk * 128].rearrange("p (j e) -> p e j", j=nk), func=AF.Copy),
                                        reads=[pKn], writes=["KVs"])
                                c.op("dve", lambda h=h: nc.vector.tensor_copy(
                                    out=GMUL[:], in_=G16[:, h:h + 1, :].to_broadcast([128, 128, 16])), reads=["G16"], writes=["GMUL"])
                                c.op("dve", lambda: nc.vector.tensor_tensor_scan(
                                    out=KVs[:].rearrange("p e n -> p (e n)"), data0=GMUL[:].rearrange("p e n -> p (e n)"),
                                    data1=KVs[:].rearrange("p e n -> p (e n)"),
                                    initial=0.0, op0=ALU.mult, op1=ALU.add), reads=["KVs", "GMUL"], writes=["KVs"])
                                c.op("dve", lambda: nc.vector.tensor_copy(out=Rbn[:], in_=KVs[:].rearrange("p e n -> p n e")),
                                     reads=["KVs"], writes=["Rbn"])
                                yt = YT[h % 2]
                                ytn = f"YT{h % 2}"
                                c.reserved = {2, 3}
                                for cg in range(4):
                                    pS, pSn = c.psf()
                                    for j in range(4):
                                        n = cg * 4 + j
                                        cs = slice(n * 128, (n + 1) * 128)
                                        c.op("pe", lambda j=j, cs=cs, pS=pS: nc.tensor.matmul(
                                            pS[:, j * 128:(j + 1) * 128], KT[:, cs], QT[:, cs], start=True, stop=True),
                                            reads=["KT", "QT"], writes=[pSn], inc=(j == 3))
                                    pt = PT[cg % 2]
                                    ptn = f"PT{cg % 2}"
                                    c.op("dve", lambda pS=pS, pt=pt, h=h: nc.vector.tensor_tensor(
                                        out=pt[:], in0=pS[:].rearrange("p (j k) -> p j k", j=4),
                                        in1=DT[:, h:h + 1, :].to_broadcast([128, 4, 128]), op=ALU.mult),
                                        reads=[pSn, "DT"], writes=[ptn])
                                    pO, pOn = c.psf_at(2 + cg % 2)
                                    for j in range(4):
                                        n = cg * 4 + j
                                        cs = slice(n * 128, (n + 1) * 128)
                                        mm(pO[:, j * 128:(j + 1) * 128], pt[:, j, :], VV[:, n, hc], True, n == 0, [ptn, "VV"], [pOn])
                                        if n > 0:
                                            mm(pO[:, j * 128:(j + 1) * 128], QX[:, cs], Rbn[:, n, :], False, True, ["QX", "Rbn"], [pOn])
                                    if ret_pend:
                                        ret_pend.pop()()
                                    ret_pend.append(lambda cg=cg, pO=pO, pOn=pOn, h=h, hc=hc, yt=yt, ytn=ytn: ret_epilogue(cg, pO, pOn, h, hc, yt, ytn))
                        if ret_pend:
                            ret_pend.pop()()
                        c.reserved = set()
                        c.barrier()
                        phase_done("ret")

                    with CleanStack() as st:
                        if "ml" in skip:
                            raise _SkipPhase()
                        MASK = c.sb(st, "MASK", [128, 128], F32)
                        c.dma("sp", MASK[:], c_mask, writes=["MASK"], sem="k0")
                        gb = c.sb(st, "gb", [4, 2], F32)
                        c.dma("sp", gb[:, 0:1], gate_b[l, 0, :].rearrange("(h o) -> h o", o=1), writes=["gb"], sem="k1")
                        c.dma("sp", gb[:, 1:2], gate_b[l, 1, :].rearrange("(h o) -> h o", o=1), writes=["gb"], sem="k1")
                        c.op("dve", lambda: nc.vector.tensor_scalar(out=gb[:, 1:2], in0=gb[:, 1:2], scalar1=-1.0, scalar2=None, op0=ALU.mult),
                             reads=["gb"], writes=["gb"])
                        NWB = c.sb(st, "NWB", [128, 1024], F32)
                        c.dma("sp", NWB[:], ml_nw[l:l + 1, :].partition_broadcast(128), writes=["NWB"], sem="k2")
                        EMT = c.sb(st, "EMT", [128, 64], F32)
                        SCb = c.sb(st, "SCb", [128, 64, 5], F32)
                        stg = ExitStack()
                        st_outer = st
                        st = stg
                        wif = [c.sb(st, f"wif{i}", [128, 16, 4], BF16) for i in range(2)]
                        c.dma("sp", wif[0][:], W[l]["mi"][0][0], reads=W[l]["mi"][0][1], writes=["wif0"], sem="k3")
                        c.dma("sp", wif[1][:], W[l]["mf"][0][0], reads=W[l]["mf"][0][1], writes=["wif1"], sem="k4")
                        LI = c.sb(st, "LI", [4, S], F32)
                        LF = c.sb(st, "LF", [4, S], F32)
                        for tb in range(4):
                            ts_ = slice(tb * 512, (tb + 1) * 512)
                            ps, pn = c.psf()
                            proj_fm(wif[0], "wif0", tb * 512, ps, pn, m=4)
                            c.op("act", lambda ps=ps, ts_=ts_: nc.scalar.activation(out=LI[:, ts_], in_=ps[0:4, :], func=AF.Identity, bias=gb[:, 0:1]),
                                 reads=[pn, "gb"], writes=["LI"])
                            ps, pn = c.psf()
                            proj_fm(wif[1], "wif1", tb * 512, ps, pn, m=4)
                            c.op("act", lambda ps=ps, ts_=ts_: nc.scalar.activation(out=LF[:, ts_], in_=ps[0:4, :], func=AF.Exp, scale=-1.0, bias=gb[:, 1:2]),
                                 reads=[pn, "gb"], writes=["LF"])
                        c.op("act", lambda: nc.scalar.activation(out=LF[:], in_=LF[:], func=AF.Ln, bias=1.0), reads=["LF"], writes=["LF"])
                        c.dma("sp", gsc[0], LI[:], reads=["LI"], writes=["D:gsc0"], sem="k5")
                        c.dma("sp", gsc[1], LF[:], reads=["LF"], writes=["D:gsc1"], sem="k6")
                        LIf = c.sb(st, "LIf", [64, 128], F32)
                        CS = c.sb(st, "CS", [64, 128], F32)
                        c.dma("sp", LIf[:], gsc[0].rearrange("h (n l) -> (h n) l", l=128), reads=["D:gsc0"], writes=["LIf"], sem="k5")
                        c.dma("sp", CS[:], gsc[1].rearrange("h (n l) -> (h n) l", l=128), reads=["D:gsc1"], writes=["CS"], sem="k6")
                        c.op("dve", lambda: nc.vector.tensor_tensor_scan(out=CS[:], data0=CS[:], data1=CS[:], initial=0.0, op0=ALU.add, op1=ALU.bypass),
                             reads=["CS"], writes=["CS"])
                        Afm = c.sb(st, "Afm", [64, 128], F32)
                        CM = c.sb(st, "CM", [64, 128], F32)
                        c.op("dve", lambda: nc.vector.tensor_tensor(out=Afm[:], in0=LIf[:], in1=CS[:], op=ALU.add), reads=["LIf", "CS"], writes=["Afm"])
                        c.op("dve", lambda: nc.vector.tensor_tensor_scan(out=CM[:], data0=Afm[:], data1=Afm[:], initial=-1e30, op0=ALU.max, op1=ALU.bypass),
                             reads=["Afm"], writes=["CM"])
                        st2 = c.sb(st, "st2", [64, 2], F32)
                        c.op("dve", lambda: nc.vector.tensor_scalar(out=st2[:, 0:1], in0=CS[:, 127:128], scalar1=-1.0, scalar2=None, op0=ALU.mult),
                             reads=["CS"], writes=["st2"])
                        c.op("dve", lambda: nc.vector.tensor_copy(out=st2[:, 1:2], in_=CM[:, 127:128]), reads=["CM"], writes=["st2"])
                        c.dma("sp", gs2, st2[:], reads=["st2"], writes=["D:gs2"], sem="k5")
                        GT = c.sb(st, "GT", [4, 16, 2], F32)
                        c.dma("sp", GT[:], gs2.rearrange("(h n) k -> h n k", n=16), reads=["D:gs2"], writes=["GT"], sem="k5")
                        ML = c.sb(st, "ML", [4, 16], F32)
                        MP = c.sb(st, "MP", [4, 17], F32)
                        c.op("dve", lambda: nc.vector.tensor_tensor(out=ML[:], in0=GT[:, :, 0], in1=GT[:, :, 1], op=ALU.add), reads=["GT"], writes=["ML"])
                        c.op("dve", lambda: nc.vector.memset(MP[:], 0.0), writes=["MP"])
                        for n in range(16):
                            c.op("dve", lambda n=n: nc.vector.scalar_tensor_tensor(
                                out=MP[:, n + 1:n + 2], in0=MP[:, n:n + 1], scalar=GT[:, n, 0:1], in1=ML[:, n:n + 1],
                                op0=ALU.add, op1=ALU.max), reads=["MP", "GT", "ML"], writes=["MP"])
                        SC = c.sb(st, "SC", [4, 16, 5], F32)
                        tq = c.sb(st, "tq", [4, 16], F32)
                        c.op("dve", lambda: nc.vector.tensor_tensor(out=tq[:], in0=GT[:, :, 0], in1=MP[:, 0:16], op=ALU.add), reads=["GT", "MP"], writes=["tq"])
                        c.op("dve", lambda: nc.vector.tensor_tensor(out=tq[:], in0=tq[:], in1=MP[:, 1:17], op=ALU.subtract), reads=["tq", "MP"], writes=["tq"])
                        c.op("act", lambda: nc.scalar.activation(out=SC[:, :, 0], in_=tq[:], func=AF.Exp), reads=["tq"], writes=["SC"])
                        tq2 = c.sb(st, "tq2", [4, 16], F32)
                        c.op("dve", lambda: nc.vector.tensor_tensor(out=tq2[:], in0=ML[:], in1=MP[:, 1:17], op=ALU.subtract), reads=["ML", "MP"], writes=["tq2"])
                        c.op("act", lambda: nc.scalar.activation(out=SC[:, :, 1], in_=tq2[:], func=AF.Exp), reads=["tq2"], writes=["SC"])
                        tq3 = c.sb(st, "tq3", [4, 16], F32)
                        c.op("dve", lambda: nc.vector.tensor_tensor(out=tq3[:], in0=MP[:, 0:16], in1=GT[:, :, 1], op=ALU.subtract), reads=["GT", "MP"], writes=["tq3"])
                        c.op("act", lambda: nc.scalar.activation(out=SC[:, :, 2], in_=tq3[:], func=AF.Exp), reads=["tq3"], writes=["SC"])
                        c.op("dve", lambda: nc.vector.tensor_copy(out=SC[:, :, 3], in_=GT[:, :, 1]), reads=["GT", "SC"], writes=["SC"])
                        c.op("dve", lambda: nc.vector.tensor_copy(out=SC[:, :, 4], in_=MP[:, 0:16]), reads=["MP", "SC"], writes=["SC"])
                        c.dma("sp", gs3, SC[:], reads=["SC"], writes=["D:gs3"], sem="k6")
                        SCc = c.sb(st, "SCc", [64, 5], F32)
                        c.dma("sp", SCc[:], gs3.rearrange("h n k -> (h n) k"), reads=["D:gs3"], writes=["SCc"], sem="k6")
                        c.dma("sp", SCb[:].rearrange("p j k -> p (j k)"),
                              gs3.rearrange("h n k -> (h n k)").rearrange("(o f) -> o f", o=1).partition_broadcast(128),
                              reads=["D:gs3"], writes=["SCb"], sem="k5")
                        Mfm = c.sb(st, "Mfm", [64, 128], F32)
                        c.op("dve", lambda: nc.vector.tensor_scalar(out=Mfm[:], in0=CM[:], scalar1=SCc[:, 4:5], scalar2=None, op0=ALU.max),
                             reads=["CM", "SCc"], writes=["Mfm"])
                        bcol = c.sb(st, "bcol", [64, 2], F32)
                        c.op("dve", lambda: nc.vector.tensor_scalar(out=bcol[:, 0:1], in0=SCc[:, 3:4], scalar1=float(math.log(128 ** -0.5)), scalar2=None, op0=ALU.add),
                             reads=["SCc"], writes=["bcol"])
                        c.op("dve", lambda: nc.vector.tensor_scalar(out=bcol[:, 1:2], in0=SCc[:, 3:4], scalar1=-1.0, scalar2=None, op0=ALU.mult),
                             reads=["SCc", "bcol"], writes=["bcol"])
                        W1f = c.sb(st, "W1f", [64, 128], F32)
                        ELf = c.sb(st, "ELf", [64, 128], F32)
                        EMf = c.sb(st, "EMf", [64, 128], F32)
                        c.op("act", lambda: nc.scalar.activation(out=W1f[:], in_=Mfm[:], func=AF.Exp, scale=-1.0, bias=bcol[:, 0:1]),
                             reads=["Mfm", "bcol"], writes=["W1f"])
                        c.op("act", lambda: nc.scalar.activation(out=ELf[:], in_=Afm[:], func=AF.Exp, bias=bcol[:, 1:2]),
                             reads=["Afm", "bcol"], writes=["ELf"])
                        c.op("dve", lambda: nc.vector.tensor_tensor(out=EMf[:], in0=CS[:], in1=Mfm[:], op=ALU.subtract), reads=["CS", "Mfm"], writes=["EMf"])
                        c.op("act", lambda: nc.scalar.activation(out=EMf[:], in_=EMf[:], func=AF.Exp), reads=["EMf"], writes=["EMf"])
                        c.dma("sp", gs4[0], W1f[:], reads=["W1f"], writes=["D:gs40"], sem="k5")
                        c.dma("sp", gs4[1], ELf[:], reads=["ELf"], writes=["D:gs41"], sem="k6")
                        ps, pn = c.psf()
                        mm(ps[:, 0:64], EMf[:], identf[0:64, 0:64], True, True, ["EMf", "identf"], [pn])
                        c.op("dve", lambda ps=ps: nc.vector.tensor_copy(out=EMT[:], in_=ps[:, 0:64]), reads=[pn], writes=["EMT"])

                        c.barrier()
                        stg.close()
                        st = st_outer
                        phase_done("mlg")
                        XP = c.sb(st, "XP", [128, 3 + S], F32)
                        acc = c.sb(st, "acc", [128, S], F32)
                        CW = c.sb(st, "CW", [128, 8, 5], F32)
                        for qk in range(2):
                            for h in range(4):
                                c0 = qk * 512 + h * 128
                                c.dma("sp", CW[:, qk * 4 + h, 0:4], conv_wT[l, c0:c0 + 128, :], writes=["CW"], sem="k0")
                                c.dma("sp", CW[:, qk * 4 + h, 4:5], conv_b[l, c0:c0 + 128].rearrange("(p o) -> p o", o=1), writes=["CW"], sem="k0")
                        c.op("dve", lambda: nc.vector.memset(XP[:, 0:3], 0.0), writes=["XP"])
                        QcT = c.sb(st, "QcT", [128, S], BF16)
                        KcT = c.sb(st, "KcT", [128, S], BF16)
                        QS = c.sb(st, "QS", [128, S], BF16)
                        KST = c.sb(st, "KST", [128, S], BF16)
                        KSm = c.sb(st, "KSm", [128, 16, 128], BF16)
                        VA = c.sb(st, "VA", [128, 16, 2, 257], BF16)
                        SGO = c.sb(st, "SGO", [128, 16, 512], BF16)
                        sgt = c.sb(st, "sgt", [128, 512], F32)
                        CA = c.sb(st, "CA", [128, 257], F32)
                        CAb = [c.sb(st, f"CAb{i}", [128, 257], BF16) for i in range(2)]
                        PTm = [c.sb(st, f"PTm{i}", [128, 4, 128], BF16) for i in range(2)]
                        sc1 = c.sb(st, "sc1", [128, 4], F32)
                        junk2 = c.sb(st, "junk2", [128, 256], F32)
                        Ym = [c.sb(st, f"Ym{i}", [128, 256], BF16) for i in range(2)]
                        YTm = [c.sb(st, "YTm0", [128, 2, S], BF16)]
                        c.op("dve", lambda: nc.vector.memset(VA[:, :, :, 256:257], 1.0), writes=["VA"])
                        wtm = WStream(c, st, "wtm", [128, 16, 512], 2,
                                      [W[l]["mv"][0], W[l]["mo"][0], W[l]["mv"][1], W[l]["mo"][1]])
                        wfm = WStream(c, st, "wfm", [128, 16, 128], 3,
                                      [W[l][k][h] for h in range(4) for k in ("mq", "mk")])
                        ml_pend = []

                        def ml_epilogue(n, pN, pNn, hn, h, hl, cs, ytm, ytmn):
                            c.op("act", lambda pN=pN: nc.scalar.activation(
                                out=sc1[:, 0:1], in_=pN[:, 256:257], func=AF.Abs), reads=[pNn], writes=["sc1"])
                            c.op("dve", lambda hn=hn: nc.vector.tensor_tensor(
                                out=sc1[:, 0:1], in0=sc1[:, 0:1], in1=EMT[:, hn:hn + 1], op=ALU.max), reads=["sc1", "EMT"], writes=["sc1"])
                            c.op("dve", lambda: nc.vector.reciprocal(out=sc1[:, 0:1], in_=sc1[:, 0:1]), reads=["sc1"], writes=["sc1"])
                            c.op("act", lambda pN=pN: nc.scalar.activation(out=junk2[:], in_=pN[:, 0:256], func=AF.Square, accum_out=sc1[:, 1:2]),
                                 reads=[pNn, "sc1"], writes=["junk2", "sc1"])
                            c.op("dve", lambda: nc.vector.scalar_tensor_tensor(
                                out=sc1[:, 2:3], in0=sc1[:, 0:1], scalar=sc1[:, 0:1], in1=sc1[:, 1:2], op0=ALU.mult, op1=ALU.mult),
                                reads=["sc1"], writes=["sc1"])
                            c.op("act", lambda: nc.scalar.activation(out=sc1[:, 2:3], in_=sc1[:, 2:3], func=AF.Sqrt, scale=1.0 / 256, bias=EPS),
                                 reads=["sc1"], writes=["sc1"])
                            c.op("dve", lambda: nc.vector.reciprocal(out=sc1[:, 2:3], in_=sc1[:, 2:3]), reads=["sc1"], writes=["sc1"])
                            c.op("dve", lambda: nc.vector.tensor_tensor(out=sc1[:, 3:4], in0=sc1[:, 2:3], in1=sc1[:, 0:1], op=ALU.mult),
                                 reads=["sc1"], writes=["sc1"])
                            ym = Ym[n % 2]
                            ymn = f"Ym{n % 2}"
                            c.op("dve", lambda pN=pN, ym=ym, n=n, hl=hl: nc.vector.scalar_tensor_tensor(
                                out=ym[:], in0=pN[:, 0:256], scalar=sc1[:, 3:4], in1=SGO[:, n, hl * 256:(hl + 1) * 256],
                                op0=ALU.mult, op1=ALU.mult), reads=[pNn, "sc1", "SGO"], writes=[ymn])
                            pb, pbn = transpose_bf(None, lambda jj, ym=ym: ym[:, jj * 128:(jj + 1) * 128], 2, [ymn], None)
                            c.op("act", lambda pb=pb, ytm=ytm, cs=cs: nc.scalar.activation(
                                out=ytm[:, :, cs], in_=pb[:, 0:256].rearrange("p (a b) -> p a b", a=2), func=AF.Copy),
                                reads=[pbn], writes=[ytmn])
                            if n == 15:
                                c.dma("sp", ybuf[1, 2 * h:2 * h + 2].rearrange("e p t -> p e t"), ytm[:], reads=[ytmn],
                                      writes=[f"D:yb1_{2 * h}", f"D:yb1_{2 * h + 1}"], sem=ytmn)

                        c.reserved = {2, 3}
                        for hp in range(2):
                            if ml_pend:
                                ml_pend.pop()()
                            wv, wvn = wtm.next(ahead=1)
                            wo, won = wtm.next(ahead=0)
                            for tl in range(16):
                                ps, pn = c.psf()
                                for k in range(16):
                                    mm(ps[:], A[:, k, tl * 128:(tl + 1) * 128], wv[:, k, :], k == 0, k == 15, ["A", wvn], [pn])
                                c.op("act", lambda ps=ps, tl=tl: nc.scalar.activation(
                                    out=VA[:, tl, :, 0:256], in_=ps[:].rearrange("p (a b) -> p a b", a=2), func=AF.Copy),
                                    reads=[pn], writes=["VA"])
                                ps, pn = c.psf()
                                for k in range(16):
                                    mm(ps[:], A[:, k, tl * 128:(tl + 1) * 128], wo[:, k, :], k == 0, k == 15, ["A", won], [pn])
                                c.op("act", lambda ps=ps: nc.scalar.activation(out=sgt[:], in_=ps[:], func=AF.Sigmoid),
                                     reads=[pn], writes=["sgt"])
                                c.op("dve", lambda tl=tl, hp=hp: nc.vector.tensor_tensor(
                                    out=SGO[:, tl, :], in0=sgt[:], in1=NWB[:, hp * 512:(hp + 1) * 512], op=ALU.mult),
                                    reads=["sgt", "NWB"], writes=["SGO"])
                            for hl in range(2):
                                h = hp * 2 + hl
                                for qk, dstT, dn in ((0, QcT, "QcT"), (1, KcT, "KcT")):
                                    wt, wn = wfm.next()
                                    ci = qk * 4 + h
                                    for tb in range(4):
                                        ps, pn = c.psf()
                                        proj_fm(wt, wn, tb * 512, ps, pn)
                                        c.op("act", lambda ps=ps, tb=tb: nc.scalar.activation(
                                            out=XP[:, 3 + tb * 512: 3 + (tb + 1) * 512], in_=ps[:], func=AF.Copy),
                                            reads=[pn], writes=["XP"])
                                    c.op("dve", lambda ci=ci: nc.vector.tensor_scalar(
                                        out=acc[:], in0=XP[:, 3:3 + S], scalar1=CW[:, ci, 3:4], scalar2=None, op0=ALU.mult),
                                        reads=["XP", "CW"], writes=["acc"])
                                    for j in (2, 1, 0):
                                        c.op("dve", lambda ci=ci, j=j: nc.vector.scalar_tensor_tensor(
                                            out=acc[:], in0=XP[:, j:j + S], scalar=CW[:, ci, j:j + 1], in1=acc[:],
                                            op0=ALU.mult, op1=ALU.add), reads=["XP", "CW", "acc"], writes=["acc"])
                                    c.op("act", lambda ci=ci, dstT=dstT: nc.scalar.activation(
                                        out=dstT[:], in_=acc[:], func=AF.Silu, bias=CW[:, ci, 4:5]),
                                        reads=["acc", "CW"], writes=[dn])
                                c.dma("sp", acc[:], gs4[0, h * 16:(h + 1) * 16, :].rearrange("n l -> (n l)").rearrange("(o f) -> o f", o=1).partition_broadcast(128),
                                      reads=["D:gs40"], writes=["acc"], sem="k1")
                                c.dma("sp", XP[:, 3:3 + S], gs4[1, h * 16:(h + 1) * 16, :].rearrange("n l -> (n l)").rearrange("(o f) -> o f", o=1).partition_broadcast(128),
                                      reads=["D:gs41"], writes=["XP"], sem="k2")
                                c.op("dve", lambda: nc.vector.tensor_tensor(out=QS[:], in0=QcT[:], in1=acc[:], op=ALU.mult),
                                     reads=["QcT", "acc"], writes=["QS"])
                                c.op("dve", lambda: nc.vector.tensor_tensor(out=KST[:], in0=KcT[:], in1=XP[:, 3:3 + S], op=ALU.mult),
                                     reads=["KcT", "XP"], writes=["KST"])
                                for cg in range(4):
                                    pb, pbn = transpose_bf(None, lambda j, cg=cg: KST[:, (cg * 4 + j) * 128:(cg * 4 + j + 1) * 128], 4, ["KST"], None)
                                    c.op("act", lambda pb=pb, cg=cg: nc.scalar.activation(
                                        out=KSm[:, cg * 4:(cg + 1) * 4, :], in_=pb[:, 0:512].rearrange("p (j k) -> p j k", j=4), func=AF.Copy),
                                        reads=[pbn], writes=["KSm"])
                                c.op("dve", lambda: nc.vector.memset(CA[:], 0.0), writes=["CA"])
                                ytm = YTm[0]
                                ytmn = "YTm0"
                                for cg in range(4):
                                    pS, pSn = c.psf()
                                    for j in range(4):
                                        n = cg * 4 + j
                                        cs = slice(n * 128, (n + 1) * 128)
                                        c.op("pe", lambda j=j, cs=cs, pS=pS: nc.tensor.matmul(
                                            pS[:, j * 128:(j + 1) * 128], KST[:, cs], QS[:, cs], start=True, stop=True),
                                            reads=["KST", "QS"], writes=[pSn], inc=(j == 3))
                                    pt = PTm[cg % 2]
                                    ptn = f"PTm{cg % 2}"
                                    c.op("dve", lambda pS=pS, pt=pt: nc.vector.tensor_tensor(
                                        out=pt[:], in0=pS[:].rearrange("p (j k) -> p j k", j=4),
                                        in1=MASK[:].unsqueeze(1).to_broadcast([128, 4, 128]), op=ALU.mult),
                                        reads=[pSn, "MASK"], writes=[ptn])
                                    for j in range(4):
                                        n = cg * 4 + j
                                        hn = h * 16 + n
                                        cs = slice(n * 128, (n + 1) * 128)
                                        pN, pNn = c.psf_at(2 + n % 2)
                                        mm(pN[:, 0:257], pt[:, j, :], VA[:, n, hl, :], True, n == 0, [ptn, "VA"], [pNn])
                                        if n > 0:
                                            mm(pN[:, 0:257], QS[:, cs], CAb[n % 2][:], False, True, ["QS", f"CAb{n % 2}"], [pNn])
                                        if n < 15:
                                            pC, pCn = c.psf()
                                            mm(pC[:, 0:257], KSm[:, n, :], VA[:, n, hl, :], True, True, ["KSm", "VA"], [pCn])
                                            c.op("dve", lambda hn=hn: nc.vector.tensor_scalar(
                                                out=CA[:], in0=CA[:], scalar1=SCb[:, hn, 0:1], scalar2=None, op0=ALU.mult),
                                                reads=["CA", "SCb"], writes=["CA"])
                                            c.op("dve", lambda hn=hn, pC=pC: nc.vector.scalar_tensor_tensor(
                                                out=CA[:], in0=pC[:, 0:257], scalar=SCb[:, hn, 1:2], in1=CA[:], op0=ALU.mult, op1=ALU.add),
                                                reads=["CA", "SCb", pCn], writes=["CA"])
                                            cb = CAb[(n + 1) % 2]
                                            c.op("act", lambda cb=cb, hn=hn: nc.scalar.activation(
                                                out=cb[:], in_=CA[:], func=AF.Copy, scale=SCb[:, hn + 1, 2:3]),
                                                reads=["CA", "SCb"], writes=[f"CAb{(n + 1) % 2}"])
                                        if ml_pend:
                                            ml_pend.pop()()
                                        ml_pend.append(lambda n=n, pN=pN, pNn=pNn, hn=hn, h=h, hl=hl, cs=cs, ytm=ytm, ytmn=ytmn:
                                                       ml_epilogue(n, pN, pNn, hn, h, hl, cs, ytm, ytmn))
                        if ml_pend:
                            ml_pend.pop()()
                        c.reserved = set()
                        c.barrier()
                        phase_done("ml")

                    with CleanStack() as st:
                        if "mb" in skip:
                            raise _SkipPhase()
                        BIAS = c.sb(st, "BIAS", [128, 8, 2, 128], F32)
                        CB = c.sb(st, "CB", [128, 8], F32)
                        PMK = c.sb(st, "PMK", [128, 16, 8], F32)
                        PIK = c.sb(st, "PIK", [128, 16, 8], F32)
                        c.dma("sp", BIAS[:], mb_bias, writes=["BIAS"], sem="k0")
                        c.dma("sp", CB[:], rel_bias[31:32, :].partition_broadcast(128), writes=["CB"], sem="k1")
                        c.dma("sp", PMK[:], c_pm, writes=["PMK"], sem="k2")
                        c.dma("sp", PIK[:], c_pi, writes=["PIK"], sem="k3")
                        QT = c.sb(st, "QT", [128, S], BF16)
                        QTf = c.sb(st, "QTf", [128, S], F32)
                        KT = c.sb(st, "KT", [128, S], BF16)
                        KM = c.sb(st, "KM", [128, 8], F32)
                        KMh = c.sb(st, "KMh", [128, 8], BF16)
                        KMl = c.sb(st, "KMl", [128, 8], BF16)
                        QL = c.sb(st, "QL", [128, S], BF16)
                        VB = c.sb(st, "VB", [128, 16, 4, 129], BF16)
                        GM = c.sb(st, "GM", [128, 16, 8], F32)
                        MX = c.sb(st, "MX", [128, 16, 8], F32)
                        SEL = c.sb(st, "SEL", [128, 16, 8], F32)
                        ACC = c.sb(st, "ACC", [128, 16, 129], F32)
                        REC = c.sb(st, "REC", [128, 16], F32)
                        PTb = [c.sb(st, f"PTb{i}", [128, 512], BF16) for i in range(4)]
                        tb_ = c.sb(st, "tb_", [128, 128], F32)
                        Yb = c.sb(st, "Yb", [128, 16, 128], BF16)
                        YTb = [c.sb(st, f"YTb{i}", [128, S], BF16) for i in range(2)]
                        c.op("dve", lambda: nc.vector.memset(VB[:, :, :, 128:129], 1.0), writes=["VB"])
                        wtm = WStream(c, st, "wtm", [128, 16, 512], 2, [W[l]["bv"][0], W[l]["bv"][1]])
                        wfm = WStream(c, st, "wfm", [128, 16, 128], 3,
                                      [W[l][k][h] for h in range(8) for k in ("bq", "bk")])
                        ipt = [0]
                        for hg in range(2):
                            wv, wvn = wtm.next()
                            for tl in range(16):
                                ps, pn = c.psf()
                                for k in range(16):
                                    mm(ps[:], A[:, k, tl * 128:(tl + 1) * 128], wv[:, k, :], k == 0, k == 15, ["A", wvn], [pn])
                                c.op("act", lambda ps=ps, tl=tl: nc.scalar.activation(
                                    out=VB[:, tl, :, 0:128], in_=ps[:].rearrange("p (a b) -> p a b", a=4), func=AF.Copy),
                                    reads=[pn], writes=["VB"])
                            if stop == "mb_v":
                                c.barrier()
                                phase_done("mb_v")
                            for hl in range(4):
                                h = hg * 4 + hl
                                wt, wn = wfm.next()
                                for tb in range(4):
                                    ts_ = slice(tb * 512, (tb + 1) * 512)
                                    ps, pn = c.psf()
                                    proj_fm(wt, wn, tb * 512, ps, pn)
                                    c.op("act", lambda ps=ps, ts_=ts_: nc.scalar.activation(out=QTf[:, ts_], in_=ps[:], func=AF.Copy, scale=float(128 ** -0.5)),
                                         reads=[pn], writes=["QTf"])
                                    c.op("dve", lambda ts_=ts_: nc.vector.tensor_copy(out=QT[:, ts_], in_=QTf[:, ts_]), reads=["QTf"], writes=["QT"])
                                wt, wn = wfm.next()
                                for tb in range(4):
                                    ts_ = slice(tb * 512, (tb + 1) * 512)
                                    ps, pn = c.psf()
                                    proj_fm(wt, wn, tb * 512, ps, pn)
                                    c.op("act", lambda ps=ps, ts_=ts_: nc.scalar.activation(out=KT[:, ts_], in_=ps[:], func=AF.Copy),
                                         reads=[pn], writes=["KT"])
                                    c.op("dve", lambda ps=ps, tb=tb: nc.vector.reduce_sum(
                                        out=KM[:, 2 * tb:2 * tb + 2], in_=ps[:].rearrange("p (a b) -> p a b", a=2), axis=AX.X),
                                        reads=[pn], writes=["KM"])
                                c.op("dve", lambda: nc.vector.tensor_scalar(out=KM[:], in0=KM[:], scalar1=1.0 / 256, scalar2=None, op0=ALU.mult),
                                     reads=["KM"], writes=["KM"])
                                if stop == "mb_km":
                                    c.barrier()
                                    phase_done("mb_km")
                                c.op("dve", lambda: nc.vector.tensor_tensor(out=QL[:], in0=QTf[:], in1=QT[:], op=ALU.subtract),
                                     reads=["QTf", "QT"], writes=["QL"])
                                c.op("dve", lambda: nc.vector.tensor_copy(out=KMh[:], in_=KM[:]), reads=["KM"], writes=["KMh"])
                                c.op("dve", lambda: nc.vector.tensor_tensor(out=KMl[:], in0=KM[:], in1=KMh[:], op=ALU.subtract),
                                     reads=["KM", "KMh"], writes=["KMl"])
                                pG, pGn = c.psf()
                                for t in range(16):
                                    tsl = slice(t * 128, (t + 1) * 128)
                                    mm(pG[:, t * 8:(t + 1) * 8], QT[:, tsl], KMh[:], True, False, ["QT", "KMh"], [pGn])
                                    mm(pG[:, t * 8:(t + 1) * 8], QT[:, tsl], KMl[:], False, False, ["QT", "KMl"], [pGn])
                                    mm(pG[:, t * 8:(t + 1) * 8], QL[:, tsl], KMh[:], False, True, ["QL", "KMh"], [pGn])
                                c.op("dve", lambda pG=pG: nc.vector.tensor_tensor(
                                    out=GM[:], in0=pG[:, 0:128].rearrange("p (t j) -> p t j", t=16), in1=PMK[:], op=ALU.add),
                                    reads=[pGn, "PMK"], writes=["GM"])
                                if stop == "mb_gm":
                                    c.barrier()
                                    phase_done("mb_gm")
                                for t in range(16):
                                    c.op("dve", lambda t=t: nc.vector.max(out=MX[:, t, :], in_=GM[:, t, :]), reads=["GM"], writes=["MX"])
                                for t in range(16):
                                    c.op("dve", lambda t=t: nc.vector.scalar_tensor_tensor(
                                        out=SEL[:, t, :], in0=GM[:, t, :], scalar=MX[:, t, 2:3], in1=PIK[:, t, :], op0=ALU.is_ge, op1=ALU.mult),
                                        reads=["GM", "MX", "PIK"], writes=["SEL"])
                                if stop == "mb_sel":
                                    c.barrier()
                                    phase_done("mb_sel")
                                c.op("dve", lambda: nc.vector.memset(ACC[:], 0.0), writes=["ACC"])
                                def mb_stage_a(g, j):
                                    pts = {}
                                    for kt in (2 * j, 2 * j + 1):
                                        t_lo = max(kt, 4 * g)
                                        if t_lo > 4 * g + 3:
                                            continue
                                        ncol = (4 * g + 4 - t_lo) * 128
                                        q0 = t_lo * 128
                                        pS, pSn = c.psf()
                                        mm(pS[:, 0:ncol], KT[:, kt * 128:(kt + 1) * 128], QT[:, q0:q0 + ncol], True, True, ["KT", "QT"], [pSn])
                                        pt = PTb[ipt[0] % 4]
                                        ptn = f"PTb{ipt[0] % 4}"
                                        ipt[0] += 1
                                        tcur = t_lo
                                        while tcur <= 4 * g + 3:
                                            o0 = (tcur - t_lo) * 128
                                            if tcur - kt <= 1:
                                                kind = tcur - kt
                                                c.op("dve", lambda pS=pS, o0=o0, kind=kind: nc.vector.tensor_tensor(
                                                    out=tb_[:], in0=pS[:, o0:o0 + 128], in1=BIAS[:, h, kind, :], op=ALU.add),
                                                    reads=[pSn, "BIAS"], writes=["tb_"])
                                                c.op("act", lambda pt=pt, o0=o0: nc.scalar.activation(out=pt[:, o0:o0 + 128], in_=tb_[:], func=AF.Exp),
                                                     reads=["tb_"], writes=[ptn])
                                                tcur += 1
                                            else:
                                                o1 = (4 * g + 4 - t_lo) * 128
                                                c.op("act", lambda pt=pt, pS=pS, o0=o0, o1=o1: nc.scalar.activation(
                                                    out=pt[:, o0:o1], in_=pS[:, o0:o1], func=AF.Exp, bias=CB[:, h:h + 1]),
                                                    reads=[pSn, "CB"], writes=[ptn])
                                                tcur = 4 * g + 4
                                        pts[kt] = (pt, ptn, t_lo)
                                    return pts

                                def mb_stage_b(g, j, pts):
                                    for half in range(2):
                                        tq_ = [t for t in (4 * g + 2 * half, 4 * g + 2 * half + 1) if t >= 2 * j]
                                        if not tq_:
                                            continue
                                        pO, pOn = c.psf()
                                        for ti, t in enumerate(tq_):
                                            kts = [kt for kt in pts if kt <= t]
                                            for ki, kt in enumerate(kts):
                                                pt, ptn, t_lo = pts[kt]
                                                o0 = (t - t_lo) * 128
                                                mm(pO[:, ti * 256:ti * 256 + 129], pt[:, o0:o0 + 128], VB[:, kt, hl, :],
                                                   ki == 0, ki == len(kts) - 1, [ptn, "VB"], [pOn])
                                        for ti, t in enumerate(tq_):
                                            own = (j == t // 2)
                                            sc = 1.0 if own else SEL[:, t, j:j + 1]
                                            c.op("dve", lambda pO=pO, ti=ti, t=t, sc=sc: nc.vector.scalar_tensor_tensor(
                                                out=ACC[:, t, :], in0=pO[:, ti * 256:ti * 256 + 129], scalar=sc, in1=ACC[:, t, :],
                                                op0=ALU.mult, op1=ALU.add), reads=[pOn, "SEL", "ACC"], writes=["ACC"])

                                blocks = [(g, j) for g in range(4) for j in range(2 * g + 2)]
                                nxt = mb_stage_a(*blocks[0])
                                for bi, (g, j) in enumerate(blocks):
                                    cur = nxt
                                    if bi + 1 < len(blocks):
                                        nxt = mb_stage_a(*blocks[bi + 1])
                                    mb_stage_b(g, j, cur)
                                if stop == "mb_att":
                                    c.barrier()
                                    phase_done("mb_att")
                                c.op("dve", lambda: nc.vector.reciprocal(out=REC[:], in_=ACC[:, :, 128]), reads=["ACC"], writes=["REC"])
                                c.op("dve", lambda: nc.vector.tensor_tensor(
                                    out=Yb[:], in0=ACC[:, :, 0:128], in1=REC[:].unsqueeze(2).to_broadcast([128, 16, 128]), op=ALU.mult),
                                    reads=["ACC", "REC"], writes=["Yb"])
                                ytb = YTb[h % 2]
                                ytbn = f"YTb{h % 2}"
                                for cg in range(4):
                                    pb, pbn = transpose_bf(None, lambda jj, cg=cg: Yb[:, cg * 4 + jj, :], 4, ["Yb"], None)
                                    c.op("act", lambda pb=pb, cg=cg, ytb=ytb: nc.scalar.activation(
                                        out=ytb[:, cg * 512:(cg + 1) * 512], in_=pb[:, 0:512], func=AF.Copy), reads=[pbn], writes=[ytbn])
                                c.dma("sp", ybuf[2, h], ytb[:], reads=[ytbn], writes=[f"D:yb2_{h}"], sem=ytbn)
                        c.barrier()
                        phase_done("mb")

                    if l == 0 and sq == 0:
                        flush_casts(["D:yb2_7"])
                    if dbg and l == 0 and sq == 0:
                        c.dma("sp", dbg_y, ybuf, reads=[f"D:yb{i}_{e}" for i in range(3) for e in range(8)], writes=["D:dbg_y"], sem="k0")

                    with CleanStack() as st:
                        YB = c.sb(st, "YB", [128, 24, 512], BF16)
                        MXT = c.sb(st, "MXT", [128, 16, 512], BF16)
                        XB = c.sb(st, "XB", [128, 16, 512], F32)
                        sqb = c.sb(st, "sqb", [128, 16, 512], BF16)
                        rs = c.sb(st, "rs", [128, 512], F32)
                        sg = [c.sb(st, f"sg{i}", [128, 512], F32) for i in range(3)]
                        pr = [c.sb(st, f"pr{i}", [128, 512], F32) for i in range(2)]
                        ggroups = []
                        bgroups = []
                        ogroups = []
                        for tb in range(4):
                            for dc in range(16):
                                for i in range(3):
                                    ggroups.append(W[l]["g"][i * 16 + dc])
                                    bgroups.append(W[l][f"br{i}"][dc])
                            for dc in range(16):
                                ogroups.append(W[l]["out"][dc])
                        wg_s = WStream(c, st, "wg", [128, 16, 128], 3, ggroups)
                        wb_s = WStream(c, st, "wbr", [128, 8, 128], 3, bgroups)
                        wo_s = WStream(c, st, "wo", [128, 16, 128], 2, ogroups)
                        for tb in range(4):
                            t0 = tb * 512
                            blk = (tok0 + t0) // 512
                            for i in range(3):
                                c.dma("sp", YB[:, i * 8:(i + 1) * 8, :], ybuf[i, :, :, t0:t0 + 512].rearrange("e p t -> p e t"),
                                      reads=[f"D:yb{i}_{e}" for e in range(8)], writes=["YB"], sem=f"YB{i}", par=True)
                            c.dma("sp", XB[:], xTv[:, :, tok0 + t0: tok0 + t0 + 512], reads=[f"D:xT{blk}"], writes=["XB"], sem="XB")
                            for dc in range(16):
                                for i in range(3):
                                    wt, wn = wg_s.next()
                                    pg, pgn = c.psf()
                                    proj_fm(wt, wn, t0, pg, pgn)
                                    c.op("act", lambda pg=pg, i=i: nc.scalar.activation(out=sg[i][:], in_=pg[:], func=AF.Sigmoid),
                                         reads=[pgn], writes=[f"sg{i}"])
                                    wt, wn = wb_s.next()
                                    pbr, pbrn = c.psf()
                                    for k in range(8):
                                        mm(pbr[:], wt[:, k, :], YB[:, i * 8 + k, :], k == 0, k == 7, [wn, "YB"], [pbrn])
                                    if i == 0:
                                        c.op("dve", lambda pbr=pbr: nc.vector.tensor_tensor(out=pr[0][:], in0=pbr[:], in1=sg[0][:], op=ALU.mult),
                                             reads=[pbrn, "sg0"], writes=["pr0"])
                                    else:
                                        c.op("dve", lambda pbr=pbr, i=i: nc.vector.tensor_tensor(out=pr[1][:], in0=pbr[:], in1=sg[i][:], op=ALU.mult),
                                             reads=[pbrn, f"sg{i}"], writes=["pr1"])
                                        if i == 1:
                                            c.op("dve", lambda: nc.vector.tensor_tensor(out=pr[0][:], in0=pr[0][:], in1=pr[1][:], op=ALU.add),
                                                 reads=["pr0", "pr1"], writes=["pr0"])
                                        else:
                                            c.op("dve", lambda dc=dc: nc.vector.tensor_tensor(out=MXT[:, dc, :], in0=pr[0][:], in1=pr[1][:], op=ALU.add),
                                                 reads=["pr0", "pr1"], writes=["MXT"])
                            for dc in range(16):
                                wt, wn = wo_s.next()
                                po, pon = c.psf()
                                for k in range(16):
                                    mm(po[:], wt[:, k, :], MXT[:, k, :], k == 0, k == 15, [wn, "MXT"], [pon])
                                c.op("dve", lambda po=po, dc=dc: nc.vector.tensor_tensor(out=XB[:, dc, :], in0=XB[:, dc, :], in1=po[:], op=ALU.add),
                                     reads=[pon, "XB"], writes=["XB"])
                            c.dma("sp", xTv[:, :, tok0 + t0: tok0 + t0 + 512], XB[:], reads=["XB"], writes=[f"D:xT{blk}"], sem="XBo")
                            norm_block((sqb, rs), XB, "XB", 2 * l + 1, lambda k, t0=t0: A[:, k, t0:t0 + 512], "A")
                        c.barrier()
                        phase_done("c1")

                    if dbg and l == 0 and sq == 0:
                        c.dma("sp", dbg_x1, xT[:, :, 0:S], reads=[f"D:xT{b}" for b in range(4)], writes=["D:dbg_x1"], sem="k0")

                    with CleanStack() as st:
                        UT = c.sb(st, "UT", [128, 64, 512], BF16)
                        sq_ = [c.sb(st, f"sq_{i}", [128, 512], F32) for i in range(2)]
                        xr = [c.sb(st, f"xr{i}", [128, 512], F32) for i in range(2)]
                        g1 = []
                        g2 = []
                        for tb in range(4):
                            for fc in range(64):
                                g1.append(W[l]["ff1"][fc])
                            for dc in range(16):
                                g2.append(W[l]["ff2"][dc])
                        w1_s = WStream(c, st, "w1", [128, 16, 128], 3, g1)
                        w2_s = WStream(c, st, "w2", [128, 64, 128], 3, g2)
                        for tb in range(4):
                            t0 = tb * 512
                            blk = (tok0 + t0) // 512
                            for fc in range(64):
                                wt, wn = w1_s.next()
                                ps, pn = c.psf()
                                proj_fm(wt, wn, t0, ps, pn)
                                s_ = sq_[fc % 2]
                                sn = f"sq_{fc % 2}"
                                c.op("act", lambda ps=ps, s_=s_: nc.scalar.activation(out=s_[:], in_=ps[:], func=AF.Square), reads=[pn], writes=[sn])
                                c.op("dve", lambda ps=ps, s_=s_, fc=fc: nc.vector.scalar_tensor_tensor(
                                    out=UT[:, fc, :], in0=ps[:], scalar=0.0, in1=s_[:], op0=ALU.is_gt, op1=ALU.mult),
                                    reads=[pn, sn], writes=["UT"])
                            for dc in range(16):
                                wt, wn = w2_s.next()
                                b = dc % 2
                                c.dma("sp", xr[b][:], xT[dc, :, tok0 + t0: tok0 + t0 + 512], reads=[f"D:xT{blk}"], writes=[f"xr{b}"], sem=f"xr{b}")
                                ps, pn = c.psf()
                                for k in range(64):
                                    mm(ps[:], wt[:, k, :], UT[:, k, :], k == 0, k == 63, [wn, "UT"], [pn])
                                c.op("dve", lambda ps=ps, b=b: nc.vector.tensor_tensor(out=xr[b][:], in0=xr[b][:], in1=ps[:], op=ALU.add),
                                     reads=[pn, f"xr{b}"], writes=[f"xr{b}"])
                                c.dma("sp", xT[dc, :, tok0 + t0: tok0 + t0 + 512], xr[b][:], reads=[f"xr{b}"], writes=[f"D:xT{blk}"], sem=f"xr{b}", par=True)
                        c.barrier()
                        phase_done("ffn")

                    if dbg and l == 0 and sq == 0:
                        c.dma("sp", dbg_x2, xT[:, :, 0:S], reads=[f"D:xT{b}" for b in range(4)], writes=["D:dbg_x2"], sem="k0")

            with CleanStack() as st:
                XB = c.sb(st, "XB", [128, 16, 512], F32)
                XN = c.sb(st, "XN", [128, 16, 512], F32)
                sqb = c.sb(st, "sqb", [128, 16, 512], BF16)
                rs = c.sb(st, "rs", [128, 512], F32)
                ot = [c.sb(st, f"ot{i}", [128, D], F32) for i in range(2)]
                for blk in range(T // 512):
                    c.dma("sp", XB[:], xTv[:, :, blk * 512:(blk + 1) * 512], reads=[f"D:xT{blk}"], writes=["XB"], sem="XB")
                    norm_block((sqb, rs), XB, "XB", 2 * DEPTH, lambda k: XN[:, k, :], "XN")
                    for tl in range(4):
                        b = tl % 2
                        for q4 in range(4):
                            ps, pn = c.psf()
                            for j in range(4):
                                dcx = q4 * 4 + j
                                c.op("pe", lambda j=j, dcx=dcx, ps=ps, tl=tl: nc.tensor.transpose(
                                    ps[:, j * 128:(j + 1) * 128], XN[:, dcx, tl * 128:(tl + 1) * 128], identf[:]),
                                    reads=["XN", "identf"], writes=[pn], inc=(j == 3))
                            if q4 % 2 == 0:
                                c.op("act", lambda ps=ps, b=b, q4=q4: nc.scalar.activation(out=ot[b][:, q4 * 512:(q4 + 1) * 512], in_=ps[:], func=AF.Copy),
                                     reads=[pn], writes=[f"ot{b}"], par=True)
                            else:
                                c.op("dve", lambda ps=ps, b=b, q4=q4: nc.vector.tensor_copy(out=ot[b][:, q4 * 512:(q4 + 1) * 512], in_=ps[:]),
                                     reads=[pn], writes=[f"ot{b}"], par=True)
                        r0 = blk * 512 + tl * 128
                        c.dma("sp", out[r0:r0 + 128, :], ot[b][:], reads=[f"ot{b}"], writes=[f"D:out{blk}_{tl}"], sem=f"ot{b}")
        try:
            body()
        except _Stop:
            c.barrier()
            if dbg:
                c.dma("sp", dbg_y, ybuf, reads=[f"D:yb{i}_{e}" for i in range(3) for e in range(8)], writes=["D:dbg_y"], sem="k0")
                c.dma("sp", dbg_x1, xT[:, :, 0:S], reads=[f"D:xT{b}" for b in range(4)], writes=["D:dbg_x1"], sem="k1")
        c.finish("sp")
    print("instructions:", c.ninst, "sems:", len(c.sem), flush=True)
    return nc


def _host_inputs(inputs, cst):
    rb = np.asarray(inputs["rel_bias"], np.float32)
    b0 = np.where(cst["_m0"][None], rb[cst["_b0"]].transpose(2, 0, 1), np.float32(NEG))
    b1 = rb[cst["_b1"]].transpose(2, 0, 1)
    mb = np.stack([b0, b1], 1)
    shared = {
        "w_in": np.ascontiguousarray(inputs["w_in"], np.float32),
        "w_branch_ret": np.ascontiguousarray(inputs["w_branch_ret"], np.float32),
        "w_branch_mlstm": np.ascontiguousarray(inputs["w_branch_mlstm"], np.float32),
        "w_branch_moba": np.ascontiguousarray(inputs["w_branch_moba"], np.float32),
        "w_out": np.ascontiguousarray(inputs["w_out"], np.float32),
        "w_ff1": np.ascontiguousarray(inputs["w_ff1"], np.float32),
        "w_ff2": np.ascontiguousarray(inputs["w_ff2"], np.float32),
        "mlstm_gate_b": np.ascontiguousarray(inputs["mlstm_gate_b"], np.float32),
        "conv_wT": np.ascontiguousarray(np.asarray(inputs["mlstm_conv_w"], np.float32).transpose(0, 2, 1)),
        "mlstm_conv_b": np.ascontiguousarray(inputs["mlstm_conv_b"], np.float32),
        "mlstm_norm_w": np.ascontiguousarray(inputs["mlstm_norm_w"], np.float32),
        "nmixT": np.ascontiguousarray(np.asarray(inputs["norm_mix_w"], np.float32).reshape(DEPTH, 16, 128).transpose(0, 2, 1)),
        "nmlpT": np.ascontiguousarray(np.asarray(inputs["norm_mlp_w"], np.float32).reshape(DEPTH, 16, 128).transpose(0, 2, 1)),
        "nfinT": np.ascontiguousarray(np.asarray(inputs["final_norm_w"], np.float32).reshape(16, 128).T),
        "rel_bias": np.ascontiguousarray(rb),
        "mb_bias": np.ascontiguousarray(mb.transpose(2, 0, 1, 3)).astype(np.float32),
    }
    for k, v in cst.items():
        if not k.startswith("_"):
            shared[k] = v
    return shared


def kernel(**inputs):
    cst = _constants()
    shared = _host_inputs(inputs, cst)
    x = np.asarray(inputs["x"], np.float32)
    B = x.shape[0]
    per = B // NCORES
    nc = build_nc(n_seq=per, depth=DEPTH, gl=cst["_gl"])
    in_maps = []
    for i in range(NCORES):
        m = dict(shared)
        m["x"] = np.ascontiguousarray(x[i * per:(i + 1) * per].reshape(per * S, D))
        in_maps.append(m)
    res = run_bass_kernel_spmd(nc, in_maps, core_ids=list(range(NCORES)))
    outs = [np.asarray(r["out"], np.float32).reshape(per, S, D) for r in res.results]
    return np.concatenate(outs, axis=0)
```

```python
import math
from contextlib import ExitStack
import numpy as np
import concourse.bass as bass
import concourse.mybir as mybir
from concourse.bass_utils import run_bass_kernel_spmd

F32 = mybir.dt.float32
BF16 = mybir.dt.bfloat16
AF = mybir.ActivationFunctionType
ALU = mybir.AluOpType
AX = mybir.AxisListType

D = 2048
S = 2048
NCORES = 8
DEPTH = 2
D_IN = 16392
D_FF = 8192
EPS = 1e-6
O_RQ, O_RK, O_RV, O_RG = 0, 1024, 2048, 3072
O_MQ, O_MK, O_MV, O_MO, O_MI, O_MF = 4096, 4608, 5120, 6144, 7168, 7172
O_BQ, O_BK, O_BV, O_G = 7176, 8200, 9224, 10248
NEG = -30000.0


class Ctx:
    def __init__(self, nc):
        self.nc = nc
        self.es = ExitStack()
        self.eng = {"pe": nc.tensor, "act": nc.scalar, "dve": nc.vector, "pool": nc.gpsimd, "sp": nc.sync}
        self.sem = {}
        self.cnt = {}
        for k in self.eng:
            self.sem[k] = self.es.enter_context(nc.semaphore("s_" + k))
            self.cnt[k] = 0
        self.waited = {k: {} for k in self.eng}
        self.res = {}
        self.ninst = {k: 0 for k in self.eng}
        self._psf = []
        self._psb = []
        self._pi = 0
        self._pb = 0

    def sb(self, st, name, shape, dt):
        self._uid = getattr(self, "_uid", 0) + 1
        return st.enter_context(self.nc.sbuf_tensor(f"{name}_u{self._uid}", list(shape), dt))

    def dsem(self, key):
        if key not in self.sem:
            self.sem[key] = self.es.enter_context(self.nc.semaphore("d_" + key))
            self.cnt[key] = 0
            assert len(self.sem) <= 100, "too many semaphores"
        return key

    def _r(self, name):
        r = self.res.get(name)
        if r is None:
            r = {"w": {}, "r": {}}
            self.res[name] = r
        return r

    def _wait(self, e, deps):
        m = {}
        for d in deps:
            if d is None:
                continue
            k, v = d
            if v > m.get(k, 0):
                m[k] = v
        for k, v in m.items():
            if k == "pe" and e == "pe":
                continue
            if self.waited[e].get(k, 0) >= v:
                continue
            self.eng[e].wait_ge(self.sem[k], v)
            self.waited[e][k] = v

    def _deps(self, reads, writes, par=False, e=None):
        deps = []
        for r in reads:
            deps.extend(self._r(r)["w"].items())
            if r.startswith("ps"):
                deps.extend((k, v) for k, v in self._r(r)["r"].items() if k != e)
        for w in writes:
            rr = self._r(w)
            if not par:
                deps.extend(rr["w"].items())
            deps.extend(rr["r"].items())
        return deps

    def _commit(self, ticket, reads, writes, par=False):
        k, v = ticket
        for r in reads:
            rr = self._r(r)
            if rr["r"].get(k, 0) < v:
                rr["r"][k] = v
        for w in writes:
            rr = self._r(w)
            if par:
                if rr["w"].get(k, 0) < v:
                    rr["w"][k] = v
            else:
                rr["w"] = {k: v}
                rr["r"] = {}

    def op(self, e, fn, reads=(), writes=(), inc=True, par=False):
        self._wait(e, self._deps(reads, writes, par, e))
        ins = fn()
        self.ninst[e] += 1
        if inc:
            ins.then_inc(self.sem[e], 1)
            self.cnt[e] += 1
            ticket = (e, self.cnt[e])
        else:
            assert e == "pe"
            ticket = (e, self.cnt[e] + 1)
        self._commit(ticket, reads, writes, par)
        return ticket

    def dma(self, q, out, in_, reads=(), writes=(), sem=None, par=False, **kw):
        key = self.dsem(sem)
        deps = self._deps(reads, writes, par)
        if self.cnt[key] > 0:
            deps.append((key, self.cnt[key]))
        self._wait(q, deps)
        ins = self.eng[q].dma_start(out=out, in_=in_, **kw)
        ins.then_inc(self.sem[key], 16)
        self.ninst[q] += 1
        self.cnt[key] += 16
        ticket = (key, self.cnt[key])
        self._commit(ticket, reads, writes, par)
        return ticket

    def barrier(self, engines=("pe", "act", "dve", "sp")):
        deps = []
        for rr in self.res.values():
            deps.extend(rr["w"].items())
            deps.extend(rr["r"].items())
        deps = [d for d in deps if d is not None and not str(d[0]).startswith("cast")]
        for e in engines:
            self._wait(e, deps)
        for n in list(self.res.keys()):
            if not n.startswith("D:"):
                del self.res[n]

    def finish(self, e="sp"):
        deps = []
        for rr in self.res.values():
            deps.extend(rr["w"].items())
            deps.extend(rr["r"].items())
        self._wait(e, deps)

    def psf(self):
        while True:
            i = self._pi % len(self._psf)
            self._pi += 1
            if i not in getattr(self, "reserved", ()):
                return self._psf[i], f"psf{i}"

    def psf_at(self, i):
        return self._psf[i], f"psf{i}"

    def psb(self):
        i = self._pb % len(self._psb)
        self._pb += 1
        return self._psb[i], f"psb{i}"


class WStream:
    def __init__(self, c, st, name, shape, nbuf, groups):
        self.c = c
        self.name = name
        self.nbuf = nbuf
        self.groups = groups
        self.tiles = [c.sb(st, f"{name}{i}", shape, BF16) for i in range(nbuf)]
        self.il = 0
        self.iu = 0

    def _load(self):
        ap, deps = self.groups[self.il]
        slot = self.il % self.nbuf
        self.c.dma("sp", self.tiles[slot][:], ap, reads=deps, writes=[f"{self.name}{slot}"], sem=f"{self.name}{slot}")
        self.il += 1

    def next(self, ahead=None):
        if ahead is None:
            ahead = self.nbuf - 1
        while self.il < min(self.iu + 1 + ahead, len(self.groups)):
            self._load()
        slot = self.iu % self.nbuf
        self.iu += 1
        return self.tiles[slot], f"{self.name}{slot}"


def _t5_bucket(dist):
    n = np.maximum(dist, 0)
    exact = 16
    nf = np.maximum(n, 1).astype(np.float32)
    large = exact + (np.log(nf / np.float32(exact)) / np.float32(math.log(128 / exact)) * np.float32(16)).astype(np.int32)
    large = np.minimum(large, 31)
    return np.where(n < exact, n, large)


def _constants():
    cst = {}
    half = 64
    inv = (np.float32(10000.0) ** (-np.arange(half, dtype=np.float32) / np.float32(half))).astype(np.float32)
    pos = np.arange(S, dtype=np.float32)
    ang = (pos[:, None] * inv[None, :]).astype(np.float32)
    cos = np.cos(ang).astype(np.float32).T
    sin = np.sin(ang).astype(np.float32).T
    cst["c_cos"] = np.ascontiguousarray(np.concatenate([cos, cos], 0))
    cst["c_sin"] = np.ascontiguousarray(np.concatenate([-sin, sin], 0))
    H = 8
    L = 128
    lg = np.log1p(-np.exp2(-5.0 - np.arange(H, dtype=np.float64)))
    idx = np.arange(L, dtype=np.float64)
    scale = 128 ** -0.5
    diff = idx[None, :] - idx[:, None]
    dt = np.where(diff[None] >= 0, np.exp(np.maximum(diff[None], 0) * lg[:, None, None]), 0.0) * scale
    cst["c_dt"] = np.ascontiguousarray(dt.transpose(1, 0, 2)).astype(np.float32)
    xi = np.exp((idx + 1.0)[None, :] * lg[:, None]) * scale
    cst["c_xi"] = np.ascontiguousarray(np.broadcast_to(xi[None], (128, H, L))).astype(np.float32)
    zeta = np.exp((L - 1 - idx)[None, :] * lg[:, None])
    cst["c_zeta"] = np.ascontiguousarray(zeta.T).astype(np.float32)
    gl = np.exp(L * lg)
    cst["_gl"] = [float(np.float32(v)) for v in gl]
    g16 = np.zeros((128, H, 16), np.float32)
    g16[:, :, 1:] = gl.astype(np.float32)[None, :, None]
    cst["c_g16"] = g16
    cst["c_mask"] = (idx[:, None] <= idx[None, :]).astype(np.float32)
    cst["c_ident"] = np.eye(128, dtype=np.float32)
    pm = np.zeros((128, 16, 8), np.float32)
    pi = np.zeros((128, 16, 8), np.float32)
    for t in range(16):
        own = t // 2
        for j in range(8):
            if j < own:
                pi[:, t, j] = 1.0
            else:
                pm[:, t, j] = -1e30
    cst["c_pm"] = pm
    cst["c_pi"] = pi
    k = np.arange(128)[:, None]
    q = np.arange(128)[None, :]
    cst["_b0"] = _t5_bucket(q - k)
    cst["_m0"] = (k <= q)
    cst["_b1"] = _t5_bucket(q - k + 128)
    return cst


class _Stop(Exception):
    pass


class _SkipPhase(Exception):
    pass


class CleanStack(ExitStack):
    def __exit__(self, *exc):
        super().__exit__(None, None, None)
        return bool(exc and exc[0] is _SkipPhase)


def build_nc(n_seq=2, depth=DEPTH, dbg=False, gl=None, stop=None, skip=()):
    T = n_seq * S
    nc = bass.Bass("TRN2", target_bir_lowering=False)

    def din(name, shape, dt=F32):
        return nc.dram_tensor(name, list(shape), dt, kind="ExternalInput").ap()

    x_in = din("x", [T, D])
    w_in = din("w_in", [DEPTH, D, D_IN])
    w_br = [din("w_branch_ret", [DEPTH, 1024, D]), din("w_branch_mlstm", [DEPTH, 1024, D]),
            din("w_branch_moba", [DEPTH, 1024, D])]
    w_out = din("w_out", [DEPTH, D, D])
    w_ff1 = din("w_ff1", [DEPTH, D, D_FF])
    w_ff2 = din("w_ff2", [DEPTH, D_FF, D])
    gate_b = din("mlstm_gate_b", [DEPTH, 2, 4])
    conv_wT = din("conv_wT", [DEPTH, 1024, 4])
    conv_b = din("mlstm_conv_b", [DEPTH, 1024])
    ml_nw = din("mlstm_norm_w", [DEPTH, 1024])
    nmix = din("nmixT", [DEPTH, 128, 16])
    nmlp = din("nmlpT", [DEPTH, 128, 16])
    nfin = din("nfinT", [128, 16])
    rel_bias = din("rel_bias", [32, 8])
    mb_bias = din("mb_bias", [128, 8, 2, 128])
    c_cos = din("c_cos", [128, S])
    c_sin = din("c_sin", [128, S])
    c_dt = din("c_dt", [128, 8, 128])
    c_xi = din("c_xi", [128, 8, 128])
    c_zeta = din("c_zeta", [128, 8])
    c_g16 = din("c_g16", [128, 8, 16])
    c_mask = din("c_mask", [128, 128])
    c_ident = din("c_ident", [128, 128])
    c_pm = din("c_pm", [128, 16, 8])
    c_pi = din("c_pi", [128, 16, 8])
    out = nc.dram_tensor("out", [T, D], F32, kind="ExternalOutput").ap()

    def dscr(name, shape, dt):
        return nc.dram_tensor(name, list(shape), dt, kind="Internal").ap()

    xT = dscr("xT", [16, 128, T], F32)
    ybuf = dscr("ybuf", [3, 8, 128, S], BF16)
    gsc = dscr("gsc", [2, 4, S], F32)
    gs2 = dscr("gs2", [64, 2], F32)
    gs3 = dscr("gs3", [4, 16, 5], F32)
    gs4 = dscr("gs4", [2, 64, 128], F32)
    if dbg:
        dbg_y = nc.dram_tensor("dbg_y", [3, 8, 128, S], BF16, kind="ExternalOutput").ap()
        dbg_x1 = nc.dram_tensor("dbg_x1", [16, 128, S], F32, kind="ExternalOutput").ap()
        dbg_x2 = nc.dram_tensor("dbg_x2", [16, 128, S], F32, kind="ExternalOutput").ap()

    c = Ctx(nc)
    ncast = [0]
    cast_q = [[]]

    def flush_casts(wait_names=()):
        deps = []
        for n in wait_names:
            deps.extend(c._r(n)["w"].items())
        if deps:
            c._wait("pool", deps)
        for q in cast_q:
            for f in q:
                f()
        cast_q.clear()
        cast_q.append([])

    def cast_family(name, src2d, K, ncols, gw, col0=0):
        kc = K // 128
        G = ncols // gw
        dst = dscr("wb_" + name, [G, 128, kc, gw], BF16)
        srcv = src2d.rearrange("(kc p) n -> p kc n", p=128)
        groups = []
        for g in range(G):
            deps = []
            for k0 in range(0, kc, 16):
                k1 = min(kc, k0 + 16)
                rn = f"D:wb_{name}_{g}_{k0}"
                def emit(g=g, k0=k0, k1=k1, rn=rn):
                    c.dma("pool", dst[g][:, k0:k1, :], srcv[:, k0:k1, col0 + g * gw: col0 + (g + 1) * gw],
                          writes=[rn], sem=f"cast{ncast[0] % 16}")
                    ncast[0] += 1
                cast_q[-1].append(emit)
                deps.append(rn)
            groups.append((dst[g], deps))
        return groups

    with c.es:
        gst = c.es
        for i in range(6):
            c._psf.append(gst.enter_context(nc.psum_tensor(f"psf{i}", [128, 512], F32)))
        for i in range(2):
            c._psb.append(gst.enter_context(nc.psum_tensor(f"psb{i}", [128, 1024], BF16)))
        A = c.sb(gst, "A", [128, 16, S], BF16)
        identf = c.sb(gst, "identf", [128, 128], F32)
        identb = c.sb(gst, "identb", [128, 128], BF16)
        onesb = c.sb(gst, "onesb", [128, 128], BF16)
        nw_all = c.sb(gst, "nw_all", [128, 2 * DEPTH + 1, 16], F32)

        c.dma("sp", identf[:], c_ident, writes=["identf"], sem="k0")
        c.op("dve", lambda: nc.vector.tensor_copy(out=identb[:], in_=identf[:]), reads=["identf"], writes=["identb"])
        c.op("dve", lambda: nc.vector.memset(onesb[:], 1.0), writes=["onesb"])
        for l in range(DEPTH):
            c.dma("sp", nw_all[:, 2 * l, :], nmix[l], writes=["nw_all"], sem="k0")
            c.dma("sp", nw_all[:, 2 * l + 1, :], nmlp[l], writes=["nw_all"], sem="k0")
        c.dma("sp", nw_all[:, 2 * DEPTH, :], nfin, writes=["nw_all"], sem="k0")

        W = []
        for l in range(depth):
            wl = {}
            wi = w_in[l]
            wl["rv"] = cast_family(f"rv{l}", wi, D, 1024, 512, O_RV)
            wl["rg"] = cast_family(f"rg{l}", wi, D, 1024, 512, O_RG)
            wl["rq"] = cast_family(f"rq{l}", wi, D, 1024, 128, O_RQ)
            wl["rk"] = cast_family(f"rk{l}", wi, D, 1024, 128, O_RK)
            if l == 0:
                flush_casts()
            wl["mi"] = cast_family(f"mi{l}", wi, D, 4, 4, O_MI)
            wl["mf"] = cast_family(f"mf{l}", wi, D, 4, 4, O_MF)
            wl["mv"] = cast_family(f"mv{l}", wi, D, 1024, 512, O_MV)
            wl["mo"] = cast_family(f"mo{l}", wi, D, 1024, 512, O_MO)
            wl["mq"] = cast_family(f"mq{l}", wi, D, 512, 128, O_MQ)
            wl["mk"] = cast_family(f"mk{l}", wi, D, 512, 128, O_MK)
            wl["bv"] = cast_family(f"bv{l}", wi, D, 1024, 512, O_BV)
            wl["bq"] = cast_family(f"bq{l}", wi, D, 1024, 128, O_BQ)
            wl["bk"] = cast_family(f"bk{l}", wi, D, 1024, 128, O_BK)
            wl["g"] = cast_family(f"g{l}", wi, D, 6144, 128, O_G)
            for i in range(3):
                wl[f"br{i}"] = cast_family(f"br{i}_{l}", w_br[i][l], 1024, D, 128)
            wl["out"] = cast_family(f"out{l}", w_out[l], D, D, 128)
            wl["ff1"] = cast_family(f"ff1_{l}", w_ff1[l], D, D_FF, 128)
            wl["ff2"] = cast_family(f"ff2_{l}", w_ff2[l], D_FF, D, 128)
            W.append(wl)
            if l == 0:
                cast_q.append([])

        def mm(o, lhsT, rhs, start, stop, reads, writes):
            return c.op("pe", lambda: nc.tensor.matmul(o, lhsT, rhs, start=start, stop=stop),
                        reads=reads, writes=writes, inc=bool(stop))

        def proj_fm(wt, wn, t0, ps, pn, kc=16, src=None, srcn="A", m=128):
            srcT = A if src is None else src
            for k in range(kc):
                mm(ps[0:m, :], wt[:, k, 0:m], srcT[:, k, t0:t0 + 512], k == 0, k == kc - 1, [wn, srcn], [pn])

        def transpose_bf(dst_fn, src_fn, n, reads, dst_reads_writes):
            pb, pbn = c.psb()
            for j in range(n):
                c.op("pe", lambda j=j: nc.tensor.transpose(pb[:, j * 128:(j + 1) * 128], src_fn(j), identb[:]),
                     reads=list(reads) + ["identb"], writes=[pbn], inc=(j == n - 1))
            return pb, pbn

        def norm_block(st_tiles, xblk, xn, nwi, dst, dstn):
            sqb, rs = st_tiles
            c.op("act", lambda: nc.scalar.activation(out=sqb[:], in_=xblk[:], func=AF.Square), reads=[xn], writes=["sqb"])
            ps, pn = c.psf()
            for k in range(16):
                mm(ps[:], onesb[:], sqb[:, k, :], k == 0, k == 15, ["onesb", "sqb"], [pn])
            c.op("act", lambda: nc.scalar.activation(out=rs[:], in_=ps[:], func=AF.Sqrt, scale=1.0 / D, bias=EPS),
                 reads=[pn], writes=["rs"])
            c.op("dve", lambda: nc.vector.reciprocal(out=rs[:], in_=rs[:]), reads=["rs"], writes=["rs"])
            for k in range(16):
                c.op("dve", lambda k=k: nc.vector.scalar_tensor_tensor(
                    out=dst(k), in0=xblk[:, k, :], scalar=nw_all[:, nwi, k:k + 1], in1=rs[:],
                    op0=ALU.mult, op1=ALU.mult), reads=[xn, "rs", "nw_all"], writes=[dstn], par=True)

        xTv = xT.rearrange("dc p t -> p dc t")

        with CleanStack() as st:
            xt = [c.sb(st, f"xt{i}", [128, D], F32) for i in range(2)]
            xs = [c.sb(st, f"xs{i}", [128, 16, 128], F32) for i in range(2)]
            for tt in range(T // 128):
                b = tt % 2
                c.dma("sp", xt[b][:], x_in[tt * 128:(tt + 1) * 128, :], writes=[f"xt{b}"], sem=f"xt{b}")
                for q4 in range(4):
                    ps, pn = c.psf()
                    for j in range(4):
                        dcx = q4 * 4 + j
                        c.op("pe", lambda j=j, dcx=dcx, ps=ps: nc.tensor.transpose(
                            ps[:, j * 128:(j + 1) * 128], xt[b][:, dcx * 128:(dcx + 1) * 128], identf[:]),
                            reads=[f"xt{b}", "identf"], writes=[pn], inc=(j == 3))
                    eng = "act" if q4 % 2 == 0 else "dve"
                    dstap = xs[b][:, q4 * 4:(q4 + 1) * 4, :]
                    if eng == "act":
                        c.op("act", lambda ps=ps, dstap=dstap: nc.scalar.activation(
                            out=dstap, in_=ps[:].rearrange("p (j k) -> p j k", j=4), func=AF.Copy),
                            reads=[pn], writes=[f"xs{b}"], par=True)
                    else:
                        c.op("dve", lambda ps=ps, dstap=dstap: nc.vector.tensor_copy(
                            out=dstap, in_=ps[:].rearrange("p (j k) -> p j k", j=4)),
                            reads=[pn], writes=[f"xs{b}"], par=True)
                blk = tt // 4
                c.dma("sp", xTv[:, :, tt * 128:(tt + 1) * 128], xs[b][:], reads=[f"xs{b}"], writes=[f"D:xT{blk}"],
                      sem=f"xs{b}", par=True)
            c.barrier()

        gl = gl or _constants()["_gl"]

        def phase_done(name):
            if stop == name:
                raise _Stop()

        later = cast_q[1:] if len(cast_q) > 1 else []
        del cast_q[1:]
        flush_casts([f"D:xT{b}" for b in range(T // 512)])
        for q in later:
            cast_q[-1].extend(q)

        def body():
            phase_done("p0")
            for l in range(depth):
                for sq in range(n_seq):
                    tok0 = sq * S
                    with CleanStack() as st:
                        xb = [c.sb(st, f"xb{i}", [128, 16, 512], F32) for i in range(2)]
                        sqb = c.sb(st, "sqb", [128, 16, 512], BF16)
                        rs = c.sb(st, "rs", [128, 512], F32)
                        for tb in range(4):
                            b = tb % 2
                            blk = (tok0 + tb * 512) // 512
                            c.dma("sp", xb[b][:], xTv[:, :, tok0 + tb * 512: tok0 + (tb + 1) * 512],
                                  reads=[f"D:xT{blk}"], writes=[f"xb{b}"], sem=f"xb{b}")
                            norm_block((sqb, rs), xb[b], f"xb{b}", 2 * l,
                                       lambda k, tb=tb: A[:, k, tb * 512:(tb + 1) * 512], "A")
                        c.barrier()
                        phase_done("n1")

                    with CleanStack() as st:
                        if "ret" in skip:
                            raise _SkipPhase()
                        COS = c.sb(st, "COS", [128, S], F32)
                        SIN = c.sb(st, "SIN", [128, S], F32)
                        DT = c.sb(st, "DT", [128, 8, 128], F32)
                        XI = c.sb(st, "XI", [128, 8, 128], F32)
                        ZETA = c.sb(st, "ZETA", [128, 8], F32)
                        c.dma("sp", COS[:], c_cos, writes=["COS"], sem="k0")
                        c.dma("sp", SIN[:], c_sin, writes=["SIN"], sem="k1")
                        c.dma("sp", DT[:], c_dt, writes=["DT"], sem="k2")
                        c.dma("sp", XI[:], c_xi, writes=["XI"], sem="k3")
                        c.dma("sp", ZETA[:], c_zeta, writes=["ZETA"], sem="k4")
                        QT = c.sb(st, "QT", [128, S], BF16)
                        KT = c.sb(st, "KT", [128, S], BF16)
                        QX = c.sb(st, "QX", [128, S], BF16)
                        KZ = c.sb(st, "KZ", [128, 16, 128], BF16)
                        VV = c.sb(st, "VV", [128, 16, 512], BF16)
                        SG = c.sb(st, "SG", [128, 16, 512], BF16)
                        t1 = c.sb(st, "t1", [128, 512], F32)
                        t2 = c.sb(st, "t2", [128, 512], F32)
                        G16 = c.sb(st, "G16", [128, 8, 16], F32)
                        c.dma("sp", G16[:], c_g16, writes=["G16"], sem="k5")
                        KVs = c.sb(st, "KVs", [128, 128, 16], F32)
                        GMUL = c.sb(st, "GMUL", [128, 128, 16], F32)
                        Rbn = c.sb(st, "Rbn", [128, 16, 128], BF16)
                        c.op("dve", lambda: nc.vector.memset(KVs[:, :, 0:1], 0.0), writes=["KVs"])
                        PT = [c.sb(st, f"PT{i}", [128, 4, 128], BF16) for i in range(2)]
                        junk = c.sb(st, "junk", [128, 512], F32)
                        ss4 = c.sb(st, "ss4", [128, 4], F32)
                        yn = c.sb(st, "yn", [128, 4, 128], F32)
                        Yst = [c.sb(st, f"Yst{i}", [128, 4, 128], BF16) for i in range(2)]
                        YT = [c.sb(st, f"YT{i}", [128, S], BF16) for i in range(2)]
                        wtm = WStream(c, st, "wtm", [128, 16, 512], 1,
                                      [W[l]["rv"][0], W[l]["rg"][0], W[l]["rv"][1], W[l]["rg"][1]])
                        wfm = WStream(c, st, "wfm", [128, 16, 128], 3,
                                      [W[l][k][h] for h in range(8) for k in ("rq", "rk")])
                        ret_pend = []

                        def ret_epilogue(cg, pO, pOn, h, hc, yt, ytn):
                            c.op("act", lambda pO=pO: nc.scalar.activation(out=junk[:], in_=pO[:], func=AF.Square),
                                 reads=[pOn], writes=["junk"])
                            c.op("dve", lambda: nc.vector.reduce_sum(out=ss4[:], in_=junk[:].rearrange("p (j k) -> p j k", j=4), axis=AX.X),
                                 reads=["junk"], writes=["ss4"])
                            c.op("act", lambda: nc.scalar.activation(out=ss4[:], in_=ss4[:], func=AF.Sqrt, scale=1.0 / 128, bias=EPS),
                                 reads=["ss4"], writes=["ss4"])
                            c.op("dve", lambda: nc.vector.reciprocal(out=ss4[:], in_=ss4[:]), reads=["ss4"], writes=["ss4"])
                            c.op("dve", lambda pO=pO: nc.vector.tensor_tensor(
                                out=yn[:], in0=pO[:].rearrange("p (j k) -> p j k", j=4),
                                in1=ss4[:].unsqueeze(2).to_broadcast([128, 4, 128]), op=ALU.mult),
                                reads=[pOn, "ss4"], writes=["yn"])
                            ys = Yst[cg % 2]
                            ysn = f"Yst{cg % 2}"
                            c.op("dve", lambda ys=ys, cg=cg: nc.vector.tensor_tensor(
                                out=ys[:], in0=yn[:], in1=SG[:, cg * 4:(cg + 1) * 4, hc], op=ALU.mult),
                                reads=["yn", "SG"], writes=[ysn])
                            pb, pbn = transpose_bf(None, lambda j, ys=ys: ys[:, j, :], 4, [ysn], None)
                            c.op("act", lambda pb=pb, cg=cg, yt=yt: nc.scalar.activation(
                                out=yt[:, cg * 512:(cg + 1) * 512], in_=pb[:, 0:512], func=AF.Copy),
                                reads=[pbn], writes=[ytn])
                            if cg == 3:
                                c.dma("sp", ybuf[0, h], yt[:], reads=[ytn], writes=[f"D:yb0_{h}"], sem=ytn)

                        c.reserved = {2, 3}
                        for hg in range(2):
                            if ret_pend:
                                ret_pend.pop()()
                            wv, wvn = wtm.next(ahead=0)
                            for tl in range(16):
                                ps, pn = c.psf()
                                for k in range(16):
                                    mm(ps[:], A[:, k, tl * 128:(tl + 1) * 128], wv[:, k, :], k == 0, k == 15, ["A", wvn], [pn])
                                c.op("act", lambda ps=ps, tl=tl: nc.scalar.activation(out=VV[:, tl, :], in_=ps[:], func=AF.Copy),
                                     reads=[pn], writes=["VV"])
                            wg, wgn = wtm.next(ahead=0)
                            for tl in range(16):
                                ps, pn = c.psf()
                                for k in range(16):
                                    mm(ps[:], A[:, k, tl * 128:(tl + 1) * 128], wg[:, k, :], k == 0, k == 15, ["A", wgn], [pn])
                                c.op("act", lambda ps=ps, tl=tl: nc.scalar.activation(out=SG[:, tl, :], in_=ps[:], func=AF.Silu),
                                     reads=[pn], writes=["SG"])
                            for hl in range(4):
                                h = hg * 4 + hl
                                hc = slice(hl * 128, (hl + 1) * 128)
                                for dstT, dn in ((QT, "QT"), (KT, "KT")):
                                    wt, wn = wfm.next()
                                    for tb in range(4):
                                        ts_ = slice(tb * 512, (tb + 1) * 512)
                                        ps, pn = c.psf()
                                        proj_fm(wt, wn, tb * 512, ps, pn)
                                        c.op("dve", lambda ps=ps, ts_=ts_: nc.vector.tensor_tensor(
                                            out=t1[0:64, :], in0=ps[64:128, :], in1=SIN[0:64, ts_], op=ALU.mult),
                                            reads=[pn, "SIN"], writes=["t1"])
                                        c.op("dve", lambda ps=ps, ts_=ts_: nc.vector.tensor_tensor(
                                            out=t1[64:128, :], in0=ps[0:64, :], in1=SIN[64:128, ts_], op=ALU.mult),
                                            reads=[pn, "SIN"], writes=["t1"])
                                        c.op("dve", lambda ps=ps, ts_=ts_: nc.vector.tensor_tensor(
                                            out=t2[:], in0=ps[:], in1=COS[:, ts_], op=ALU.mult),
                                            reads=[pn, "COS"], writes=["t2"])
                                        c.op("dve", lambda dstT=dstT, ts_=ts_: nc.vector.tensor_tensor(
                                            out=dstT[:, ts_], in0=t1[:], in1=t2[:], op=ALU.add),
                                            reads=["t1", "t2"], writes=[dn])
                                c.op("dve", lambda h=h: nc.vector.tensor_tensor(
                                    out=QX[:].rearrange("p (n l) -> p n l", n=16), in0=QT[:].rearrange("p (n l) -> p n l", n=16),
                                    in1=XI[:, h:h + 1, :].to_broadcast([128, 16, 128]), op=ALU.mult),
                                    reads=["QT", "XI"], writes=["QX"])
                                for cg in range(4):
                                    pb, pbn = transpose_bf(None, lambda j, cg=cg: KT[:, (cg * 4 + j) * 128:(cg * 4 + j + 1) * 128], 4, ["KT"], None)
                                    c.op("act", lambda pb=pb, cg=cg, h=h: nc.scalar.activation(
                                        out=KZ[:, cg * 4:(cg + 1) * 4, :], in_=pb[:, 0:512].rearrange("p (j k) -> p j k", j=4),
                                        func=AF.Copy, scale=ZETA[:, h:h + 1]), reads=[pbn, "ZETA"], writes=["KZ"])
                                for cgk in range(4):
                                    nk = 4 if cgk < 3 else 3
                                    pK, pKn = c.psf()
                                    for j in range(nk):
                                        n = cgk * 4 + j
                                        c.op("pe", lambda j=j, n=n, pK=pK: nc.tensor.matmul(
                                            pK[:, j * 128:(j + 1) * 128], KZ[:, n, :], VV[:, n, hc], start=True, stop=True),
                                            reads=["KZ", "VV"], writes=[pKn], inc=(j == nk - 1))
                                    c.op("act", lambda pK=pK, cgk=cgk, nk=nk: nc.scalar.activation(
                                        out=KVs[:, :, cgk * 4 + 1: cgk * 4 + 1 + nk],
                                        in_=pK[:, 0:nk * 128].rearrange("p (j e) -> p e j", j=nk), func=AF.Copy),
                                        reads=[pKn], writes=["KVs"])
                                c.op("dve", lambda h=h: nc.vector.tensor_copy(
                                    out=GMUL[:], in_=G16[:, h:h + 1, :].to_broadcast([128, 128, 16])), reads=["G16"], writes=["GMUL"])
                                c.op("dve", lambda: nc.vector.tensor_tensor_scan(
                                    out=KVs[:].rearrange("p e n -> p (e n)"), data0=GMUL[:].rearrange("p e n -> p (e n)"),
                                    data1=KVs[:].rearrange("p e n -> p (e n)"),
                                    initial=0.0, op0=ALU.mult, op1=ALU.add), reads=["KVs", "GMUL"], writes=["KVs"])
                                c.op("dve", lambda: nc.vector.tensor_copy(out=Rbn[:], in_=KVs[:].rearrange("p e n -> p n e")),
                                     reads=["KVs"], writes=["Rbn"])
                                yt = YT[h % 2]
                                ytn = f"YT{h % 2}"
                                c.reserved = {2, 3}
                                for cg in range(4):
                                    pS, pSn = c.psf()
                                    for j in range(4):
                                        n = cg * 4 + j
                                        cs = slice(n * 128, (n + 1) * 128)
                                        c.op("pe", lambda j=j, cs=cs, pS=pS: nc.tensor.matmul(
                                            pS[:, j * 128:(j + 1) * 128], KT[:, cs], QT[:, cs], start=True, stop=True),
                                            reads=["KT", "QT"], writes=[pSn], inc=(j == 3))
                                    pt = PT[cg % 2]
                                    ptn = f"PT{cg % 2}"
                                    c.op("dve", lambda pS=pS, pt=pt, h=h: nc.vector.tensor_tensor(
                                        out=pt[:], in0=pS[:].rearrange("p (j k) -> p j k", j=4),
                                        in1=DT[:, h:h + 1, :].to_broadcast([128, 4, 128]), op=ALU.mult),
                                        reads=[pSn, "DT"], writes=[ptn])
                                    pO, pOn = c.psf_at(2 + cg % 2)
                                    for j in range(4):
                                        n = cg * 4 + j
                                        cs = slice(n * 128, (n + 1) * 128)
                                        mm(pO[:, j * 128:(j + 1) * 128], pt[:, j, :], VV[:, n, hc], True, n == 0, [ptn, "VV"], [pOn])
                                        if n > 0:
                                            mm(pO[:, j * 128:(j + 1) * 128], QX[:, cs], Rbn[:, n, :], False, True, ["QX", "Rbn"], [pOn])
                                    if ret_pend:
                                        ret_pend.pop()()
                                    ret_pend.append(lambda cg=cg, pO=pO, pOn=pOn, h=h, hc=hc, yt=yt, ytn=ytn: ret_epilogue(cg, pO, pOn, h, hc, yt, ytn))
                        if ret_pend:
                            ret_pend.pop()()
                        c.reserved = set()
                        c.barrier()
                        phase_done("ret")

                    with CleanStack() as st:
                        if "ml" in skip:
                            raise _SkipPhase()
                        MASK = c.sb(st, "MASK", [128, 128], F32)
                        c.dma("sp", MASK[:], c_mask, writes=["MASK"], sem="k0")
                        gb = c.sb(st, "gb", [4, 2], F32)
                        c.dma("sp", gb[:, 0:1], gate_b[l, 0, :].rearrange("(h o) -> h o", o=1), writes=["gb"], sem="k1")
                        c.dma("sp", gb[:, 1:2], gate_b[l, 1, :].rearrange("(h o) -> h o", o=1), writes=["gb"], sem="k1")
                        c.op("dve", lambda: nc.vector.tensor_scalar(out=gb[:, 1:2], in0=gb[:, 1:2], scalar1=-1.0, scalar2=None, op0=ALU.mult),
                             reads=["gb"], writes=["gb"])
                        NWB = c.sb(st, "NWB", [128, 1024], F32)
                        c.dma("sp", NWB[:], ml_nw[l:l + 1, :].partition_broadcast(128), writes=["NWB"], sem="k2")
                        EMT = c.sb(st, "EMT", [128, 64], F32)
                        SCb = c.sb(st, "SCb", [128, 64, 5], F32)
                        stg = ExitStack()
                        st_outer = st
                        st = stg
                        wif = [c.sb(st, f"wif{i}", [128, 16, 4], BF16) for i in range(2)]
                        c.dma("sp", wif[0][:], W[l]["mi"][0][0], reads=W[l]["mi"][0][1], writes=["wif0"], sem="k3")
                        c.dma("sp", wif[1][:], W[l]["mf"][0][0], reads=W[l]["mf"][0][1], writes=["wif1"], sem="k4")
                        LI = c.sb(st, "LI", [4, S], F32)
                        LF = c.sb(st, "LF", [4, S], F32)
                        for tb in range(4):
                            ts_ = slice(tb * 512, (tb + 1) * 512)
                            ps, pn = c.psf()
                            proj_fm(wif[0], "wif0", tb * 512, ps, pn, m=4)
                            c.op("act", lambda ps=ps, ts_=ts_: nc.scalar.activation(out=LI[:, ts_], in_=ps[0:4, :], func=AF.Identity, bias=gb[:, 0:1]),
                                 reads=[pn, "gb"], writes=["LI"])
                            ps, pn = c.psf()
                            proj_fm(wif[1], "wif1", tb * 512, ps, pn, m=4)
                            c.op("act", lambda ps=ps, ts_=ts_: nc.scalar.activation(out=LF[:, ts_], in_=ps[0:4, :], func=AF.Exp, scale=-1.0, bias=gb[:, 1:2]),
                                 reads=[pn, "gb"], writes=["LF"])
                        c.op("act", lambda: nc.scalar.activation(out=LF[:], in_=LF[:], func=AF.Ln, bias=1.0), reads=["LF"], writes=["LF"])
                        c.dma("sp", gsc[0], LI[:], reads=["LI"], writes=["D:gsc0"], sem="k5")
                        c.dma("sp", gsc[1], LF[:], reads=["LF"], writes=["D:gsc1"], sem="k6")
                        LIf = c.sb(st, "LIf", [64, 128], F32)
                        CS = c.sb(st, "CS", [64, 128], F32)
                        c.dma("sp", LIf[:], gsc[0].rearrange("h (n l) -> (h n) l", l=128), reads=["D:gsc0"], writes=["LIf"], sem="k5")
                        c.dma("sp", CS[:], gsc[1].rearrange("h (n l) -> (h n) l", l=128), reads=["D:gsc1"], writes=["CS"], sem="k6")
                        c.op("dve", lambda: nc.vector.tensor_tensor_scan(out=CS[:], data0=CS[:], data1=CS[:], initial=0.0, op0=ALU.add, op1=ALU.bypass),
                             reads=["CS"], writes=["CS"])
                        Afm = c.sb(st, "Afm", [64, 128], F32)
                        CM = c.sb(st, "CM", [64, 128], F32)
                        c.op("dve", lambda: nc.vector.tensor_tensor(out=Afm[:], in0=LIf[:], in1=CS[:], op=ALU.add), reads=["LIf", "CS"], writes=["Afm"])
                        c.op("dve", lambda: nc.vector.tensor_tensor_scan(out=CM[:], data0=Afm[:], data1=Afm[:], initial=-1e30, op0=ALU.max, op1=ALU.bypass),
                             reads=["Afm"], writes=["CM"])
                        st2 = c.sb(st, "st2", [64, 2], F32)
                        c.op("dve", lambda: nc.vector.tensor_scalar(out=st2[:, 0:1], in0=CS[:, 127:128], scalar1=-1.0, scalar2=None, op0=ALU.mult),
                             reads=["CS"], writes=["st2"])
                        c.op("dve", lambda: nc.vector.tensor_copy(out=st2[:, 1:2], in_=CM[:, 127:128]), reads=["CM"], writes=["st2"])
                        c.dma("sp", gs2, st2[:], reads=["st2"], writes=["D:gs2"], sem="k5")
                        GT = c.sb(st, "GT", [4, 16, 2], F32)
                        c.dma("sp", GT[:], gs2.rearrange("(h n) k -> h n k", n=16), reads=["D:gs2"], writes=["GT"], sem="k5")
                        ML = c.sb(st, "ML", [4, 16], F32)
                        MP = c.sb(st, "MP", [4, 17], F32)
                        c.op("dve", lambda: nc.vector.tensor_tensor(out=ML[:], in0=GT[:, :, 0], in1=GT[:, :, 1], op=ALU.add), reads=["GT"], writes=["ML"])
                        c.op("dve", lambda: nc.vector.memset(MP[:], 0.0), writes=["MP"])
                        for n in range(16):
                            c.op("dve", lambda n=n: nc.vector.scalar_tensor_tensor(
                                out=MP[:, n + 1:n + 2], in0=MP[:, n:n + 1], scalar=GT[:, n, 0:1], in1=ML[:, n:n + 1],
                                op0=ALU.add, op1=ALU.max), reads=["MP", "GT", "ML"], writes=["MP"])
                        SC = c.sb(st, "SC", [4, 16, 5], F32)
                        tq = c.sb(st, "tq", [4, 16], F32)
                        c.op("dve", lambda: nc.vector.tensor_tensor(out=tq[:], in0=GT[:, :, 0], in1=MP[:, 0:16], op=ALU.add), reads=["GT", "MP"], writes=["tq"])
                        c.op("dve", lambda: nc.vector.tensor_tensor(out=tq[:], in0=tq[:], in1=MP[:, 1:17], op=ALU.subtract), reads=["tq", "MP"], writes=["tq"])
                        c.op("act", lambda: nc.scalar.activation(out=SC[:, :, 0], in_=tq[:], func=AF.Exp), reads=["tq"], writes=["SC"])
                        tq2 = c.sb(st, "tq2", [4, 16], F32)
                        c.op("dve", lambda: nc.vector.tensor_tensor(out=tq2[:], in0=ML[:], in1=MP[:, 1:17], op=ALU.subtract), reads=["ML", "MP"], writes=["tq2"])
                        c.op("act", lambda: nc.scalar.activation(out=SC[:, :, 1], in_=tq2[:], func=AF.Exp), reads=["tq2"], writes=["SC"])
                        tq3 = c.sb(st, "tq3", [4, 16], F32)
                        c.op("dve", lambda: nc.vector.tensor_tensor(out=tq3[:], in0=MP[:, 0:16], in1=GT[:, :, 1], op=ALU.subtract), reads=["GT", "MP"], writes=["tq3"])
                        c.op("act", lambda: nc.scalar.activation(out=SC[:, :, 2], in_=tq3[:], func=AF.Exp), reads=["tq3"], writes=["SC"])
                        c.op("dve", lambda: nc.vector.tensor_copy(out=SC[:, :, 3], in_=GT[:, :, 1]), reads=["GT", "SC"], writes=["SC"])
                        c.op("dve", lambda: nc.vector.tensor_copy(out=SC[:, :, 4], in_=MP[:, 0:16]), reads=["MP", "SC"], writes=["SC"])
                        c.dma("sp", gs3, SC[:], reads=["SC"], writes=["D:gs3"], sem="k6")
                        SCc = c.sb(st, "SCc", [64, 5], F32)
                        c.dma("sp", SCc[:], gs3.rearrange("h n k -> (h n) k"), reads=["D:gs3"], writes=["SCc"], sem="k6")
                        c.dma("sp", SCb[:].rearrange("p j k -> p (j k)"),
                              gs3.rearrange("h n k -> (h n k)").rearrange("(o f) -> o f", o=1).partition_broadcast(128),
                              reads=["D:gs3"], writes=["SCb"], sem="k5")
                        Mfm = c.sb(st, "Mfm", [64, 128], F32)
                        c.op("dve", lambda: nc.vector.tensor_scalar(out=Mfm[:], in0=CM[:], scalar1=SCc[:, 4:5], scalar2=None, op0=ALU.max),
                             reads=["CM", "SCc"], writes=["Mfm"])
                        bcol = c.sb(st, "bcol", [64, 2], F32)
                        c.op("dve", lambda: nc.vector.tensor_scalar(out=bcol[:, 0:1], in0=SCc[:, 3:4], scalar1=float(math.log(128 ** -0.5)), scalar2=None, op0=ALU.add),
                             reads=["SCc"], writes=["bcol"])
                        c.op("dve", lambda: nc.vector.tensor_scalar(out=bcol[:, 1:2], in0=SCc[:, 3:4], scalar1=-1.0, scalar2=None, op0=ALU.mult),
                             reads=["SCc", "bcol"], writes=["bcol"])
                        W1f = c.sb(st, "W1f", [64, 128], F32)
                        ELf = c.sb(st, "ELf", [64, 128], F32)
                        EMf = c.sb(st, "EMf", [64, 128], F32)
                        c.op("act", lambda: nc.scalar.activation(out=W1f[:], in_=Mfm[:], func=AF.Exp, scale=-1.0, bias=bcol[:, 0:1]),
                             reads=["Mfm", "bcol"], writes=["W1f"])
                        c.op("act", lambda: nc.scalar.activation(out=ELf[:], in_=Afm[:], func=AF.Exp, bias=bcol[:, 1:2]),
                             reads=["Afm", "bcol"], writes=["ELf"])
                        c.op("dve", lambda: nc.vector.tensor_tensor(out=EMf[:], in0=CS[:], in1=Mfm[:], op=ALU.subtract), reads=["CS", "Mfm"], writes=["EMf"])
                        c.op("act", lambda: nc.scalar.activation(out=EMf[:], in_=EMf[:], func=AF.Exp), reads=["EMf"], writes=["EMf"])
                        c.dma("sp", gs4[0], W1f[:], reads=["W1f"], writes=["D:gs40"], sem="k5")
                        c.dma("sp", gs4[1], ELf[:], reads=["ELf"], writes=["D:gs41"], sem="k6")
                        ps, pn = c.psf()
                        mm(ps[:, 0:64], EMf[:], identf[0:64, 0:64], True, True, ["EMf", "identf"], [pn])
                        c.op("dve", lambda ps=ps: nc.vector.tensor_copy(out=EMT[:], in_=ps[:, 0:64]), reads=[pn], writes=["EMT"])

                        c.barrier()
                        stg.close()
                        st = st_outer
                        phase_done("mlg")
                        XP = c.sb(st, "XP", [128, 3 + S], F32)
                        acc = c.sb(st, "acc", [128, S], F32)
                        CW = c.sb(st, "CW", [128, 8, 5], F32)
                        for qk in range(2):
                            for h in range(4):
                                c0 = qk * 512 + h * 128
                                c.dma("sp", CW[:, qk * 4 + h, 0:4], conv_wT[l, c0:c0 + 128, :], writes=["CW"], sem="k0")
                                c.dma("sp", CW[:, qk * 4 + h, 4:5], conv_b[l, c0:c0 + 128].rearrange("(p o) -> p o", o=1), writes=["CW"], sem="k0")
                        c.op("dve", lambda: nc.vector.memset(XP[:, 0:3], 0.0), writes=["XP"])
                        QcT = c.sb(st, "QcT", [128, S], BF16)
                        KcT = c.sb(st, "KcT", [128, S], BF16)
                        QS = c.sb(st, "QS", [128, S], BF16)
                        KST = c.sb(st, "KST", [128, S], BF16)
                        KSm = c.sb(st, "KSm", [128, 16, 128], BF16)
                        VA = c.sb(st, "VA", [128, 16, 2, 257], BF16)
                        SGO = c.sb(st, "SGO", [128, 16, 512], BF16)
                        sgt = c.sb(st, "sgt", [128, 512], F32)
                        CA = c.sb(st, "CA", [128, 257], F32)
                        CLs = c.sb(st, "CLs", [128, 15, 257], F32)
                        Cb = c.sb(st, "Cb", [128, 16, 257], BF16)
                        PTm = [c.sb(st, f"PTm{i}", [128, 4, 128], BF16) for i in range(2)]
                        sc1 = c.sb(st, "sc1", [128, 4], F32)
                        junk2 = c.sb(st, "junk2", [128, 256], F32)
                        Ym = [c.sb(st, f"Ym{i}", [128, 256], BF16) for i in range(2)]
                        YTm = [c.sb(st, "YTm0", [128, 2, S], BF16)]
                        c.op("dve", lambda: nc.vector.memset(VA[:, :, :, 256:257], 1.0), writes=["VA"])
                        wtm = WStream(c, st, "wtm", [128, 16, 512], 1,
                                      [W[l]["mv"][0], W[l]["mo"][0], W[l]["mv"][1], W[l]["mo"][1]])
                        wfm = WStream(c, st, "wfm", [128, 16, 128], 3,
                                      [W[l][k][h] for h in range(4) for k in ("mq", "mk")])
                        ml_pend = []

                        def ml_epilogue(n, pN, pNn, hn, h, hl, cs, ytm, ytmn):
                            c.op("act", lambda pN=pN: nc.scalar.activation(
                                out=sc1[:, 0:1], in_=pN[:, 256:257], func=AF.Abs), reads=[pNn], writes=["sc1"])
                            c.op("dve", lambda hn=hn: nc.vector.tensor_tensor(
                                out=sc1[:, 0:1], in0=sc1[:, 0:1], in1=EMT[:, hn:hn + 1], op=ALU.max), reads=["sc1", "EMT"], writes=["sc1"])
                            c.op("dve", lambda: nc.vector.reciprocal(out=sc1[:, 0:1], in_=sc1[:, 0:1]), reads=["sc1"], writes=["sc1"])
                            c.op("act", lambda pN=pN: nc.scalar.activation(out=junk2[:], in_=pN[:, 0:256], func=AF.Square, accum_out=sc1[:, 1:2]),
                                 reads=[pNn, "sc1"], writes=["junk2", "sc1"])
                            c.op("dve", lambda: nc.vector.scalar_tensor_tensor(
                                out=sc1[:, 2:3], in0=sc1[:, 0:1], scalar=sc1[:, 0:1], in1=sc1[:, 1:2], op0=ALU.mult, op1=ALU.mult),
                                reads=["sc1"], writes=["sc1"])
                            c.op("act", lambda: nc.scalar.activation(out=sc1[:, 2:3], in_=sc1[:, 2:3], func=AF.Sqrt, scale=1.0 / 256, bias=EPS),
                                 reads=["sc1"], writes=["sc1"])
                            c.op("dve", lambda: nc.vector.reciprocal(out=sc1[:, 2:3], in_=sc1[:, 2:3]), reads=["sc1"], writes=["sc1"])
                            c.op("dve", lambda: nc.vector.tensor_tensor(out=sc1[:, 3:4], in0=sc1[:, 2:3], in1=sc1[:, 0:1], op=ALU.mult),
                                 reads=["sc1"], writes=["sc1"])
                            ym = Ym[n % 2]
                            ymn = f"Ym{n % 2}"
                            c.op("dve", lambda pN=pN, ym=ym, n=n, hl=hl: nc.vector.scalar_tensor_tensor(
                                out=ym[:], in0=pN[:, 0:256], scalar=sc1[:, 3:4], in1=SGO[:, n, hl * 256:(hl + 1) * 256],
                                op0=ALU.mult, op1=ALU.mult), reads=[pNn, "sc1", "SGO"], writes=[ymn])
                            pb, pbn = transpose_bf(None, lambda jj, ym=ym: ym[:, jj * 128:(jj + 1) * 128], 2, [ymn], None)
                            c.op("act", lambda pb=pb, ytm=ytm, cs=cs: nc.scalar.activation(
                                out=ytm[:, :, cs], in_=pb[:, 0:256].rearrange("p (a b) -> p a b", a=2), func=AF.Copy),
                                reads=[pbn], writes=[ytmn])
                            if n == 15:
                                c.dma("sp", ybuf[1, 2 * h:2 * h + 2].rearrange("e p t -> p e t"), ytm[:], reads=[ytmn],
                                      writes=[f"D:yb1_{2 * h}", f"D:yb1_{2 * h + 1}"], sem=ytmn)

                        c.reserved = {2, 3}
                        for hp in range(2):
                            if ml_pend:
                                ml_pend.pop()()
                            wv, wvn = wtm.next(ahead=0)
                            for tl in range(16):
                                ps, pn = c.psf()
                                for k in range(16):
                                    mm(ps[:], A[:, k, tl * 128:(tl + 1) * 128], wv[:, k, :], k == 0, k == 15, ["A", wvn], [pn])
                                c.op("act", lambda ps=ps, tl=tl: nc.scalar.activation(
                                    out=VA[:, tl, :, 0:256], in_=ps[:].rearrange("p (a b) -> p a b", a=2), func=AF.Copy),
                                    reads=[pn], writes=["VA"])
                            wo, won = wtm.next(ahead=0)
                            for tl in range(16):
                                ps, pn = c.psf()
                                for k in range(16):
                                    mm(ps[:], A[:, k, tl * 128:(tl + 1) * 128], wo[:, k, :], k == 0, k == 15, ["A", won], [pn])
                                c.op("act", lambda ps=ps: nc.scalar.activation(out=sgt[:], in_=ps[:], func=AF.Sigmoid),
                                     reads=[pn], writes=["sgt"])
                                c.op("dve", lambda tl=tl, hp=hp: nc.vector.tensor_tensor(
                                    out=SGO[:, tl, :], in0=sgt[:], in1=NWB[:, hp * 512:(hp + 1) * 512], op=ALU.mult),
                                    reads=["sgt", "NWB"], writes=["SGO"])
                            for hl in range(2):
                                h = hp * 2 + hl
                                for qk, dstT, dn in ((0, QcT, "QcT"), (1, KcT, "KcT")):
                                    wt, wn = wfm.next()
                                    ci = qk * 4 + h
                                    for tb in range(4):
                                        ps, pn = c.psf()
                                        proj_fm(wt, wn, tb * 512, ps, pn)
                                        c.op("act", lambda ps=ps, tb=tb: nc.scalar.activation(
                                            out=XP[:, 3 + tb * 512: 3 + (tb + 1) * 512], in_=ps[:], func=AF.Copy),
                                            reads=[pn], writes=["XP"])
                                    c.op("dve", lambda ci=ci: nc.vector.tensor_scalar(
                                        out=acc[:], in0=XP[:, 3:3 + S], scalar1=CW[:, ci, 3:4], scalar2=None, op0=ALU.mult),
                                        reads=["XP", "CW"], writes=["acc"])
                                    for j in (2, 1, 0):
                                        c.op("dve", lambda ci=ci, j=j: nc.vector.scalar_tensor_tensor(
                                            out=acc[:], in0=XP[:, j:j + S], scalar=CW[:, ci, j:j + 1], in1=acc[:],
                                            op0=ALU.mult, op1=ALU.add), reads=["XP", "CW", "acc"], writes=["acc"])
                                    c.op("act", lambda ci=ci, dstT=dstT: nc.scalar.activation(
                                        out=dstT[:], in_=acc[:], func=AF.Silu, bias=CW[:, ci, 4:5]),
                                        reads=["acc", "CW"], writes=[dn])
                                c.dma("sp", acc[:], gs4[0, h * 16:(h + 1) * 16, :].rearrange("n l -> (n l)").rearrange("(o f) -> o f", o=1).partition_broadcast(128),
                                      reads=["D:gs40"], writes=["acc"], sem="k1")
                                c.dma("sp", XP[:, 3:3 + S], gs4[1, h * 16:(h + 1) * 16, :].rearrange("n l -> (n l)").rearrange("(o f) -> o f", o=1).partition_broadcast(128),
                                      reads=["D:gs41"], writes=["XP"], sem="k2")
                                c.op("dve", lambda: nc.vector.tensor_tensor(out=QS[:], in0=QcT[:], in1=acc[:], op=ALU.mult),
                                     reads=["QcT", "acc"], writes=["QS"])
                                c.op("dve", lambda: nc.vector.tensor_tensor(out=KST[:], in0=KcT[:], in1=XP[:, 3:3 + S], op=ALU.mult),
                                     reads=["KcT", "XP"], writes=["KST"])
                                for cg in range(4):
                                    pb, pbn = transpose_bf(None, lambda j, cg=cg: KST[:, (cg * 4 + j) * 128:(cg * 4 + j + 1) * 128], 4, ["KST"], None)
                                    c.op("act", lambda pb=pb, cg=cg: nc.scalar.activation(
                                        out=KSm[:, cg * 4:(cg + 1) * 4, :], in_=pb[:, 0:512].rearrange("p (j k) -> p j k", j=4), func=AF.Copy),
                                        reads=[pbn], writes=["KSm"])
                                for n in range(15):
                                    hn = h * 16 + n
                                    pC, pCn = c.psf()
                                    mm(pC[:, 0:257], KSm[:, n, :], VA[:, n, hl, :], True, True, ["KSm", "VA"], [pCn])
                                    c.op("act", lambda pC=pC, n=n, hn=hn: nc.scalar.activation(
                                        out=CLs[:, n, :], in_=pC[:, 0:257], func=AF.Copy, scale=SCb[:, hn, 1:2]),
                                        reads=[pCn, "SCb"], writes=["CLs"], par=True)
                                for n in range(15):
                                    hn = h * 16 + n
                                    if n == 0:
                                        c.op("dve", lambda: nc.vector.tensor_copy(out=CA[:], in_=CLs[:, 0, :]), reads=["CLs"], writes=["CA"])
                                    else:
                                        c.op("dve", lambda n=n, hn=hn: nc.vector.scalar_tensor_tensor(
                                            out=CA[:], in0=CA[:], scalar=SCb[:, hn, 0:1], in1=CLs[:, n, :], op0=ALU.mult, op1=ALU.add),
                                            reads=["CA", "SCb", "CLs"], writes=["CA"])
                                    c.op("act", lambda n=n, hn=hn: nc.scalar.activation(
                                        out=Cb[:, n + 1, :], in_=CA[:], func=AF.Copy, scale=SCb[:, hn + 1, 2:3]),
                                        reads=["CA", "SCb"], writes=["Cb"], par=True)
                                ytm = YTm[0]
                                ytmn = "YTm0"
                                for cg in range(4):
                                    pS, pSn = c.psf()
                                    for j in range(4):
                                        n = cg * 4 + j
                                        cs = slice(n * 128, (n + 1) * 128)
                                        c.op("pe", lambda j=j, cs=cs, pS=pS: nc.tensor.matmul(
                                            pS[:, j * 128:(j + 1) * 128], KST[:, cs], QS[:, cs], start=True, stop=True),
                                            reads=["KST", "QS"], writes=[pSn], inc=(j == 3))
                                    pt = PTm[cg % 2]
                                    ptn = f"PTm{cg % 2}"
                                    c.op("dve", lambda pS=pS, pt=pt: nc.vector.tensor_tensor(
                                        out=pt[:], in0=pS[:].rearrange("p (j k) -> p j k", j=4),
                                        in1=MASK[:].unsqueeze(1).to_broadcast([128, 4, 128]), op=ALU.mult),
                                        reads=[pSn, "MASK"], writes=[ptn])
                                    for j in range(4):
                                        n = cg * 4 + j
                                        hn = h * 16 + n
                                        cs = slice(n * 128, (n + 1) * 128)
                                        pN, pNn = c.psf_at(2 + n % 2)
                                        mm(pN[:, 0:257], pt[:, j, :], VA[:, n, hl, :], True, n == 0, [ptn, "VA"], [pNn])
                                        if n > 0:
                                            mm(pN[:, 0:257], QS[:, cs], Cb[:, n, :], False, True, ["QS", "Cb"], [pNn])
                                        if ml_pend:
                                            ml_pend.pop()()
                                        ml_pend.append(lambda n=n, pN=pN, pNn=pNn, hn=hn, h=h, hl=hl, cs=cs, ytm=ytm, ytmn=ytmn:
                                                       ml_epilogue(n, pN, pNn, hn, h, hl, cs, ytm, ytmn))
                        if ml_pend:
                            ml_pend.pop()()
                        c.reserved = set()
                        c.barrier()
                        phase_done("ml")

                    with CleanStack() as st:
                        if "mb" in skip:
                            raise _SkipPhase()
                        BIAS = c.sb(st, "BIAS", [128, 8, 2, 128], F32)
                        CB = c.sb(st, "CB", [128, 8], F32)
                        PMK = c.sb(st, "PMK", [128, 16, 8], F32)
                        PIK = c.sb(st, "PIK", [128, 16, 8], F32)
                        c.dma("sp", BIAS[:], mb_bias, writes=["BIAS"], sem="k0")
                        c.dma("sp", CB[:], rel_bias[31:32, :].partition_broadcast(128), writes=["CB"], sem="k1")
                        c.dma("sp", PMK[:], c_pm, writes=["PMK"], sem="k2")
                        c.dma("sp", PIK[:], c_pi, writes=["PIK"], sem="k3")
                        QT = c.sb(st, "QT", [128, S], BF16)
                        QTf = c.sb(st, "QTf", [128, S], F32)
                        KT = c.sb(st, "KT", [128, S], BF16)
                        KM = c.sb(st, "KM", [128, 8], F32)
                        KMh = c.sb(st, "KMh", [128, 8], BF16)
                        KMl = c.sb(st, "KMl", [128, 8], BF16)
                        QL = c.sb(st, "QL", [128, S], BF16)
                        VB = c.sb(st, "VB", [128, 16, 4, 129], BF16)
                        GM = c.sb(st, "GM", [128, 16, 8], F32)
                        MX = c.sb(st, "MX", [128, 16, 8], F32)
                        SEL = c.sb(st, "SEL", [128, 16, 8], F32)
                        ACC = c.sb(st, "ACC", [128, 16, 129], F32)
                        REC = c.sb(st, "REC", [128, 16], F32)
                        PTb = [c.sb(st, f"PTb{i}", [128, 512], BF16) for i in range(4)]
                        tb_ = c.sb(st, "tb_", [128, 128], F32)
                        Yb = c.sb(st, "Yb", [128, 16, 128], BF16)
                        YTb = [c.sb(st, f"YTb{i}", [128, S], BF16) for i in range(2)]
                        c.op("dve", lambda: nc.vector.memset(VB[:, :, :, 128:129], 1.0), writes=["VB"])
                        wtm = WStream(c, st, "wtm", [128, 16, 512], 2, [W[l]["bv"][0], W[l]["bv"][1]])
                        wfm = WStream(c, st, "wfm", [128, 16, 128], 3,
                                      [W[l][k][h] for h in range(8) for k in ("bq", "bk")])
                        ipt = [0]
                        for hg in range(2):
                            wv, wvn = wtm.next()
                            for tl in range(16):
                                ps, pn = c.psf()
                                for k in range(16):
                                    mm(ps[:], A[:, k, tl * 128:(tl + 1) * 128], wv[:, k, :], k == 0, k == 15, ["A", wvn], [pn])
                                c.op("act", lambda ps=ps, tl=tl: nc.scalar.activation(
                                    out=VB[:, tl, :, 0:128], in_=ps[:].rearrange("p (a b) -> p a b", a=4), func=AF.Copy),
                                    reads=[pn], writes=["VB"])
                            if stop == "mb_v":
                                c.barrier()
                                phase_done("mb_v")
                            for hl in range(4):
                                h = hg * 4 + hl
                                wt, wn = wfm.next()
                                for tb in range(4):
                                    ts_ = slice(tb * 512, (tb + 1) * 512)
                                    ps, pn = c.psf()
                                    proj_fm(wt, wn, tb * 512, ps, pn)
                                    c.op("act", lambda ps=ps, ts_=ts_: nc.scalar.activation(out=QTf[:, ts_], in_=ps[:], func=AF.Copy, scale=float(128 ** -0.5)),
                                         reads=[pn], writes=["QTf"])
                                    c.op("dve", lambda ts_=ts_: nc.vector.tensor_copy(out=QT[:, ts_], in_=QTf[:, ts_]), reads=["QTf"], writes=["QT"])
                                wt, wn = wfm.next()
                                for tb in range(4):
                                    ts_ = slice(tb * 512, (tb + 1) * 512)
                                    ps, pn = c.psf()
                                    proj_fm(wt, wn, tb * 512, ps, pn)
                                    c.op("act", lambda ps=ps, ts_=ts_: nc.scalar.activation(out=KT[:, ts_], in_=ps[:], func=AF.Copy),
                                         reads=[pn], writes=["KT"])
                                    c.op("dve", lambda ps=ps, tb=tb: nc.vector.reduce_sum(
                                        out=KM[:, 2 * tb:2 * tb + 2], in_=ps[:].rearrange("p (a b) -> p a b", a=2), axis=AX.X),
                                        reads=[pn], writes=["KM"])
                                c.op("dve", lambda: nc.vector.tensor_scalar(out=KM[:], in0=KM[:], scalar1=1.0 / 256, scalar2=None, op0=ALU.mult),
                                     reads=["KM"], writes=["KM"])
                                if stop == "mb_km":
                                    c.barrier()
                                    phase_done("mb_km")
                                c.op("dve", lambda: nc.vector.tensor_tensor(out=QL[:], in0=QTf[:], in1=QT[:], op=ALU.subtract),
                                     reads=["QTf", "QT"], writes=["QL"])
                                c.op("dve", lambda: nc.vector.tensor_copy(out=KMh[:], in_=KM[:]), reads=["KM"], writes=["KMh"])
                                c.op("dve", lambda: nc.vector.tensor_tensor(out=KMl[:], in0=KM[:], in1=KMh[:], op=ALU.subtract),
                                     reads=["KM", "KMh"], writes=["KMl"])
                                pG, pGn = c.psf()
                                for t in range(16):
                                    tsl = slice(t * 128, (t + 1) * 128)
                                    mm(pG[:, t * 8:(t + 1) * 8], QT[:, tsl], KMh[:], True, False, ["QT", "KMh"], [pGn])
                                    mm(pG[:, t * 8:(t + 1) * 8], QT[:, tsl], KMl[:], False, False, ["QT", "KMl"], [pGn])
                                    mm(pG[:, t * 8:(t + 1) * 8], QL[:, tsl], KMh[:], False, True, ["QL", "KMh"], [pGn])
                                c.op("dve", lambda pG=pG: nc.vector.tensor_tensor(
                                    out=GM[:], in0=pG[:, 0:128].rearrange("p (t j) -> p t j", t=16), in1=PMK[:], op=ALU.add),
                                    reads=[pGn, "PMK"], writes=["GM"])
                                if stop == "mb_gm":
                                    c.barrier()
                                    phase_done("mb_gm")
                                for t in range(16):
                                    c.op("dve", lambda t=t: nc.vector.max(out=MX[:, t, :], in_=GM[:, t, :]), reads=["GM"], writes=["MX"])
                                for t in range(16):
                                    c.op("dve", lambda t=t: nc.vector.scalar_tensor_tensor(
                                        out=SEL[:, t, :], in0=GM[:, t, :], scalar=MX[:, t, 2:3], in1=PIK[:, t, :], op0=ALU.is_ge, op1=ALU.mult),
                                        reads=["GM", "MX", "PIK"], writes=["SEL"])
                                if stop == "mb_sel":
                                    c.barrier()
                                    phase_done("mb_sel")
                                c.op("dve", lambda: nc.vector.memset(ACC[:], 0.0), writes=["ACC"])
                                def mb_stage_a(g, j):
                                    pts = {}
                                    for kt in (2 * j, 2 * j + 1):
                                        t_lo = max(kt, 4 * g)
                                        if t_lo > 4 * g + 3:
                                            continue
                                        ncol = (4 * g + 4 - t_lo) * 128
                                        q0 = t_lo * 128
                                        pS, pSn = c.psf()
                                        mm(pS[:, 0:ncol], KT[:, kt * 128:(kt + 1) * 128], QT[:, q0:q0 + ncol], True, True, ["KT", "QT"], [pSn])
                                        pt = PTb[ipt[0] % 4]
                                        ptn = f"PTb{ipt[0] % 4}"
                                        ipt[0] += 1
                                        tcur = t_lo
                                        while tcur <= 4 * g + 3:
                                            o0 = (tcur - t_lo) * 128
                                            if tcur - kt <= 1:
                                                kind = tcur - kt
                                                c.op("dve", lambda pS=pS, o0=o0, kind=kind: nc.vector.tensor_tensor(
                                                    out=tb_[:], in0=pS[:, o0:o0 + 128], in1=BIAS[:, h, kind, :], op=ALU.add),
                                                    reads=[pSn, "BIAS"], writes=["tb_"])
                                                c.op("act", lambda pt=pt, o0=o0: nc.scalar.activation(out=pt[:, o0:o0 + 128], in_=tb_[:], func=AF.Exp),
                                                     reads=["tb_"], writes=[ptn])
                                                tcur += 1
                                            else:
                                                o1 = (4 * g + 4 - t_lo) * 128
                                                c.op("act", lambda pt=pt, pS=pS, o0=o0, o1=o1: nc.scalar.activation(
                                                    out=pt[:, o0:o1], in_=pS[:, o0:o1], func=AF.Exp, bias=CB[:, h:h + 1]),
                                                    reads=[pSn, "CB"], writes=[ptn])
                                                tcur = 4 * g + 4
                                        pts[kt] = (pt, ptn, t_lo)
                                    return pts

                                def mb_stage_b(g, j, pts):
                                    for half in range(2):
                                        tq_ = [t for t in (4 * g + 2 * half, 4 * g + 2 * half + 1) if t >= 2 * j]
                                        if not tq_:
                                            continue
                                        pO, pOn = c.psf()
                                        for ti, t in enumerate(tq_):
                                            kts = [kt for kt in pts if kt <= t]
                                            for ki, kt in enumerate(kts):
                                                pt, ptn, t_lo = pts[kt]
                                                o0 = (t - t_lo) * 128
                                                mm(pO[:, ti * 256:ti * 256 + 129], pt[:, o0:o0 + 128], VB[:, kt, hl, :],
                                                   ki == 0, ki == len(kts) - 1, [ptn, "VB"], [pOn])
                                        for ti, t in enumerate(tq_):
                                            own = (j == t // 2)
                                            sc = 1.0 if own else SEL[:, t, j:j + 1]
                                            c.op("dve", lambda pO=pO, ti=ti, t=t, sc=sc: nc.vector.scalar_tensor_tensor(
                                                out=ACC[:, t, :], in0=pO[:, ti * 256:ti * 256 + 129], scalar=sc, in1=ACC[:, t, :],
                                                op0=ALU.mult, op1=ALU.add), reads=[pOn, "SEL", "ACC"], writes=["ACC"])

                                blocks = [(g, j) for g in range(4) for j in range(2 * g + 2)]
                                nxt = mb_stage_a(*blocks[0])
                                for bi, (g, j) in enumerate(blocks):
                                    cur = nxt
                                    if bi + 1 < len(blocks):
                                        nxt = mb_stage_a(*blocks[bi + 1])
                                    mb_stage_b(g, j, cur)
                                if stop == "mb_att":
                                    c.barrier()
                                    phase_done("mb_att")
                                c.op("dve", lambda: nc.vector.reciprocal(out=REC[:], in_=ACC[:, :, 128]), reads=["ACC"], writes=["REC"])
                                c.op("dve", lambda: nc.vector.tensor_tensor(
                                    out=Yb[:], in0=ACC[:, :, 0:128], in1=REC[:].unsqueeze(2).to_broadcast([128, 16, 128]), op=ALU.mult),
                                    reads=["ACC", "REC"], writes=["Yb"])
                                ytb = YTb[h % 2]
                                ytbn = f"YTb{h % 2}"
                                for cg in range(4):
                                    pb, pbn = transpose_bf(None, lambda jj, cg=cg: Yb[:, cg * 4 + jj, :], 4, ["Yb"], None)
                                    c.op("act", lambda pb=pb, cg=cg, ytb=ytb: nc.scalar.activation(
                                        out=ytb[:, cg * 512:(cg + 1) * 512], in_=pb[:, 0:512], func=AF.Copy), reads=[pbn], writes=[ytbn])
                                c.dma("sp", ybuf[2, h], ytb[:], reads=[ytbn], writes=[f"D:yb2_{h}"], sem=ytbn)
                        c.barrier()
                        phase_done("mb")

                    if l == 0 and sq == 0:
                        flush_casts(["D:yb2_7"])
                    if dbg and l == 0 and sq == 0:
                        c.dma("sp", dbg_y, ybuf, reads=[f"D:yb{i}_{e}" for i in range(3) for e in range(8)], writes=["D:dbg_y"], sem="k0")

                    with CleanStack() as st:
                        YB = c.sb(st, "YB", [128, 24, 512], BF16)
                        MXT = c.sb(st, "MXT", [128, 16, 512], BF16)
                        XB = c.sb(st, "XB", [128, 16, 512], F32)
                        sqb = c.sb(st, "sqb", [128, 16, 512], BF16)
                        rs = c.sb(st, "rs", [128, 512], F32)
                        sg = [c.sb(st, f"sg{i}", [128, 512], F32) for i in range(3)]
                        pr = [c.sb(st, f"pr{i}", [128, 512], F32) for i in range(2)]
                        ggroups = []
                        bgroups = []
                        ogroups = []
                        for tb in range(4):
                            for dc in range(16):
                                for i in range(3):
                                    ggroups.append(W[l]["g"][i * 16 + dc])
                                    bgroups.append(W[l][f"br{i}"][dc])
                            for dc in range(16):
                                ogroups.append(W[l]["out"][dc])
                        wg_s = WStream(c, st, "wg", [128, 16, 128], 3, ggroups)
                        wb_s = WStream(c, st, "wbr", [128, 8, 128], 3, bgroups)
                        wo_s = WStream(c, st, "wo", [128, 16, 128], 2, ogroups)
                        for tb in range(4):
                            t0 = tb * 512
                            blk = (tok0 + t0) // 512
                            for i in range(3):
                                c.dma("sp", YB[:, i * 8:(i + 1) * 8, :], ybuf[i, :, :, t0:t0 + 512].rearrange("e p t -> p e t"),
                                      reads=[f"D:yb{i}_{e}" for e in range(8)], writes=["YB"], sem=f"YB{i}", par=True)
                            c.dma("sp", XB[:], xTv[:, :, tok0 + t0: tok0 + t0 + 512], reads=[f"D:xT{blk}"], writes=["XB"], sem="XB")
                            for dc in range(16):
                                for i in range(3):
                                    wt, wn = wg_s.next()
                                    pg, pgn = c.psf()
                                    proj_fm(wt, wn, t0, pg, pgn)
                                    c.op("act", lambda pg=pg, i=i: nc.scalar.activation(out=sg[i][:], in_=pg[:], func=AF.Sigmoid),
                                         reads=[pgn], writes=[f"sg{i}"])
                                    wt, wn = wb_s.next()
                                    pbr, pbrn = c.psf()
                                    for k in range(8):
                                        mm(pbr[:], wt[:, k, :], YB[:, i * 8 + k, :], k == 0, k == 7, [wn, "YB"], [pbrn])
                                    if i == 0:
                                        c.op("dve", lambda pbr=pbr: nc.vector.tensor_tensor(out=pr[0][:], in0=pbr[:], in1=sg[0][:], op=ALU.mult),
                                             reads=[pbrn, "sg0"], writes=["pr0"])
                                    else:
                                        c.op("dve", lambda pbr=pbr, i=i: nc.vector.tensor_tensor(out=pr[1][:], in0=pbr[:], in1=sg[i][:], op=ALU.mult),
                                             reads=[pbrn, f"sg{i}"], writes=["pr1"])
                                        if i == 1:
                                            c.op("dve", lambda: nc.vector.tensor_tensor(out=pr[0][:], in0=pr[0][:], in1=pr[1][:], op=ALU.add),
                                                 reads=["pr0", "pr1"], writes=["pr0"])
                                        else:
                                            c.op("dve", lambda dc=dc: nc.vector.tensor_tensor(out=MXT[:, dc, :], in0=pr[0][:], in1=pr[1][:], op=ALU.add),
                                                 reads=["pr0", "pr1"], writes=["MXT"])
                            for dc in range(16):
                                wt, wn = wo_s.next()
                                po, pon = c.psf()
                                for k in range(16):
                                    mm(po[:], wt[:, k, :], MXT[:, k, :], k == 0, k == 15, [wn, "MXT"], [pon])
                                c.op("dve", lambda po=po, dc=dc: nc.vector.tensor_tensor(out=XB[:, dc, :], in0=XB[:, dc, :], in1=po[:], op=ALU.add),
                                     reads=[pon, "XB"], writes=["XB"])
                            c.dma("sp", xTv[:, :, tok0 + t0: tok0 + t0 + 512], XB[:], reads=["XB"], writes=[f"D:xT{blk}"], sem="XBo")
                            norm_block((sqb, rs), XB, "XB", 2 * l + 1, lambda k, t0=t0: A[:, k, t0:t0 + 512], "A")
                        c.barrier()
                        phase_done("c1")

                    if dbg and l == 0 and sq == 0:
                        c.dma("sp", dbg_x1, xT[:, :, 0:S], reads=[f"D:xT{b}" for b in range(4)], writes=["D:dbg_x1"], sem="k0")

                    with CleanStack() as st:
                        UT = c.sb(st, "UT", [128, 64, 512], BF16)
                        sq_ = [c.sb(st, f"sq_{i}", [128, 512], F32) for i in range(2)]
                        xr = [c.sb(st, f"xr{i}", [128, 512], F32) for i in range(2)]
                        g1 = []
                        g2 = []
                        for tb in range(4):
                            for fc in range(64):
                                g1.append(W[l]["ff1"][fc])
                            for dc in range(16):
                                g2.append(W[l]["ff2"][dc])
                        w1_s = WStream(c, st, "w1", [128, 16, 128], 3, g1)
                        w2_s = WStream(c, st, "w2", [128, 64, 128], 3, g2)
                        for tb in range(4):
                            t0 = tb * 512
                            blk = (tok0 + t0) // 512
                            for fc in range(64):
                                wt, wn = w1_s.next()
                                ps, pn = c.psf()
                                proj_fm(wt, wn, t0, ps, pn)
                                s_ = sq_[fc % 2]
                                sn = f"sq_{fc % 2}"
                                c.op("act", lambda ps=ps, s_=s_: nc.scalar.activation(out=s_[:], in_=ps[:], func=AF.Square), reads=[pn], writes=[sn])
                                c.op("dve", lambda ps=ps, s_=s_, fc=fc: nc.vector.scalar_tensor_tensor(
                                    out=UT[:, fc, :], in0=ps[:], scalar=0.0, in1=s_[:], op0=ALU.is_gt, op1=ALU.mult),
                                    reads=[pn, sn], writes=["UT"])
                            for dc in range(16):
                                wt, wn = w2_s.next()
                                b = dc % 2
                                c.dma("sp", xr[b][:], xT[dc, :, tok0 + t0: tok0 + t0 + 512], reads=[f"D:xT{blk}"], writes=[f"xr{b}"], sem=f"xr{b}")
                                ps, pn = c.psf()
                                for k in range(64):
                                    mm(ps[:], wt[:, k, :], UT[:, k, :], k == 0, k == 63, [wn, "UT"], [pn])
                                c.op("dve", lambda ps=ps, b=b: nc.vector.tensor_tensor(out=xr[b][:], in0=xr[b][:], in1=ps[:], op=ALU.add),
                                     reads=[pn, f"xr{b}"], writes=[f"xr{b}"])
                                c.dma("sp", xT[dc, :, tok0 + t0: tok0 + t0 + 512], xr[b][:], reads=[f"xr{b}"], writes=[f"D:xT{blk}"], sem=f"xr{b}", par=True)
                        c.barrier()
                        phase_done("ffn")

                    if dbg and l == 0 and sq == 0:
                        c.dma("sp", dbg_x2, xT[:, :, 0:S], reads=[f"D:xT{b}" for b in range(4)], writes=["D:dbg_x2"], sem="k0")

            with CleanStack() as st:
                XB = c.sb(st, "XB", [128, 16, 512], F32)
                XN = c.sb(st, "XN", [128, 16, 512], F32)
                sqb = c.sb(st, "sqb", [128, 16, 512], BF16)
                rs = c.sb(st, "rs", [128, 512], F32)
                ot = [c.sb(st, f"ot{i}", [128, D], F32) for i in range(2)]
                for blk in range(T // 512):
                    c.dma("sp", XB[:], xTv[:, :, blk * 512:(blk + 1) * 512], reads=[f"D:xT{blk}"], writes=["XB"], sem="XB")
                    norm_block((sqb, rs), XB, "XB", 2 * DEPTH, lambda k: XN[:, k, :], "XN")
                    for tl in range(4):
                        b = tl % 2
                        for q4 in range(4):
                            ps, pn = c.psf()
                            for j in range(4):
                                dcx = q4 * 4 + j
                                c.op("pe", lambda j=j, dcx=dcx, ps=ps, tl=tl: nc.tensor.transpose(
                                    ps[:, j * 128:(j + 1) * 128], XN[:, dcx, tl * 128:(tl + 1) * 128], identf[:]),
                                    reads=["XN", "identf"], writes=[pn], inc=(j == 3))
                            if q4 % 2 == 0:
                                c.op("act", lambda ps=ps, b=b, q4=q4: nc.scalar.activation(out=ot[b][:, q4 * 512:(q4 + 1) * 512], in_=ps[:], func=AF.Copy),
                                     reads=[pn], writes=[f"ot{b}"], par=True)
                            else:
                                c.op("dve", lambda ps=ps, b=b, q4=q4: nc.vector.tensor_copy(out=ot[b][:, q4 * 512:(q4 + 1) * 512], in_=ps[:]),
                                     reads=[pn], writes=[f"ot{b}"], par=True)
                        r0 = blk * 512 + tl * 128
                        c.dma("sp", out[r0:r0 + 128, :], ot[b][:], reads=[f"ot{b}"], writes=[f"D:out{blk}_{tl}"], sem=f"ot{b}")
        try:
            body()
        except _Stop:
            c.barrier()
            if dbg:
                c.dma("sp", dbg_y, ybuf, reads=[f"D:yb{i}_{e}" for i in range(3) for e in range(8)], writes=["D:dbg_y"], sem="k0")
                c.dma("sp", dbg_x1, xT[:, :, 0:S], reads=[f"D:xT{b}" for b in range(4)], writes=["D:dbg_x1"], sem="k1")
        c.finish("sp")
    print("instructions:", c.ninst, "sems:", len(c.sem), flush=True)
    return nc


def _host_inputs(inputs, cst):
    rb = np.asarray(inputs["rel_bias"], np.float32)
    b0 = np.where(cst["_m0"][None], rb[cst["_b0"]].transpose(2, 0, 1), np.float32(NEG))
    b1 = rb[cst["_b1"]].transpose(2, 0, 1)
    mb = np.stack([b0, b1], 1)
    shared = {
        "w_in": np.ascontiguousarray(inputs["w_in"], np.float32),
        "w_branch_ret": np.ascontiguousarray(inputs["w_branch_ret"], np.float32),
        "w_branch_mlstm": np.ascontiguousarray(inputs["w_branch_mlstm"], np.float32),
        "w_branch_moba": np.ascontiguousarray(inputs["w_branch_moba"], np.float32),
        "w_out": np.ascontiguousarray(inputs["w_out"], np.float32),
        "w_ff1": np.ascontiguousarray(inputs["w_ff1"], np.float32),
        "w_ff2": np.ascontiguousarray(inputs["w_ff2"], np.float32),
        "mlstm_gate_b": np.ascontiguousarray(inputs["mlstm_gate_b"], np.float32),
        "conv_wT": np.ascontiguousarray(np.asarray(inputs["mlstm_conv_w"], np.float32).transpose(0, 2, 1)),
        "mlstm_conv_b": np.ascontiguousarray(inputs["mlstm_conv_b"], np.float32),
        "mlstm_norm_w": np.ascontiguousarray(inputs["mlstm_norm_w"], np.float32),
        "nmixT": np.ascontiguousarray(np.asarray(inputs["norm_mix_w"], np.float32).reshape(DEPTH, 16, 128).transpose(0, 2, 1)),
        "nmlpT": np.ascontiguousarray(np.asarray(inputs["norm_mlp_w"], np.float32).reshape(DEPTH, 16, 128).transpose(0, 2, 1)),
        "nfinT": np.ascontiguousarray(np.asarray(inputs["final_norm_w"], np.float32).reshape(16, 128).T),
        "rel_bias": np.ascontiguousarray(rb),
        "mb_bias": np.ascontiguousarray(mb.transpose(2, 0, 1, 3)).astype(np.float32),
    }
    for k, v in cst.items():
        if not k.startswith("_"):
            shared[k] = v
    return shared


def kernel(**inputs):
    cst = _constants()
    shared = _host_inputs(inputs, cst)
    x = np.asarray(inputs["x"], np.float32)
    B = x.shape[0]
    per = B // NCORES
    nc = build_nc(n_seq=per, depth=DEPTH, gl=cst["_gl"])
    in_maps = []
    for i in range(NCORES):
        m = dict(shared)
        m["x"] = np.ascontiguousarray(x[i * per:(i + 1) * per].reshape(per * S, D))
        in_maps.append(m)
    res = run_bass_kernel_spmd(nc, in_maps, core_ids=list(range(NCORES)))
    outs = [np.asarray(r["out"], np.float32).reshape(per, S, D) for r in res.results]
    return np.concatenate(outs, axis=0)
```

```python
import math
from contextlib import ExitStack
import numpy as np
import concourse.bass as bass
import concourse.mybir as mybir
from concourse.bass_utils import run_bass_kernel_spmd

F32 = mybir.dt.float32
BF16 = mybir.dt.bfloat16
AF = mybir.ActivationFunctionType
ALU = mybir.AluOpType
AX = mybir.AxisListType

D = 2048
S = 2048
NCORES = 8
DEPTH = 2
D_IN = 16392
D_FF = 8192
EPS = 1e-6
O_RQ, O_RK, O_RV, O_RG = 0, 1024, 2048, 3072
O_MQ, O_MK, O_MV, O_MO, O_MI, O_MF = 4096, 4608, 5120, 6144, 7168, 7172
O_BQ, O_BK, O_BV, O_G = 7176, 8200, 9224, 10248
NEG = -30000.0


class Ctx:
    def __init__(self, nc):
        self.nc = nc
        self.es = ExitStack()
        self.eng = {"pe": nc.tensor, "act": nc.scalar, "dve": nc.vector, "pool": nc.gpsimd, "sp": nc.sync}
        self.sem = {}
        self.cnt = {}
        for k in self.eng:
            self.sem[k] = self.es.enter_context(nc.semaphore("s_" + k))
            self.cnt[k] = 0
        self.waited = {k: {} for k in self.eng}
        self.res = {}
        self.ninst = {k: 0 for k in self.eng}
        self._psf = []
        self._psb = []
        self._pi = 0
        self._pb = 0

    def sb(self, st, name, shape, dt):
        self._uid = getattr(self, "_uid", 0) + 1
        return st.enter_context(self.nc.sbuf_tensor(f"{name}_u{self._uid}", list(shape), dt))

    def dsem(self, key):
        if key not in self.sem:
            self.sem[key] = self.es.enter_context(self.nc.semaphore("d_" + key))
            self.cnt[key] = 0
            assert len(self.sem) <= 100, "too many semaphores"
        return key

    def _r(self, name):
        r = self.res.get(name)
        if r is None:
            r = {"w": {}, "r": {}}
            self.res[name] = r
        return r

    def _wait(self, e, deps):
        m = {}
        for d in deps:
            if d is None:
                continue
            k, v = d
            if v > m.get(k, 0):
                m[k] = v
        for k, v in m.items():
            if k == "pe" and e == "pe":
                continue
            if self.waited[e].get(k, 0) >= v:
                continue
            self.eng[e].wait_ge(self.sem[k], v)
            self.waited[e][k] = v

    def _deps(self, reads, writes, par=False, e=None):
        deps = []
        for r in reads:
            deps.extend(self._r(r)["w"].items())
            if r.startswith("ps"):
                deps.extend((k, v) for k, v in self._r(r)["r"].items() if k != e)
        for w in writes:
            rr = self._r(w)
            if not par:
                deps.extend(rr["w"].items())
            deps.extend(rr["r"].items())
        return deps

    def _commit(self, ticket, reads, writes, par=False):
        k, v = ticket
        for r in reads:
            rr = self._r(r)
            if rr["r"].get(k, 0) < v:
                rr["r"][k] = v
        for w in writes:
            rr = self._r(w)
            if par:
                if rr["w"].get(k, 0) < v:
                    rr["w"][k] = v
            else:
                rr["w"] = {k: v}
                rr["r"] = {}

    def op(self, e, fn, reads=(), writes=(), inc=True, par=False):
        self._wait(e, self._deps(reads, writes, par, e))
        ins = fn()
        self.ninst[e] += 1
        if inc:
            ins.then_inc(self.sem[e], 1)
            self.cnt[e] += 1
            ticket = (e, self.cnt[e])
        else:
            assert e == "pe"
            ticket = (e, self.cnt[e] + 1)
        self._commit(ticket, reads, writes, par)
        return ticket

    def dma(self, q, out, in_, reads=(), writes=(), sem=None, par=False, **kw):
        key = self.dsem(sem)
        deps = self._deps(reads, writes, par)
        if self.cnt[key] > 0:
            deps.append((key, self.cnt[key]))
        self._wait(q, deps)
        ins = self.eng[q].dma_start(out=out, in_=in_, **kw)
        ins.then_inc(self.sem[key], 16)
        self.ninst[q] += 1
        self.cnt[key] += 16
        ticket = (key, self.cnt[key])
        self._commit(ticket, reads, writes, par)
        return ticket

    def barrier(self, engines=("pe", "act", "dve", "sp")):
        deps = []
        for rr in self.res.values():
            deps.extend(rr["w"].items())
            deps.extend(rr["r"].items())
        deps = [d for d in deps if d is not None and not str(d[0]).startswith("cast")]
        for e in engines:
            self._wait(e, deps)
        for n in list(self.res.keys()):
            if not n.startswith("D:"):
                del self.res[n]

    def finish(self, e="sp"):
        deps = []
        for rr in self.res.values():
            deps.extend(rr["w"].items())
            deps.extend(rr["r"].items())
        self._wait(e, deps)

    def psf(self):
        while True:
            i = self._pi % len(self._psf)
            self._pi += 1
            if i not in getattr(self, "reserved", ()):
                return self._psf[i], f"psf{i}"

    def psf_at(self, i):
        return self._psf[i], f"psf{i}"

    def psb(self):
        i = self._pb % len(self._psb)
        self._pb += 1
        return self._psb[i], f"psb{i}"


class WStream:
    def __init__(self, c, st, name, shape, nbuf, groups):
        self.c = c
        self.name = name
        self.nbuf = nbuf
        self.groups = groups
        self.tiles = [c.sb(st, f"{name}{i}", shape, BF16) for i in range(nbuf)]
        self.il = 0
        self.iu = 0

    def _load(self):
        ap, deps = self.groups[self.il]
        slot = self.il % self.nbuf
        self.c.dma("sp", self.tiles[slot][:], ap, reads=deps, writes=[f"{self.name}{slot}"], sem=f"{self.name}{slot}")
        self.il += 1

    def next(self, ahead=None):
        if ahead is None:
            ahead = self.nbuf - 1
        while self.il < min(self.iu + 1 + ahead, len(self.groups)):
            self._load()
        slot = self.iu % self.nbuf
        self.iu += 1
        return self.tiles[slot], f"{self.name}{slot}"


def _t5_bucket(dist):
    n = np.maximum(dist, 0)
    exact = 16
    nf = np.maximum(n, 1).astype(np.float32)
    large = exact + (np.log(nf / np.float32(exact)) / np.float32(math.log(128 / exact)) * np.float32(16)).astype(np.int32)
    large = np.minimum(large, 31)
    return np.where(n < exact, n, large)


def _constants():
    cst = {}
    half = 64
    inv = (np.float32(10000.0) ** (-np.arange(half, dtype=np.float32) / np.float32(half))).astype(np.float32)
    pos = np.arange(S, dtype=np.float32)
    ang = (pos[:, None] * inv[None, :]).astype(np.float32)
    cos = np.cos(ang).astype(np.float32).T
    sin = np.sin(ang).astype(np.float32).T
    cst["c_cos"] = np.ascontiguousarray(np.concatenate([cos, cos], 0))
    cst["c_sin"] = np.ascontiguousarray(np.concatenate([-sin, sin], 0))
    H = 8
    L = 128
    lg = np.log1p(-np.exp2(-5.0 - np.arange(H, dtype=np.float64)))
    idx = np.arange(L, dtype=np.float64)
    scale = 128 ** -0.5
    diff = idx[None, :] - idx[:, None]
    dt = np.where(diff[None] >= 0, np.exp(np.maximum(diff[None], 0) * lg[:, None, None]), 0.0) * scale
    cst["c_dt"] = np.ascontiguousarray(dt.transpose(1, 0, 2)).astype(np.float32)
    xi = np.exp((idx + 1.0)[None, :] * lg[:, None]) * scale
    cst["c_xi"] = np.ascontiguousarray(np.broadcast_to(xi[None], (128, H, L))).astype(np.float32)
    zeta = np.exp((L - 1 - idx)[None, :] * lg[:, None])
    cst["c_zeta"] = np.ascontiguousarray(zeta.T).astype(np.float32)
    gl = np.exp(L * lg)
    cst["_gl"] = [float(np.float32(v)) for v in gl]
    g16 = np.zeros((128, H, 16), np.float32)
    g16[:, :, 1:] = gl.astype(np.float32)[None, :, None]
    cst["c_g16"] = g16
    cst["c_mask"] = (idx[:, None] <= idx[None, :]).astype(np.float32)
    cst["c_ident"] = np.eye(128, dtype=np.float32)
    pm = np.zeros((128, 16, 8), np.float32)
    pi = np.zeros((128, 16, 8), np.float32)
    for t in range(16):
        own = t // 2
        for j in range(8):
            if j < own:
                pi[:, t, j] = 1.0
            else:
                pm[:, t, j] = -1e30
    cst["c_pm"] = pm
    cst["c_pi"] = pi
    k = np.arange(128)[:, None]
    q = np.arange(128)[None, :]
    cst["_b0"] = _t5_bucket(q - k)
    cst["_m0"] = (k <= q)
    cst["_b1"] = _t5_bucket(q - k + 128)
    return cst


class _Stop(Exception):
    pass


class _SkipPhase(Exception):
    pass


class CleanStack(ExitStack):
    def __exit__(self, *exc):
        super().__exit__(None, None, None)
        return bool(exc and exc[0] is _SkipPhase)


def build_nc(n_seq=2, depth=DEPTH, dbg=False, gl=None, stop=None, skip=()):
    T = n_seq * S
    nc = bass.Bass("TRN2", target_bir_lowering=False)

    def din(name, shape, dt=F32):
        return nc.dram_tensor(name, list(shape), dt, kind="ExternalInput").ap()

    x_in = din("x", [T, D])
    w_in = din("w_in", [DEPTH, D, D_IN])
    w_br = [din("w_branch_ret", [DEPTH, 1024, D]), din("w_branch_mlstm", [DEPTH, 1024, D]),
            din("w_branch_moba", [DEPTH, 1024, D])]
    w_out = din("w_out", [DEPTH, D, D])
    w_ff1 = din("w_ff1", [DEPTH, D, D_FF])
    w_ff2 = din("w_ff2", [DEPTH, D_FF, D])
    gate_b = din("mlstm_gate_b", [DEPTH, 2, 4])
    conv_wT = din("conv_wT", [DEPTH, 1024, 4])
    conv_b = din("mlstm_conv_b", [DEPTH, 1024])
    ml_nw = din("mlstm_norm_w", [DEPTH, 1024])
    nmix = din("nmixT", [DEPTH, 128, 16])
    nmlp = din("nmlpT", [DEPTH, 128, 16])
    nfin = din("nfinT", [128, 16])
    rel_bias = din("rel_bias", [32, 8])
    mb_bias = din("mb_bias", [128, 8, 2, 128])
    c_cos = din("c_cos", [128, S])
    c_sin = din("c_sin", [128, S])
    c_dt = din("c_dt", [128, 8, 128])
    c_xi = din("c_xi", [128, 8, 128])
    c_zeta = din("c_zeta", [128, 8])
    c_g16 = din("c_g16", [128, 8, 16])
    c_mask = din("c_mask", [128, 128])
    c_ident = din("c_ident", [128, 128])
    c_pm = din("c_pm", [128, 16, 8])
    c_pi = din("c_pi", [128, 16, 8])
    out = nc.dram_tensor("out", [T, D], F32, kind="ExternalOutput").ap()

    def dscr(name, shape, dt):
        return nc.dram_tensor(name, list(shape), dt, kind="Internal").ap()

    xT = dscr("xT", [16, 128, T], F32)
    ybuf = dscr("ybuf", [3, 8, 128, S], BF16)
    gsc = dscr("gsc", [2, 4, S], F32)
    gs2 = dscr("gs2", [64, 2], F32)
    gs3 = dscr("gs3", [4, 16, 5], F32)
    gs4 = dscr("gs4", [2, 64, 128], F32)
    if dbg:
        dbg_y = nc.dram_tensor("dbg_y", [3, 8, 128, S], BF16, kind="ExternalOutput").ap()
        dbg_x1 = nc.dram_tensor("dbg_x1", [16, 128, S], F32, kind="ExternalOutput").ap()
        dbg_x2 = nc.dram_tensor("dbg_x2", [16, 128, S], F32, kind="ExternalOutput").ap()

    c = Ctx(nc)
    ncast = [0]
    cast_q = [[]]

    def flush_casts(wait_names=()):
        deps = []
        for n in wait_names:
            deps.extend(c._r(n)["w"].items())
        if deps:
            c._wait("pool", deps)
        for q in cast_q:
            for f in q:
                f()
        cast_q.clear()
        cast_q.append([])

    def cast_family(name, src2d, K, ncols, gw, col0=0):
        kc = K // 128
        G = ncols // gw
        dst = dscr("wb_" + name, [G, 128, kc, gw], BF16)
        srcv = src2d.rearrange("(kc p) n -> p kc n", p=128)
        groups = []
        for g in range(G):
            deps = []
            for k0 in range(0, kc, 16):
                k1 = min(kc, k0 + 16)
                rn = f"D:wb_{name}_{g}_{k0}"
                def emit(g=g, k0=k0, k1=k1, rn=rn):
                    c.dma("pool", dst[g][:, k0:k1, :], srcv[:, k0:k1, col0 + g * gw: col0 + (g + 1) * gw],
                          writes=[rn], sem=f"cast{ncast[0] % 16}")
                    ncast[0] += 1
                cast_q[-1].append(emit)
                deps.append(rn)
            groups.append((dst[g], deps))
        return groups

    with c.es:
        gst = c.es
        for i in range(6):
            c._psf.append(gst.enter_context(nc.psum_tensor(f"psf{i}", [128, 512], F32)))
        for i in range(2):
            c._psb.append(gst.enter_context(nc.psum_tensor(f"psb{i}", [128, 1024], BF16)))
        A = c.sb(gst, "A", [128, 16, S], BF16)
        identf = c.sb(gst, "identf", [128, 128], F32)
        identb = c.sb(gst, "identb", [128, 128], BF16)
        onesb = c.sb(gst, "onesb", [128, 128], BF16)
        nw_all = c.sb(gst, "nw_all", [128, 2 * DEPTH + 1, 16], F32)

        c.dma("sp", identf[:], c_ident, writes=["identf"], sem="k0")
        c.op("dve", lambda: nc.vector.tensor_copy(out=identb[:], in_=identf[:]), reads=["identf"], writes=["identb"])
        c.op("dve", lambda: nc.vector.memset(onesb[:], 1.0), writes=["onesb"])
        for l in range(DEPTH):
            c.dma("sp", nw_all[:, 2 * l, :], nmix[l], writes=["nw_all"], sem="k0")
            c.dma("sp", nw_all[:, 2 * l + 1, :], nmlp[l], writes=["nw_all"], sem="k0")
        c.dma("sp", nw_all[:, 2 * DEPTH, :], nfin, writes=["nw_all"], sem="k0")

        W = []
        for l in range(depth):
            wl = {}
            wi = w_in[l]
            wl["rv"] = cast_family(f"rv{l}", wi, D, 1024, 512, O_RV)
            wl["rg"] = cast_family(f"rg{l}", wi, D, 1024, 512, O_RG)
            wl["rq"] = cast_family(f"rq{l}", wi, D, 1024, 128, O_RQ)
            wl["rk"] = cast_family(f"rk{l}", wi, D, 1024, 128, O_RK)
            if l == 0:
                flush_casts()
            wl["mi"] = cast_family(f"mi{l}", wi, D, 4, 4, O_MI)
            wl["mf"] = cast_family(f"mf{l}", wi, D, 4, 4, O_MF)
            wl["mv"] = cast_family(f"mv{l}", wi, D, 1024, 512, O_MV)
            wl["mo"] = cast_family(f"mo{l}", wi, D, 1024, 512, O_MO)
            wl["mq"] = cast_family(f"mq{l}", wi, D, 512, 128, O_MQ)
            wl["mk"] = cast_family(f"mk{l}", wi, D, 512, 128, O_MK)
            wl["bv"] = cast_family(f"bv{l}", wi, D, 1024, 512, O_BV)
            wl["bq"] = cast_family(f"bq{l}", wi, D, 1024, 128, O_BQ)
            wl["bk"] = cast_family(f"bk{l}", wi, D, 1024, 128, O_BK)
            wl["g"] = cast_family(f"g{l}", wi, D, 6144, 128, O_G)
            for i in range(3):
                wl[f"br{i}"] = cast_family(f"br{i}_{l}", w_br[i][l], 1024, D, 128)
            wl["out"] = cast_family(f"out{l}", w_out[l], D, D, 128)
            wl["ff1"] = cast_family(f"ff1_{l}", w_ff1[l], D, D_FF, 128)
            wl["ff2"] = cast_family(f"ff2_{l}", w_ff2[l], D_FF, D, 128)
            W.append(wl)
            if l == 0:
                cast_q.append([])

        def mm(o, lhsT, rhs, start, stop, reads, writes):
            return c.op("pe", lambda: nc.tensor.matmul(o, lhsT, rhs, start=start, stop=stop),
                        reads=reads, writes=writes, inc=bool(stop))

        def proj_fm(wt, wn, t0, ps, pn, kc=16, src=None, srcn="A", m=128):
            srcT = A if src is None else src
            for k in range(kc):
                mm(ps[0:m, :], wt[:, k, 0:m], srcT[:, k, t0:t0 + 512], k == 0, k == kc - 1, [wn, srcn], [pn])

        def transpose_bf(dst_fn, src_fn, n, reads, dst_reads_writes):
            pb, pbn = c.psb()
            for j in range(n):
                c.op("pe", lambda j=j: nc.tensor.transpose(pb[:, j * 128:(j + 1) * 128], src_fn(j), identb[:]),
                     reads=list(reads) + ["identb"], writes=[pbn], inc=(j == n - 1))
            return pb, pbn

        def norm_block(st_tiles, xblk, xn, nwi, dst, dstn):
            sqb, rs = st_tiles
            c.op("act", lambda: nc.scalar.activation(out=sqb[:], in_=xblk[:], func=AF.Square), reads=[xn], writes=["sqb"])
            ps, pn = c.psf()
            for k in range(16):
                mm(ps[:], onesb[:], sqb[:, k, :], k == 0, k == 15, ["onesb", "sqb"], [pn])
            c.op("act", lambda: nc.scalar.activation(out=rs[:], in_=ps[:], func=AF.Sqrt, scale=1.0 / D, bias=EPS),
                 reads=[pn], writes=["rs"])
            c.op("dve", lambda: nc.vector.reciprocal(out=rs[:], in_=rs[:]), reads=["rs"], writes=["rs"])
            for k in range(16):
                c.op("dve", lambda k=k: nc.vector.scalar_tensor_tensor(
                    out=dst(k), in0=xblk[:, k, :], scalar=nw_all[:, nwi, k:k + 1], in1=rs[:],
                    op0=ALU.mult, op1=ALU.mult), reads=[xn, "rs", "nw_all"], writes=[dstn], par=True)

        xTv = xT.rearrange("dc p t -> p dc t")

        with CleanStack() as st:
            xt = [c.sb(st, f"xt{i}", [128, D], F32) for i in range(2)]
            xs = [c.sb(st, f"xs{i}", [128, 16, 128], F32) for i in range(2)]
            for tt in range(T // 128):
                b = tt % 2
                c.dma("sp", xt[b][:], x_in[tt * 128:(tt + 1) * 128, :], writes=[f"xt{b}"], sem=f"xt{b}")
                for q4 in range(4):
                    ps, pn = c.psf()
                    for j in range(4):
                        dcx = q4 * 4 + j
                        c.op("pe", lambda j=j, dcx=dcx, ps=ps: nc.tensor.transpose(
                            ps[:, j * 128:(j + 1) * 128], xt[b][:, dcx * 128:(dcx + 1) * 128], identf[:]),
                            reads=[f"xt{b}", "identf"], writes=[pn], inc=(j == 3))
                    eng = "act" if q4 % 2 == 0 else "dve"
                    dstap = xs[b][:, q4 * 4:(q4 + 1) * 4, :]
                    if eng == "act":
                        c.op("act", lambda ps=ps, dstap=dstap: nc.scalar.activation(
                            out=dstap, in_=ps[:].rearrange("p (j k) -> p j k", j=4), func=AF.Copy),
                            reads=[pn], writes=[f"xs{b}"], par=True)
                    else:
                        c.op("dve", lambda ps=ps, dstap=dstap: nc.vector.tensor_copy(
                            out=dstap, in_=ps[:].rearrange("p (j k) -> p j k", j=4)),
                            reads=[pn], writes=[f"xs{b}"], par=True)
                blk = tt // 4
                c.dma("sp", xTv[:, :, tt * 128:(tt + 1) * 128], xs[b][:], reads=[f"xs{b}"], writes=[f"D:xT{blk}"],
                      sem=f"xs{b}", par=True)
            c.barrier()

        gl = gl or _constants()["_gl"]

        def phase_done(name):
            if stop == name:
                raise _Stop()

        later = cast_q[1:] if len(cast_q) > 1 else []
        del cast_q[1:]
        flush_casts([f"D:xT{b}" for b in range(T // 512)])
        for q in later:
            cast_q[-1].extend(q)

        def body():
            phase_done("p0")
            for l in range(depth):
                for sq in range(n_seq):
                    tok0 = sq * S
                    with CleanStack() as st:
                        xb = [c.sb(st, f"xb{i}", [128, 16, 512], F32) for i in range(2)]
                        sqb = c.sb(st, "sqb", [128, 16, 512], BF16)
                        rs = c.sb(st, "rs", [128, 512], F32)
                        for tb in range(4):
                            b = tb % 2
                            blk = (tok0 + tb * 512) // 512
                            c.dma("sp", xb[b][:], xTv[:, :, tok0 + tb * 512: tok0 + (tb + 1) * 512],
                                  reads=[f"D:xT{blk}"], writes=[f"xb{b}"], sem=f"xb{b}")
                            norm_block((sqb, rs), xb[b], f"xb{b}", 2 * l,
                                       lambda k, tb=tb: A[:, k, tb * 512:(tb + 1) * 512], "A")
                        c.barrier()
                        phase_done("n1")

                    with CleanStack() as st:
                        if "ret" in skip:
                            raise _SkipPhase()
                        COS = c.sb(st, "COS", [128, S], F32)
                        SIN = c.sb(st, "SIN", [128, S], F32)
                        DT = c.sb(st, "DT", [128, 8, 128], F32)
                        XI = c.sb(st, "XI", [128, 8, 128], F32)
                        ZETA = c.sb(st, "ZETA", [128, 8], F32)
                        c.dma("sp", COS[:], c_cos, writes=["COS"], sem="k0")
                        c.dma("sp", SIN[:], c_sin, writes=["SIN"], sem="k1")
                        c.dma("sp", DT[:], c_dt, writes=["DT"], sem="k2")
                        c.dma("sp", XI[:], c_xi, writes=["XI"], sem="k3")
                        c.dma("sp", ZETA[:], c_zeta, writes=["ZETA"], sem="k4")
                        QT = c.sb(st, "QT", [128, S], BF16)
                        KT = c.sb(st, "KT", [128, S], BF16)
                        QX = c.sb(st, "QX", [128, S], BF16)
                        KZ = c.sb(st, "KZ", [128, 16, 128], BF16)
                        VV = c.sb(st, "VV", [128, 16, 512], BF16)
                        SG = c.sb(st, "SG", [128, 16, 512], BF16)
                        t1 = c.sb(st, "t1", [128, 512], F32)
                        t2 = c.sb(st, "t2", [128, 512], F32)
                        G16 = c.sb(st, "G16", [128, 8, 16], F32)
                        c.dma("sp", G16[:], c_g16, writes=["G16"], sem="k5")
                        KVs = c.sb(st, "KVs", [128, 128, 16], F32)
                        GMUL = c.sb(st, "GMUL", [128, 128, 16], F32)
                        Rbn = c.sb(st, "Rbn", [128, 16, 128], BF16)
                        c.op("dve", lambda: nc.vector.memset(KVs[:, :, 0:1], 0.0), writes=["KVs"])
                        PT = [c.sb(st, f"PT{i}", [128, 4, 128], BF16) for i in range(2)]
                        junk = c.sb(st, "junk", [128, 512], F32)
                        ss4 = c.sb(st, "ss4", [128, 4], F32)
                        yn = c.sb(st, "yn", [128, 4, 128], F32)
                        Yst = [c.sb(st, f"Yst{i}", [128, 4, 128], BF16) for i in range(2)]
                        YT = [c.sb(st, f"YT{i}", [128, S], BF16) for i in range(2)]
                        wtm = WStream(c, st, "wtm", [128, 16, 512], 1,
                                      [W[l]["rv"][0], W[l]["rg"][0], W[l]["rv"][1], W[l]["rg"][1]])
                        wfm = WStream(c, st, "wfm", [128, 16, 128], 3,
                                      [W[l][k][h] for h in range(8) for k in ("rq", "rk")])
                        ret_pend = []

                        def ret_epilogue(cg, pO, pOn, h, hc, yt, ytn):
                            c.op("act", lambda pO=pO: nc.scalar.activation(out=junk[:], in_=pO[:], func=AF.Square),
                                 reads=[pOn], writes=["junk"])
                            c.op("dve", lambda: nc.vector.reduce_sum(out=ss4[:], in_=junk[:].rearrange("p (j k) -> p j k", j=4), axis=AX.X),
                                 reads=["junk"], writes=["ss4"])
                            c.op("act", lambda: nc.scalar.activation(out=ss4[:], in_=ss4[:], func=AF.Sqrt, scale=1.0 / 128, bias=EPS),
                                 reads=["ss4"], writes=["ss4"])
                            c.op("dve", lambda: nc.vector.reciprocal(out=ss4[:], in_=ss4[:]), reads=["ss4"], writes=["ss4"])
                            c.op("dve", lambda pO=pO: nc.vector.tensor_tensor(
                                out=yn[:], in0=pO[:].rearrange("p (j k) -> p j k", j=4),
                                in1=ss4[:].unsqueeze(2).to_broadcast([128, 4, 128]), op=ALU.mult),
                                reads=[pOn, "ss4"], writes=["yn"])
                            ys = Yst[cg % 2]
                            ysn = f"Yst{cg % 2}"
                            c.op("dve", lambda ys=ys, cg=cg: nc.vector.tensor_tensor(
                                out=ys[:], in0=yn[:], in1=SG[:, cg * 4:(cg + 1) * 4, hc], op=ALU.mult),
                                reads=["yn", "SG"], writes=[ysn])
                            pb, pbn = transpose_bf(None, lambda j, ys=ys: ys[:, j, :], 4, [ysn], None)
                            c.op("act", lambda pb=pb, cg=cg, yt=yt: nc.scalar.activation(
                                out=yt[:, cg * 512:(cg + 1) * 512], in_=pb[:, 0:512], func=AF.Copy),
                                reads=[pbn], writes=[ytn])
                            if cg == 3:
                                c.dma("sp", ybuf[0, h], yt[:], reads=[ytn], writes=[f"D:yb0_{h}"], sem=ytn)

                        c.reserved = {2, 3}
                        for hg in range(2):
                            if ret_pend:
                                ret_pend.pop()()
                            wv, wvn = wtm.next(ahead=0)
                            for tl in range(16):
                                ps, pn = c.psf()
                                for k in range(16):
                                    mm(ps[:], A[:, k, tl * 128:(tl + 1) * 128], wv[:, k, :], k == 0, k == 15, ["A", wvn], [pn])
                                c.op("act", lambda ps=ps, tl=tl: nc.scalar.activation(out=VV[:, tl, :], in_=ps[:], func=AF.Copy),
                                     reads=[pn], writes=["VV"])
                            wg, wgn = wtm.next(ahead=0)
                            for tl in range(16):
                                ps, pn = c.psf()
                                for k in range(16):
                                    mm(ps[:], A[:, k, tl * 128:(tl + 1) * 128], wg[:, k, :], k == 0, k == 15, ["A", wgn], [pn])
                                c.op("act", lambda ps=ps, tl=tl: nc.scalar.activation(out=SG[:, tl, :], in_=ps[:], func=AF.Silu),
                                     reads=[pn], writes=["SG"])
                            for hl in range(4):
                                h = hg * 4 + hl
                                hc = slice(hl * 128, (hl + 1) * 128)
                                for dstT, dn in ((QT, "QT"), (KT, "KT")):
                                    wt, wn = wfm.next()
                                    for tb in range(4):
                                        ts_ = slice(tb * 512, (tb + 1) * 512)
                                        ps, pn = c.psf()
                                        proj_fm(wt, wn, tb * 512, ps, pn)
                                        c.op("dve", lambda ps=ps, ts_=ts_: nc.vector.tensor_tensor(
                                            out=t1[0:64, :], in0=ps[64:128, :], in1=SIN[0:64, ts_], op=ALU.mult),
                                            reads=[pn, "SIN"], writes=["t1"])
                                        c.op("dve", lambda ps=ps, ts_=ts_: nc.vector.tensor_tensor(
                                            out=t1[64:128, :], in0=ps[0:64, :], in1=SIN[64:128, ts_], op=ALU.mult),
                                            reads=[pn, "SIN"], writes=["t1"])
                                        c.op("dve", lambda ps=ps, ts_=ts_: nc.vector.tensor_tensor(
                                            out=t2[:], in0=ps[:], in1=COS[:, ts_], op=ALU.mult),
                                            reads=[pn, "COS"], writes=["t2"])
                                        c.op("dve", lambda dstT=dstT, ts_=ts_: nc.vector.tensor_tensor(
                                            out=dstT[:, ts_], in0=t1[:], in1=t2[:], op=ALU.add),
                                            reads=["t1", "t2"], writes=[dn])
                                c.op("dve", lambda h=h: nc.vector.tensor_tensor(
                                    out=QX[:].rearrange("p (n l) -> p n l", n=16), in0=QT[:].rearrange("p (n l) -> p n l", n=16),
                                    in1=XI[:, h:h + 1, :].to_broadcast([128, 16, 128]), op=ALU.mult),
                                    reads=["QT", "XI"], writes=["QX"])
                                for cg in range(4):
                                    pb, pbn = transpose_bf(None, lambda j, cg=cg: KT[:, (cg * 4 + j) * 128:(cg * 4 + j + 1) * 128], 4, ["KT"], None)
                                    c.op("act", lambda pb=pb, cg=cg, h=h: nc.scalar.activation(
                                        out=KZ[:, cg * 4:(cg + 1) * 4, :], in_=pb[:, 0:512].rearrange("p (j k) -> p j k", j=4),
                                        func=AF.Copy, scale=ZETA[:, h:h + 1]), reads=[pbn, "ZETA"], writes=["KZ"])
                                for cgk in range(4):
                                    nk = 4 if cgk < 3 else 3
                                    pK, pKn = c.psf()
                                    for j in range(nk):
                                        n = cgk * 4 + j
                                        c.op("pe", lambda j=j, n=n, pK=pK: nc.tensor.matmul(
                                            pK[:, j * 128:(j + 1) * 128], KZ[:, n, :], VV[:, n, hc], start=True, stop=True),
                                            reads=["KZ", "VV"], writes=[pKn], inc=(j == nk - 1))
                                    c.op("act", lambda pK=pK, cgk=cgk, nk=nk: nc.scalar.activation(
                                        out=KVs[:, :, cgk * 4 + 1: cgk * 4 + 1 + nk],
                                        in_=pK[:, 0:nk * 128].rearrange("p (j e) -> p e j", j=nk), func=AF.Copy),
                                        reads=[pKn], writes=["KVs"])
                                c.op("dve", lambda h=h: nc.vector.tensor_copy(
                                    out=GMUL[:], in_=G16[:, h:h + 1, :].to_broadcast([128, 128, 16])), reads=["G16"], writes=["GMUL"])
                                c.op("dve", lambda: nc.vector.tensor_tensor_scan(
                                    out=KVs[:].rearrange("p e n -> p (e n)"), data0=GMUL[:].rearrange("p e n -> p (e n)"),
                                    data1=KVs[:].rearrange("p e n -> p (e n)"),
                                    initial=0.0, op0=ALU.mult, op1=ALU.add), reads=["KVs", "GMUL"], writes=["KVs"])
                                c.op("dve", lambda: nc.vector.tensor_copy(out=Rbn[:], in_=KVs[:].rearrange("p e n -> p n e")),
                                     reads=["KVs"], writes=["Rbn"])
                                yt = YT[h % 2]
                                ytn = f"YT{h % 2}"
                                c.reserved = {2, 3}
                                for cg in range(4):
                                    pS, pSn = c.psf()
                                    for j in range(4):
                                        n = cg * 4 + j
                                        cs = slice(n * 128, (n + 1) * 128)
                                        c.op("pe", lambda j=j, cs=cs, pS=pS: nc.tensor.matmul(
                                            pS[:, j * 128:(j + 1) * 128], KT[:, cs], QT[:, cs], start=True, stop=True),
                                            reads=["KT", "QT"], writes=[pSn], inc=(j == 3))
                                    pt = PT[cg % 2]
                                    ptn = f"PT{cg % 2}"
                                    c.op("dve", lambda pS=pS, pt=pt, h=h: nc.vector.tensor_tensor(
                                        out=pt[:], in0=pS[:].rearrange("p (j k) -> p j k", j=4),
                                        in1=DT[:, h:h + 1, :].to_broadcast([128, 4, 128]), op=ALU.mult),
                                        reads=[pSn, "DT"], writes=[ptn])
                                    pO, pOn = c.psf_at(2 + cg % 2)
                                    for j in range(4):
                                        n = cg * 4 + j
                                        cs = slice(n * 128, (n + 1) * 128)
                                        mm(pO[:, j * 128:(j + 1) * 128], pt[:, j, :], VV[:, n, hc], True, n == 0, [ptn, "VV"], [pOn])
                                        if n > 0:
                                            mm(pO[:, j * 128:(j + 1) * 128], QX[:, cs], Rbn[:, n, :], False, True, ["QX", "Rbn"], [pOn])
                                    if ret_pend:
                                        ret_pend.pop()()
                                    ret_pend.append(lambda cg=cg, pO=pO, pOn=pOn, h=h, hc=hc, yt=yt, ytn=ytn: ret_epilogue(cg, pO, pOn, h, hc, yt, ytn))
                        if ret_pend:
                            ret_pend.pop()()
                        c.reserved = set()
                        c.barrier()
                        phase_done("ret")

                    with CleanStack() as st:
                        if "ml" in skip:
                            raise _SkipPhase()
                        MASK = c.sb(st, "MASK", [128, 128], F32)
                        c.dma("sp", MASK[:], c_mask, writes=["MASK"], sem="k0")
                        gb = c.sb(st, "gb", [4, 2], F32)
                        c.dma("sp", gb[:, 0:1], gate_b[l, 0, :].rearrange("(h o) -> h o", o=1), writes=["gb"], sem="k1")
                        c.dma("sp", gb[:, 1:2], gate_b[l, 1, :].rearrange("(h o) -> h o", o=1), writes=["gb"], sem="k1")
                        c.op("dve", lambda: nc.vector.tensor_scalar(out=gb[:, 1:2], in0=gb[:, 1:2], scalar1=-1.0, scalar2=None, op0=ALU.mult),
                             reads=["gb"], writes=["gb"])
                        NWB = c.sb(st, "NWB", [128, 1024], F32)
                        c.dma("sp", NWB[:], ml_nw[l:l + 1, :].partition_broadcast(128), writes=["NWB"], sem="k2")
                        EMT = c.sb(st, "EMT", [128, 64], F32)
                        SCb = c.sb(st, "SCb", [128, 64, 5], F32)
                        VA = c.sb(st, "VA", [128, 16, 2, 257], BF16)
                        SGO = c.sb(st, "SGO", [128, 16, 512], BF16)
                        sgt = c.sb(st, "sgt", [128, 512], F32)
                        c.op("dve", lambda: nc.vector.memset(VA[:, :, :, 256:257], 1.0), writes=["VA"])
                        wtm = WStream(c, st, "wtm", [128, 16, 512], 1,
                                      [W[l]["mv"][0], W[l]["mo"][0], W[l]["mv"][1], W[l]["mo"][1]])

                        def tm_proj_v(hp):
                            wv, wvn = wtm.next(ahead=0)
                            for tl in range(16):
                                ps, pn = c.psf()
                                for k in range(16):
                                    mm(ps[:], A[:, k, tl * 128:(tl + 1) * 128], wv[:, k, :], k == 0, k == 15, ["A", wvn], [pn])
                                c.op("act", lambda ps=ps, tl=tl: nc.scalar.activation(
                                    out=VA[:, tl, :, 0:256], in_=ps[:].rearrange("p (a b) -> p a b", a=2), func=AF.Copy),
                                    reads=[pn], writes=["VA"])

                        def tm_proj_o(hp):
                            wo, won = wtm.next(ahead=0)
                            for tl in range(16):
                                ps, pn = c.psf()
                                for k in range(16):
                                    mm(ps[:], A[:, k, tl * 128:(tl + 1) * 128], wo[:, k, :], k == 0, k == 15, ["A", won], [pn])
                                c.op("act", lambda ps=ps: nc.scalar.activation(out=sgt[:], in_=ps[:], func=AF.Sigmoid),
                                     reads=[pn], writes=["sgt"])
                                c.op("dve", lambda tl=tl, hp=hp: nc.vector.tensor_tensor(
                                    out=SGO[:, tl, :], in0=sgt[:], in1=NWB[:, hp * 512:(hp + 1) * 512], op=ALU.mult),
                                    reads=["sgt", "NWB"], writes=["SGO"])

                        stg = ExitStack()
                        st_outer = st
                        st = stg
                        wif = [c.sb(st, f"wif{i}", [128, 16, 4], BF16) for i in range(2)]
                        c.dma("sp", wif[0][:], W[l]["mi"][0][0], reads=W[l]["mi"][0][1], writes=["wif0"], sem="k3")
                        c.dma("sp", wif[1][:], W[l]["mf"][0][0], reads=W[l]["mf"][0][1], writes=["wif1"], sem="k4")
                        LI = c.sb(st, "LI", [4, S], F32)
                        LF = c.sb(st, "LF", [4, S], F32)
                        for tb in range(4):
                            ts_ = slice(tb * 512, (tb + 1) * 512)
                            ps, pn = c.psf()
                            proj_fm(wif[0], "wif0", tb * 512, ps, pn, m=4)
                            c.op("act", lambda ps=ps, ts_=ts_: nc.scalar.activation(out=LI[:, ts_], in_=ps[0:4, :], func=AF.Identity, bias=gb[:, 0:1]),
                                 reads=[pn, "gb"], writes=["LI"])
                            ps, pn = c.psf()
                            proj_fm(wif[1], "wif1", tb * 512, ps, pn, m=4)
                            c.op("act", lambda ps=ps, ts_=ts_: nc.scalar.activation(out=LF[:, ts_], in_=ps[0:4, :], func=AF.Exp, scale=-1.0, bias=gb[:, 1:2]),
                                 reads=[pn, "gb"], writes=["LF"])
                        c.op("act", lambda: nc.scalar.activation(out=LF[:], in_=LF[:], func=AF.Ln, bias=1.0), reads=["LF"], writes=["LF"])
                        c.dma("sp", gsc[0], LI[:], reads=["LI"], writes=["D:gsc0"], sem="k5")
                        c.dma("sp", gsc[1], LF[:], reads=["LF"], writes=["D:gsc1"], sem="k6")
                        tm_proj_v(0)
                        LIf = c.sb(st, "LIf", [64, 128], F32)
                        CS = c.sb(st, "CS", [64, 128], F32)
                        c.dma("sp", LIf[:], gsc[0].rearrange("h (n l) -> (h n) l", l=128), reads=["D:gsc0"], writes=["LIf"], sem="k5")
                        c.dma("sp", CS[:], gsc[1].rearrange("h (n l) -> (h n) l", l=128), reads=["D:gsc1"], writes=["CS"], sem="k6")
                        c.op("dve", lambda: nc.vector.tensor_tensor_scan(out=CS[:], data0=CS[:], data1=CS[:], initial=0.0, op0=ALU.add, op1=ALU.bypass),
                             reads=["CS"], writes=["CS"])
                        Afm = c.sb(st, "Afm", [64, 128], F32)
                        CM = c.sb(st, "CM", [64, 128], F32)
                        c.op("dve", lambda: nc.vector.tensor_tensor(out=Afm[:], in0=LIf[:], in1=CS[:], op=ALU.add), reads=["LIf", "CS"], writes=["Afm"])
                        c.op("dve", lambda: nc.vector.tensor_tensor_scan(out=CM[:], data0=Afm[:], data1=Afm[:], initial=-1e30, op0=ALU.max, op1=ALU.bypass),
                             reads=["Afm"], writes=["CM"])
                        st2 = c.sb(st, "st2", [64, 2], F32)
                        c.op("dve", lambda: nc.vector.tensor_scalar(out=st2[:, 0:1], in0=CS[:, 127:128], scalar1=-1.0, scalar2=None, op0=ALU.mult),
                             reads=["CS"], writes=["st2"])
                        c.op("dve", lambda: nc.vector.tensor_copy(out=st2[:, 1:2], in_=CM[:, 127:128]), reads=["CM"], writes=["st2"])
                        c.dma("sp", gs2, st2[:], reads=["st2"], writes=["D:gs2"], sem="k5")
                        GT = c.sb(st, "GT", [4, 16, 2], F32)
                        c.dma("sp", GT[:], gs2.rearrange("(h n) k -> h n k", n=16), reads=["D:gs2"], writes=["GT"], sem="k5")
                        ML = c.sb(st, "ML", [4, 16], F32)
                        MP = c.sb(st, "MP", [4, 17], F32)
                        c.op("dve", lambda: nc.vector.tensor_tensor(out=ML[:], in0=GT[:, :, 0], in1=GT[:, :, 1], op=ALU.add), reads=["GT"], writes=["ML"])
                        c.op("dve", lambda: nc.vector.memset(MP[:], 0.0), writes=["MP"])
                        for n in range(16):
                            c.op("dve", lambda n=n: nc.vector.scalar_tensor_tensor(
                                out=MP[:, n + 1:n + 2], in0=MP[:, n:n + 1], scalar=GT[:, n, 0:1], in1=ML[:, n:n + 1],
                                op0=ALU.add, op1=ALU.max), reads=["MP", "GT", "ML"], writes=["MP"])
                        tm_proj_o(0)
                        SC = c.sb(st, "SC", [4, 16, 5], F32)
                        tq = c.sb(st, "tq", [4, 16], F32)
                        c.op("dve", lambda: nc.vector.tensor_tensor(out=tq[:], in0=GT[:, :, 0], in1=MP[:, 0:16], op=ALU.add), reads=["GT", "MP"], writes=["tq"])
                        c.op("dve", lambda: nc.vector.tensor_tensor(out=tq[:], in0=tq[:], in1=MP[:, 1:17], op=ALU.subtract), reads=["tq", "MP"], writes=["tq"])
                        c.op("act", lambda: nc.scalar.activation(out=SC[:, :, 0], in_=tq[:], func=AF.Exp), reads=["tq"], writes=["SC"])
                        tq2 = c.sb(st, "tq2", [4, 16], F32)
                        c.op("dve", lambda: nc.vector.tensor_tensor(out=tq2[:], in0=ML[:], in1=MP[:, 1:17], op=ALU.subtract), reads=["ML", "MP"], writes=["tq2"])
                        c.op("act", lambda: nc.scalar.activation(out=SC[:, :, 1], in_=tq2[:], func=AF.Exp), reads=["tq2"], writes=["SC"])
                        tq3 = c.sb(st, "tq3", [4, 16], F32)
                        c.op("dve", lambda: nc.vector.tensor_tensor(out=tq3[:], in0=MP[:, 0:16], in1=GT[:, :, 1], op=ALU.subtract), reads=["GT", "MP"], writes=["tq3"])
                        c.op("act", lambda: nc.scalar.activation(out=SC[:, :, 2], in_=tq3[:], func=AF.Exp), reads=["tq3"], writes=["SC"])
                        c.op("dve", lambda: nc.vector.tensor_copy(out=SC[:, :, 3], in_=GT[:, :, 1]), reads=["GT", "SC"], writes=["SC"])
                        c.op("dve", lambda: nc.vector.tensor_copy(out=SC[:, :, 4], in_=MP[:, 0:16]), reads=["MP", "SC"], writes=["SC"])
                        c.dma("sp", gs3, SC[:], reads=["SC"], writes=["D:gs3"], sem="k6")
                        SCc = c.sb(st, "SCc", [64, 5], F32)
                        c.dma("sp", SCc[:], gs3.rearrange("h n k -> (h n) k"), reads=["D:gs3"], writes=["SCc"], sem="k6")
                        c.dma("sp", SCb[:].rearrange("p j k -> p (j k)"),
                              gs3.rearrange("h n k -> (h n k)").rearrange("(o f) -> o f", o=1).partition_broadcast(128),
                              reads=["D:gs3"], writes=["SCb"], sem="k5")
                        Mfm = c.sb(st, "Mfm", [64, 128], F32)
                        c.op("dve", lambda: nc.vector.tensor_scalar(out=Mfm[:], in0=CM[:], scalar1=SCc[:, 4:5], scalar2=None, op0=ALU.max),
                             reads=["CM", "SCc"], writes=["Mfm"])
                        bcol = c.sb(st, "bcol", [64, 2], F32)
                        c.op("dve", lambda: nc.vector.tensor_scalar(out=bcol[:, 0:1], in0=SCc[:, 3:4], scalar1=float(math.log(128 ** -0.5)), scalar2=None, op0=ALU.add),
                             reads=["SCc"], writes=["bcol"])
                        c.op("dve", lambda: nc.vector.tensor_scalar(out=bcol[:, 1:2], in0=SCc[:, 3:4], scalar1=-1.0, scalar2=None, op0=ALU.mult),
                             reads=["SCc", "bcol"], writes=["bcol"])
                        W1f = c.sb(st, "W1f", [64, 128], F32)
                        ELf = c.sb(st, "ELf", [64, 128], F32)
                        EMf = c.sb(st, "EMf", [64, 128], F32)
                        c.op("act", lambda: nc.scalar.activation(out=W1f[:], in_=Mfm[:], func=AF.Exp, scale=-1.0, bias=bcol[:, 0:1]),
                             reads=["Mfm", "bcol"], writes=["W1f"])
                        c.op("act", lambda: nc.scalar.activation(out=ELf[:], in_=Afm[:], func=AF.Exp, bias=bcol[:, 1:2]),
                             reads=["Afm", "bcol"], writes=["ELf"])
                        c.op("dve", lambda: nc.vector.tensor_tensor(out=EMf[:], in0=CS[:], in1=Mfm[:], op=ALU.subtract), reads=["CS", "Mfm"], writes=["EMf"])
                        c.op("act", lambda: nc.scalar.activation(out=EMf[:], in_=EMf[:], func=AF.Exp), reads=["EMf"], writes=["EMf"])
                        c.dma("sp", gs4[0], W1f[:], reads=["W1f"], writes=["D:gs40"], sem="k5")
                        c.dma("sp", gs4[1], ELf[:], reads=["ELf"], writes=["D:gs41"], sem="k6")
                        ps, pn = c.psf()
                        mm(ps[:, 0:64], EMf[:], identf[0:64, 0:64], True, True, ["EMf", "identf"], [pn])
                        c.op("dve", lambda ps=ps: nc.vector.tensor_copy(out=EMT[:], in_=ps[:, 0:64]), reads=[pn], writes=["EMT"])

                        c.barrier()
                        stg.close()
                        st = st_outer
                        phase_done("mlg")
                        XP = c.sb(st, "XP", [128, 3 + S], F32)
                        acc = c.sb(st, "acc", [128, S], F32)
                        CW = c.sb(st, "CW", [128, 8, 5], F32)
                        for qk in range(2):
                            for h in range(4):
                                c0 = qk * 512 + h * 128
                                c.dma("sp", CW[:, qk * 4 + h, 0:4], conv_wT[l, c0:c0 + 128, :], writes=["CW"], sem="k0")
                                c.dma("sp", CW[:, qk * 4 + h, 4:5], conv_b[l, c0:c0 + 128].rearrange("(p o) -> p o", o=1), writes=["CW"], sem="k0")
                        c.op("dve", lambda: nc.vector.memset(XP[:, 0:3], 0.0), writes=["XP"])
                        QcT = c.sb(st, "QcT", [128, S], BF16)
                        KcT = c.sb(st, "KcT", [128, S], BF16)
                        QS = c.sb(st, "QS", [128, S], BF16)
                        KST = c.sb(st, "KST", [128, S], BF16)
                        KSm = c.sb(st, "KSm", [128, 16, 128], BF16)
                        CA = c.sb(st, "CA", [128, 257], F32)
                        CLs = c.sb(st, "CLs", [128, 15, 257], F32)
                        Cb = c.sb(st, "Cb", [128, 16, 257], BF16)
                        PTm = [c.sb(st, f"PTm{i}", [128, 4, 128], BF16) for i in range(2)]
                        sc1 = c.sb(st, "sc1", [128, 4], F32)
                        junk2 = c.sb(st, "junk2", [128, 256], F32)
                        Ym = [c.sb(st, f"Ym{i}", [128, 256], BF16) for i in range(2)]
                        YTm = [c.sb(st, "YTm0", [128, 2, S], BF16)]
                        wfm = WStream(c, st, "wfm", [128, 16, 128], 3,
                                      [W[l][k][h] for h in range(4) for k in ("mq", "mk")])
                        ml_pend = []

                        def ml_epilogue(n, pN, pNn, hn, h, hl, cs, ytm, ytmn):
                            c.op("act", lambda pN=pN: nc.scalar.activation(
                                out=sc1[:, 0:1], in_=pN[:, 256:257], func=AF.Abs), reads=[pNn], writes=["sc1"])
                            c.op("dve", lambda hn=hn: nc.vector.tensor_tensor(
                                out=sc1[:, 0:1], in0=sc1[:, 0:1], in1=EMT[:, hn:hn + 1], op=ALU.max), reads=["sc1", "EMT"], writes=["sc1"])
                            c.op("dve", lambda: nc.vector.reciprocal(out=sc1[:, 0:1], in_=sc1[:, 0:1]), reads=["sc1"], writes=["sc1"])
                            c.op("act", lambda pN=pN: nc.scalar.activation(out=junk2[:], in_=pN[:, 0:256], func=AF.Square, accum_out=sc1[:, 1:2]),
                                 reads=[pNn, "sc1"], writes=["junk2", "sc1"])
                            c.op("dve", lambda: nc.vector.scalar_tensor_tensor(
                                out=sc1[:, 2:3], in0=sc1[:, 0:1], scalar=sc1[:, 0:1], in1=sc1[:, 1:2], op0=ALU.mult, op1=ALU.mult),
                                reads=["sc1"], writes=["sc1"])
                            c.op("act", lambda: nc.scalar.activation(out=sc1[:, 2:3], in_=sc1[:, 2:3], func=AF.Sqrt, scale=1.0 / 256, bias=EPS),
                                 reads=["sc1"], writes=["sc1"])
                            c.op("dve", lambda: nc.vector.reciprocal(out=sc1[:, 2:3], in_=sc1[:, 2:3]), reads=["sc1"], writes=["sc1"])
                            c.op("dve", lambda: nc.vector.tensor_tensor(out=sc1[:, 3:4], in0=sc1[:, 2:3], in1=sc1[:, 0:1], op=ALU.mult),
                                 reads=["sc1"], writes=["sc1"])
                            ym = Ym[n % 2]
                            ymn = f"Ym{n % 2}"
                            c.op("dve", lambda pN=pN, ym=ym, n=n, hl=hl: nc.vector.scalar_tensor_tensor(
                                out=ym[:], in0=pN[:, 0:256], scalar=sc1[:, 3:4], in1=SGO[:, n, hl * 256:(hl + 1) * 256],
                                op0=ALU.mult, op1=ALU.mult), reads=[pNn, "sc1", "SGO"], writes=[ymn])
                            pb, pbn = transpose_bf(None, lambda jj, ym=ym: ym[:, jj * 128:(jj + 1) * 128], 2, [ymn], None)
                            c.op("act", lambda pb=pb, ytm=ytm, cs=cs: nc.scalar.activation(
                                out=ytm[:, :, cs], in_=pb[:, 0:256].rearrange("p (a b) -> p a b", a=2), func=AF.Copy),
                                reads=[pbn], writes=[ytmn])
                            if n == 15:
                                c.dma("sp", ybuf[1, 2 * h:2 * h + 2].rearrange("e p t -> p e t"), ytm[:], reads=[ytmn],
                                      writes=[f"D:yb1_{2 * h}", f"D:yb1_{2 * h + 1}"], sem=ytmn)

                        c.reserved = {2, 3}
                        for hp in range(2):
                            if ml_pend:
                                ml_pend.pop()()
                            if hp > 0:
                                tm_proj_v(hp)
                                tm_proj_o(hp)
                            for hl in range(2):
                                h = hp * 2 + hl
                                for qk, dstT, dn in ((0, QcT, "QcT"), (1, KcT, "KcT")):
                                    wt, wn = wfm.next()
                                    ci = qk * 4 + h
                                    for tb in range(4):
                                        ps, pn = c.psf()
                                        proj_fm(wt, wn, tb * 512, ps, pn)
                                        c.op("act", lambda ps=ps, tb=tb: nc.scalar.activation(
                                            out=XP[:, 3 + tb * 512: 3 + (tb + 1) * 512], in_=ps[:], func=AF.Copy),
                                            reads=[pn], writes=["XP"])
                                    c.op("dve", lambda ci=ci: nc.vector.tensor_scalar(
                                        out=acc[:], in0=XP[:, 3:3 + S], scalar1=CW[:, ci, 3:4], scalar2=None, op0=ALU.mult),
                                        reads=["XP", "CW"], writes=["acc"])
                                    for j in (2, 1, 0):
                                        c.op("dve", lambda ci=ci, j=j: nc.vector.scalar_tensor_tensor(
                                            out=acc[:], in0=XP[:, j:j + S], scalar=CW[:, ci, j:j + 1], in1=acc[:],
                                            op0=ALU.mult, op1=ALU.add), reads=["XP", "CW", "acc"], writes=["acc"])
                                    c.op("act", lambda ci=ci, dstT=dstT: nc.scalar.activation(
                                        out=dstT[:], in_=acc[:], func=AF.Silu, bias=CW[:, ci, 4:5]),
                                        reads=["acc", "CW"], writes=[dn])
                                c.dma("sp", acc[:], gs4[0, h * 16:(h + 1) * 16, :].rearrange("n l -> (n l)").rearrange("(o f) -> o f", o=1).partition_broadcast(128),
                                      reads=["D:gs40"], writes=["acc"], sem="k1")
                                c.dma("sp", XP[:, 3:3 + S], gs4[1, h * 16:(h + 1) * 16, :].rearrange("n l -> (n l)").rearrange("(o f) -> o f", o=1).partition_broadcast(128),
                                      reads=["D:gs41"], writes=["XP"], sem="k2")
                                c.op("dve", lambda: nc.vector.tensor_tensor(out=QS[:], in0=QcT[:], in1=acc[:], op=ALU.mult),
                                     reads=["QcT", "acc"], writes=["QS"])
                                c.op("dve", lambda: nc.vector.tensor_tensor(out=KST[:], in0=KcT[:], in1=XP[:, 3:3 + S], op=ALU.mult),
                                     reads=["KcT", "XP"], writes=["KST"])
                                for cg in range(4):
                                    pb, pbn = transpose_bf(None, lambda j, cg=cg: KST[:, (cg * 4 + j) * 128:(cg * 4 + j + 1) * 128], 4, ["KST"], None)
                                    c.op("act", lambda pb=pb, cg=cg: nc.scalar.activation(
                                        out=KSm[:, cg * 4:(cg + 1) * 4, :], in_=pb[:, 0:512].rearrange("p (j k) -> p j k", j=4), func=AF.Copy),
                                        reads=[pbn], writes=["KSm"])
                                for n in range(15):
                                    hn = h * 16 + n
                                    pC, pCn = c.psf()
                                    mm(pC[:, 0:257], KSm[:, n, :], VA[:, n, hl, :], True, True, ["KSm", "VA"], [pCn])
                                    c.op("act", lambda pC=pC, n=n, hn=hn: nc.scalar.activation(
                                        out=CLs[:, n, :], in_=pC[:, 0:257], func=AF.Copy, scale=SCb[:, hn, 1:2]),
                                        reads=[pCn, "SCb"], writes=["CLs"], par=True)
                                for n in range(15):
                                    hn = h * 16 + n
                                    if n == 0:
                                        c.op("dve", lambda: nc.vector.tensor_copy(out=CA[:], in_=CLs[:, 0, :]), reads=["CLs"], writes=["CA"])
                                    else:
                                        c.op("dve", lambda n=n, hn=hn: nc.vector.scalar_tensor_tensor(
                                            out=CA[:], in0=CA[:], scalar=SCb[:, hn, 0:1], in1=CLs[:, n, :], op0=ALU.mult, op1=ALU.add),
                                            reads=["CA", "SCb", "CLs"], writes=["CA"])
                                    c.op("act", lambda n=n, hn=hn: nc.scalar.activation(
                                        out=Cb[:, n + 1, :], in_=CA[:], func=AF.Copy, scale=SCb[:, hn + 1, 2:3]),
                                        reads=["CA", "SCb"], writes=["Cb"], par=True)
                                ytm = YTm[0]
                                ytmn = "YTm0"
                                for cg in range(4):
                                    pS, pSn = c.psf()
                                    for j in range(4):
                                        n = cg * 4 + j
                                        cs = slice(n * 128, (n + 1) * 128)
                                        c.op("pe", lambda j=j, cs=cs, pS=pS: nc.tensor.matmul(
                                            pS[:, j * 128:(j + 1) * 128], KST[:, cs], QS[:, cs], start=True, stop=True),
                                            reads=["KST", "QS"], writes=[pSn], inc=(j == 3))
                                    pt = PTm[cg % 2]
                                    ptn = f"PTm{cg % 2}"
                                    c.op("dve", lambda pS=pS, pt=pt: nc.vector.tensor_tensor(
                                        out=pt[:], in0=pS[:].rearrange("p (j k) -> p j k", j=4),
                                        in1=MASK[:].unsqueeze(1).to_broadcast([128, 4, 128]), op=ALU.mult),
                                        reads=[pSn, "MASK"], writes=[ptn])
                                    for j in range(4):
                                        n = cg * 4 + j
                                        hn = h * 16 + n
                                        cs = slice(n * 128, (n + 1) * 128)
                                        pN, pNn = c.psf_at(2 + n % 2)
                                        mm(pN[:, 0:257], pt[:, j, :], VA[:, n, hl, :], True, n == 0, [ptn, "VA"], [pNn])
                                        if n > 0:
                                            mm(pN[:, 0:257], QS[:, cs], Cb[:, n, :], False, True, ["QS", "Cb"], [pNn])
                                        if ml_pend:
                                            ml_pend.pop()()
                                        ml_pend.append(lambda n=n, pN=pN, pNn=pNn, hn=hn, h=h, hl=hl, cs=cs, ytm=ytm, ytmn=ytmn:
                                                       ml_epilogue(n, pN, pNn, hn, h, hl, cs, ytm, ytmn))
                        if ml_pend:
                            ml_pend.pop()()
                        c.reserved = set()
                        c.barrier()
                        phase_done("ml")

                    with CleanStack() as st:
                        if "mb" in skip:
                            raise _SkipPhase()
                        BIAS = c.sb(st, "BIAS", [128, 8, 2, 128], F32)
                        CB = c.sb(st, "CB", [128, 8], F32)
                        PMK = c.sb(st, "PMK", [128, 16, 8], F32)
                        PIK = c.sb(st, "PIK", [128, 16, 8], F32)
                        c.dma("sp", BIAS[:], mb_bias, writes=["BIAS"], sem="k0")
                        c.dma("sp", CB[:], rel_bias[31:32, :].partition_broadcast(128), writes=["CB"], sem="k1")
                        c.dma("sp", PMK[:], c_pm, writes=["PMK"], sem="k2")
                        c.dma("sp", PIK[:], c_pi, writes=["PIK"], sem="k3")
                        QT = c.sb(st, "QT", [128, S], BF16)
                        QTf = c.sb(st, "QTf", [128, S], F32)
                        KT = c.sb(st, "KT", [128, S], BF16)
                        KM = c.sb(st, "KM", [128, 8], F32)
                        KMh = c.sb(st, "KMh", [128, 8], BF16)
                        KMl = c.sb(st, "KMl", [128, 8], BF16)
                        QL = c.sb(st, "QL", [128, S], BF16)
                        VB = c.sb(st, "VB", [128, 16, 4, 129], BF16)
                        GM = c.sb(st, "GM", [128, 16, 8], F32)
                        MX = c.sb(st, "MX", [128, 16, 8], F32)
                        SEL = c.sb(st, "SEL", [128, 16, 8], F32)
                        ACC = c.sb(st, "ACC", [128, 16, 129], F32)
                        REC = c.sb(st, "REC", [128, 16], F32)
                        PTb = [c.sb(st, f"PTb{i}", [128, 512], BF16) for i in range(4)]
                        tb_ = c.sb(st, "tb_", [128, 128], F32)
                        Yb = c.sb(st, "Yb", [128, 16, 128], BF16)
                        YTb = [c.sb(st, f"YTb{i}", [128, S], BF16) for i in range(2)]
                        c.op("dve", lambda: nc.vector.memset(VB[:, :, :, 128:129], 1.0), writes=["VB"])
                        wtm = WStream(c, st, "wtm", [128, 16, 512], 2, [W[l]["bv"][0], W[l]["bv"][1]])
                        wfm = WStream(c, st, "wfm", [128, 16, 128], 3,
                                      [W[l][k][h] for h in range(8) for k in ("bq", "bk")])
                        ipt = [0]
                        for hg in range(2):
                            wv, wvn = wtm.next()
                            for tl in range(16):
                                ps, pn = c.psf()
                                for k in range(16):
                                    mm(ps[:], A[:, k, tl * 128:(tl + 1) * 128], wv[:, k, :], k == 0, k == 15, ["A", wvn], [pn])
                                c.op("act", lambda ps=ps, tl=tl: nc.scalar.activation(
                                    out=VB[:, tl, :, 0:128], in_=ps[:].rearrange("p (a b) -> p a b", a=4), func=AF.Copy),
                                    reads=[pn], writes=["VB"])
                            if stop == "mb_v":
                                c.barrier()
                                phase_done("mb_v")
                            for hl in range(4):
                                h = hg * 4 + hl
                                wt, wn = wfm.next()
                                for tb in range(4):
                                    ts_ = slice(tb * 512, (tb + 1) * 512)
                                    ps, pn = c.psf()
                                    proj_fm(wt, wn, tb * 512, ps, pn)
                                    c.op("act", lambda ps=ps, ts_=ts_: nc.scalar.activation(out=QTf[:, ts_], in_=ps[:], func=AF.Copy, scale=float(128 ** -0.5)),
                                         reads=[pn], writes=["QTf"])
                                    c.op("dve", lambda ts_=ts_: nc.vector.tensor_copy(out=QT[:, ts_], in_=QTf[:, ts_]), reads=["QTf"], writes=["QT"])
                                wt, wn = wfm.next()
                                for tb in range(4):
                                    ts_ = slice(tb * 512, (tb + 1) * 512)
                                    ps, pn = c.psf()
                                    proj_fm(wt, wn, tb * 512, ps, pn)
                                    c.op("act", lambda ps=ps, ts_=ts_: nc.scalar.activation(out=KT[:, ts_], in_=ps[:], func=AF.Copy),
                                         reads=[pn], writes=["KT"])
                                    c.op("dve", lambda ps=ps, tb=tb: nc.vector.reduce_sum(
                                        out=KM[:, 2 * tb:2 * tb + 2], in_=ps[:].rearrange("p (a b) -> p a b", a=2), axis=AX.X),
                                        reads=[pn], writes=["KM"])
                                c.op("dve", lambda: nc.vector.tensor_scalar(out=KM[:], in0=KM[:], scalar1=1.0 / 256, scalar2=None, op0=ALU.mult),
                                     reads=["KM"], writes=["KM"])
                                if stop == "mb_km":
                                    c.barrier()
                                    phase_done("mb_km")
                                c.op("dve", lambda: nc.vector.tensor_tensor(out=QL[:], in0=QTf[:], in1=QT[:], op=ALU.subtract),
                                     reads=["QTf", "QT"], writes=["QL"])
                                c.op("dve", lambda: nc.vector.tensor_copy(out=KMh[:], in_=KM[:]), reads=["KM"], writes=["KMh"])
                                c.op("dve", lambda: nc.vector.tensor_tensor(out=KMl[:], in0=KM[:], in1=KMh[:], op=ALU.subtract),
                                     reads=["KM", "KMh"], writes=["KMl"])
                                pG, pGn = c.psf()
                                for t in range(16):
                                    tsl = slice(t * 128, (t + 1) * 128)
                                    mm(pG[:, t * 8:(t + 1) * 8], QT[:, tsl], KMh[:], True, False, ["QT", "KMh"], [pGn])
                                    mm(pG[:, t * 8:(t + 1) * 8], QT[:, tsl], KMl[:], False, False, ["QT", "KMl"], [pGn])
                                    mm(pG[:, t * 8:(t + 1) * 8], QL[:, tsl], KMh[:], False, True, ["QL", "KMh"], [pGn])
                                c.op("dve", lambda pG=pG: nc.vector.tensor_tensor(
                                    out=GM[:], in0=pG[:, 0:128].rearrange("p (t j) -> p t j", t=16), in1=PMK[:], op=ALU.add),
                                    reads=[pGn, "PMK"], writes=["GM"])
                                if stop == "mb_gm":
                                    c.barrier()
                                    phase_done("mb_gm")
                                for t in range(16):
                                    c.op("dve", lambda t=t: nc.vector.max(out=MX[:, t, :], in_=GM[:, t, :]), reads=["GM"], writes=["MX"])
                                for t in range(16):
                                    c.op("dve", lambda t=t: nc.vector.scalar_tensor_tensor(
                                        out=SEL[:, t, :], in0=GM[:, t, :], scalar=MX[:, t, 2:3], in1=PIK[:, t, :], op0=ALU.is_ge, op1=ALU.mult),
                                        reads=["GM", "MX", "PIK"], writes=["SEL"])
                                if stop == "mb_sel":
                                    c.barrier()
                                    phase_done("mb_sel")
                                c.op("dve", lambda: nc.vector.memset(ACC[:], 0.0), writes=["ACC"])
                                def mb_stage_a(g, j):
                                    pts = {}
                                    for kt in (2 * j, 2 * j + 1):
                                        t_lo = max(kt, 4 * g)
                                        if t_lo > 4 * g + 3:
                                            continue
                                        ncol = (4 * g + 4 - t_lo) * 128
                                        q0 = t_lo * 128
                                        pS, pSn = c.psf()
                                        mm(pS[:, 0:ncol], KT[:, kt * 128:(kt + 1) * 128], QT[:, q0:q0 + ncol], True, True, ["KT", "QT"], [pSn])
                                        pt = PTb[ipt[0] % 4]
                                        ptn = f"PTb{ipt[0] % 4}"
                                        ipt[0] += 1
                                        tcur = t_lo
                                        while tcur <= 4 * g + 3:
                                            o0 = (tcur - t_lo) * 128
                                            if tcur - kt <= 1:
                                                kind = tcur - kt
                                                c.op("dve", lambda pS=pS, o0=o0, kind=kind: nc.vector.tensor_tensor(
                                                    out=tb_[:], in0=pS[:, o0:o0 + 128], in1=BIAS[:, h, kind, :], op=ALU.add),
                                                    reads=[pSn, "BIAS"], writes=["tb_"])
                                                c.op("act", lambda pt=pt, o0=o0: nc.scalar.activation(out=pt[:, o0:o0 + 128], in_=tb_[:], func=AF.Exp),
                                                     reads=["tb_"], writes=[ptn])
                                                tcur += 1
                                            else:
                                                o1 = (4 * g + 4 - t_lo) * 128
                                                c.op("act", lambda pt=pt, pS=pS, o0=o0, o1=o1: nc.scalar.activation(
                                                    out=pt[:, o0:o1], in_=pS[:, o0:o1], func=AF.Exp, bias=CB[:, h:h + 1]),
                                                    reads=[pSn, "CB"], writes=[ptn])
                                                tcur = 4 * g + 4
                                        pts[kt] = (pt, ptn, t_lo)
                                    return pts

                                def mb_stage_b(g, j, pts):
                                    for half in range(2):
                                        tq_ = [t for t in (4 * g + 2 * half, 4 * g + 2 * half + 1) if t >= 2 * j]
                                        if not tq_:
                                            continue
                                        pO, pOn = c.psf()
                                        for ti, t in enumerate(tq_):
                                            kts = [kt for kt in pts if kt <= t]
                                            for ki, kt in enumerate(kts):
                                                pt, ptn, t_lo = pts[kt]
                                                o0 = (t - t_lo) * 128
                                                mm(pO[:, ti * 256:ti * 256 + 129], pt[:, o0:o0 + 128], VB[:, kt, hl, :],
                                                   ki == 0, ki == len(kts) - 1, [ptn, "VB"], [pOn])
                                        for ti, t in enumerate(tq_):
                                            own = (j == t // 2)
                                            sc = 1.0 if own else SEL[:, t, j:j + 1]
                                            c.op("dve", lambda pO=pO, ti=ti, t=t, sc=sc: nc.vector.scalar_tensor_tensor(
                                                out=ACC[:, t, :], in0=pO[:, ti * 256:ti * 256 + 129], scalar=sc, in1=ACC[:, t, :],
                                                op0=ALU.mult, op1=ALU.add), reads=[pOn, "SEL", "ACC"], writes=["ACC"])

                                blocks = [(g, j) for g in range(4) for j in range(2 * g + 2)]
                                nxt = mb_stage_a(*blocks[0])
                                for bi, (g, j) in enumerate(blocks):
                                    cur = nxt
                                    if bi + 1 < len(blocks):
                                        nxt = mb_stage_a(*blocks[bi + 1])
                                    mb_stage_b(g, j, cur)
                                if stop == "mb_att":
                                    c.barrier()
                                    phase_done("mb_att")
                                c.op("dve", lambda: nc.vector.reciprocal(out=REC[:], in_=ACC[:, :, 128]), reads=["ACC"], writes=["REC"])
                                c.op("dve", lambda: nc.vector.tensor_tensor(
                                    out=Yb[:], in0=ACC[:, :, 0:128], in1=REC[:].unsqueeze(2).to_broadcast([128, 16, 128]), op=ALU.mult),
                                    reads=["ACC", "REC"], writes=["Yb"])
                                ytb = YTb[h % 2]
                                ytbn = f"YTb{h % 2}"
                                for cg in range(4):
                                    pb, pbn = transpose_bf(None, lambda jj, cg=cg: Yb[:, cg * 4 + jj, :], 4, ["Yb"], None)
                                    c.op("act", lambda pb=pb, cg=cg, ytb=ytb: nc.scalar.activation(
                                        out=ytb[:, cg * 512:(cg + 1) * 512], in_=pb[:, 0:512], func=AF.Copy), reads=[pbn], writes=[ytbn])
                                c.dma("sp", ybuf[2, h], ytb[:], reads=[ytbn], writes=[f"D:yb2_{h}"], sem=ytbn)
                        c.barrier()
                        phase_done("mb")

                    if l == 0 and sq == 0:
                        flush_casts(["D:yb2_7"])
                    if dbg and l == 0 and sq == 0:
                        c.dma("sp", dbg_y, ybuf, reads=[f"D:yb{i}_{e}" for i in range(3) for e in range(8)], writes=["D:dbg_y"], sem="k0")

                    with CleanStack() as st:
                        YB = c.sb(st, "YB", [128, 24, 512], BF16)
                        MXT = c.sb(st, "MXT", [128, 16, 512], BF16)
                        XB = c.sb(st, "XB", [128, 16, 512], F32)
                        sqb = c.sb(st, "sqb", [128, 16, 512], BF16)
                        rs = c.sb(st, "rs", [128, 512], F32)
                        sg = [c.sb(st, f"sg{i}", [128, 512], F32) for i in range(3)]
                        pr = [c.sb(st, f"pr{i}", [128, 512], F32) for i in range(2)]
                        ggroups = []
                        bgroups = []
                        ogroups = []
                        for tb in range(4):
                            for dc in range(16):
                                for i in range(3):
                                    ggroups.append(W[l]["g"][i * 16 + dc])
                                    bgroups.append(W[l][f"br{i}"][dc])
                            for dc in range(16):
                                ogroups.append(W[l]["out"][dc])
                        wg_s = WStream(c, st, "wg", [128, 16, 128], 3, ggroups)
                        wb_s = WStream(c, st, "wbr", [128, 8, 128], 3, bgroups)
                        wo_s = WStream(c, st, "wo", [128, 16, 128], 2, ogroups)
                        for tb in range(4):
                            t0 = tb * 512
                            blk = (tok0 + t0) // 512
                            for i in range(3):
                                c.dma("sp", YB[:, i * 8:(i + 1) * 8, :], ybuf[i, :, :, t0:t0 + 512].rearrange("e p t -> p e t"),
                                      reads=[f"D:yb{i}_{e}" for e in range(8)], writes=["YB"], sem=f"YB{i}", par=True)
                            c.dma("sp", XB[:], xTv[:, :, tok0 + t0: tok0 + t0 + 512], reads=[f"D:xT{blk}"], writes=["XB"], sem="XB")
                            for dc in range(16):
                                for i in range(3):
                                    wt, wn = wg_s.next()
                                    pg, pgn = c.psf()
                                    proj_fm(wt, wn, t0, pg, pgn)
                                    c.op("act", lambda pg=pg, i=i: nc.scalar.activation(out=sg[i][:], in_=pg[:], func=AF.Sigmoid),
                                         reads=[pgn], writes=[f"sg{i}"])
                                    wt, wn = wb_s.next()
                                    pbr, pbrn = c.psf()
                                    for k in range(8):
                                        mm(pbr[:], wt[:, k, :], YB[:, i * 8 + k, :], k == 0, k == 7, [wn, "YB"], [pbrn])
                                    if i == 0:
                                        c.op("dve", lambda pbr=pbr: nc.vector.tensor_tensor(out=pr[0][:], in0=pbr[:], in1=sg[0][:], op=ALU.mult),
                                             reads=[pbrn, "sg0"], writes=["pr0"])
                                    else:
                                        c.op("dve", lambda pbr=pbr, i=i: nc.vector.tensor_tensor(out=pr[1][:], in0=pbr[:], in1=sg[i][:], op=ALU.mult),
                                             reads=[pbrn, f"sg{i}"], writes=["pr1"])
                                        if i == 1:
                                            c.op("dve", lambda: nc.vector.tensor_tensor(out=pr[0][:], in0=pr[0][:], in1=pr[1][:], op=ALU.add),
                                                 reads=["pr0", "pr1"], writes=["pr0"])
                                        else:
                                            c.op("dve", lambda dc=dc: nc.vector.tensor_tensor(out=MXT[:, dc, :], in0=pr[0][:], in1=pr[1][:], op=ALU.add),
                                                 reads=["pr0", "pr1"], writes=["MXT"])
                            for dc in range(16):
                                wt, wn = wo_s.next()
                                po, pon = c.psf()
                                for k in range(16):
                                    mm(po[:], wt[:, k, :], MXT[:, k, :], k == 0, k == 15, [wn, "MXT"], [pon])
                                c.op("dve", lambda po=po, dc=dc: nc.vector.tensor_tensor(out=XB[:, dc, :], in0=XB[:, dc, :], in1=po[:], op=ALU.add),
                                     reads=[pon, "XB"], writes=["XB"])
                            c.dma("sp", xTv[:, :, tok0 + t0: tok0 + t0 + 512], XB[:], reads=["XB"], writes=[f"D:xT{blk}"], sem="XBo")
                            norm_block((sqb, rs), XB, "XB", 2 * l + 1, lambda k, t0=t0: A[:, k, t0:t0 + 512], "A")
                        c.barrier()
                        phase_done("c1")

                    if dbg and l == 0 and sq == 0:
                        c.dma("sp", dbg_x1, xT[:, :, 0:S], reads=[f"D:xT{b}" for b in range(4)], writes=["D:dbg_x1"], sem="k0")

                    with CleanStack() as st:
                        UT = c.sb(st, "UT", [128, 64, 512], BF16)
                        sq_ = [c.sb(st, f"sq_{i}", [128, 512], F32) for i in range(2)]
                        xr = [c.sb(st, f"xr{i}", [128, 512], F32) for i in range(2)]
                        g1 = []
                        g2 = []
                        for tb in range(4):
                            for fc in range(64):
                                g1.append(W[l]["ff1"][fc])
                            for dc in range(16):
                                g2.append(W[l]["ff2"][dc])
                        w1_s = WStream(c, st, "w1", [128, 16, 128], 3, g1)
                        w2_s = WStream(c, st, "w2", [128, 64, 128], 3, g2)
                        for tb in range(4):
                            t0 = tb * 512
                            blk = (tok0 + t0) // 512
                            for fc in range(64):
                                wt, wn = w1_s.next()
                                ps, pn = c.psf()
                                proj_fm(wt, wn, t0, ps, pn)
                                s_ = sq_[fc % 2]
                                sn = f"sq_{fc % 2}"
                                c.op("act", lambda ps=ps, s_=s_: nc.scalar.activation(out=s_[:], in_=ps[:], func=AF.Square), reads=[pn], writes=[sn])
                                c.op("dve", lambda ps=ps, s_=s_, fc=fc: nc.vector.scalar_tensor_tensor(
                                    out=UT[:, fc, :], in0=ps[:], scalar=0.0, in1=s_[:], op0=ALU.is_gt, op1=ALU.mult),
                                    reads=[pn, sn], writes=["UT"])
                            for dc in range(16):
                                wt, wn = w2_s.next()
                                b = dc % 2
                                c.dma("sp", xr[b][:], xT[dc, :, tok0 + t0: tok0 + t0 + 512], reads=[f"D:xT{blk}"], writes=[f"xr{b}"], sem=f"xr{b}")
                                ps, pn = c.psf()
                                for k in range(64):
                                    mm(ps[:], wt[:, k, :], UT[:, k, :], k == 0, k == 63, [wn, "UT"], [pn])
                                c.op("dve", lambda ps=ps, b=b: nc.vector.tensor_tensor(out=xr[b][:], in0=xr[b][:], in1=ps[:], op=ALU.add),
                                     reads=[pn, f"xr{b}"], writes=[f"xr{b}"])
                                c.dma("sp", xT[dc, :, tok0 + t0: tok0 + t0 + 512], xr[b][:], reads=[f"xr{b}"], writes=[f"D:xT{blk}"], sem=f"xr{b}", par=True)
                        c.barrier()
                        phase_done("ffn")

                    if dbg and l == 0 and sq == 0:
                        c.dma("sp", dbg_x2, xT[:, :, 0:S], reads=[f"D:xT{b}" for b in range(4)], writes=["D:dbg_x2"], sem="k0")

            with CleanStack() as st:
                XB = c.sb(st, "XB", [128, 16, 512], F32)
                XN = c.sb(st, "XN", [128, 16, 512], F32)
                sqb = c.sb(st, "sqb", [128, 16, 512], BF16)
                rs = c.sb(st, "rs", [128, 512], F32)
                ot = [c.sb(st, f"ot{i}", [128, D], F32) for i in range(2)]
                for blk in range(T // 512):
                    c.dma("sp", XB[:], xTv[:, :, blk * 512:(blk + 1) * 512], reads=[f"D:xT{blk}"], writes=["XB"], sem="XB")
                    norm_block((sqb, rs), XB, "XB", 2 * DEPTH, lambda k: XN[:, k, :], "XN")
                    for tl in range(4):
                        b = tl % 2
                        for q4 in range(4):
                            ps, pn = c.psf()
                            for j in range(4):
                                dcx = q4 * 4 + j
                                c.op("pe", lambda j=j, dcx=dcx, ps=ps, tl=tl: nc.tensor.transpose(
                                    ps[:, j * 128:(j + 1) * 128], XN[:, dcx, tl * 128:(tl + 1) * 128], identf[:]),
                                    reads=["XN", "identf"], writes=[pn], inc=(j == 3))
                            if q4 % 2 == 0:
                                c.op("act", lambda ps=ps, b=b, q4=q4: nc.scalar.activation(out=ot[b][:, q4 * 512:(q4 + 1) * 512], in_=ps[:], func=AF.Copy),
                                     reads=[pn], writes=[f"ot{b}"], par=True)
                            else:
                                c.op("dve", lambda ps=ps, b=b, q4=q4: nc.vector.tensor_copy(out=ot[b][:, q4 * 512:(q4 + 1) * 512], in_=ps[:]),
                                     reads=[pn], writes=[f"ot{b}"], par=True)
                        r0 = blk * 512 + tl * 128
                        c.dma("sp", out[r0:r0 + 128, :], ot[b][:], reads=[f"ot{b}"], writes=[f"D:out{blk}_{tl}"], sem=f"ot{b}")
        try:
            body()
        except _Stop:
            c.barrier()
            if dbg:
                c.dma("sp", dbg_y, ybuf, reads=[f"D:yb{i}_{e}" for i in range(3) for e in range(8)], writes=["D:dbg_y"], sem="k0")
                c.dma("sp", dbg_x1, xT[:, :, 0:S], reads=[f"D:xT{b}" for b in range(4)], writes=["D:dbg_x1"], sem="k1")
        c.finish("sp")
    print("instructions:", c.ninst, "sems:", len(c.sem), flush=True)
    return nc


def _host_inputs(inputs, cst):
    rb = np.asarray(inputs["rel_bias"], np.float32)
    b0 = np.where(cst["_m0"][None], rb[cst["_b0"]].transpose(2, 0, 1), np.float32(NEG))
    b1 = rb[cst["_b1"]].transpose(2, 0, 1)
    mb = np.stack([b0, b1], 1)
    shared = {
        "w_in": np.ascontiguousarray(inputs["w_in"], np.float32),
        "w_branch_ret": np.ascontiguousarray(inputs["w_branch_ret"], np.float32),
        "w_branch_mlstm": np.ascontiguousarray(inputs["w_branch_mlstm"], np.float32),
        "w_branch_moba": np.ascontiguousarray(inputs["w_branch_moba"], np.float32),
        "w_out": np.ascontiguousarray(inputs["w_out"], np.float32),
        "w_ff1": np.ascontiguousarray(inputs["w_ff1"], np.float32),
        "w_ff2": np.ascontiguousarray(inputs["w_ff2"], np.float32),
        "mlstm_gate_b": np.ascontiguousarray(inputs["mlstm_gate_b"], np.float32),
        "conv_wT": np.ascontiguousarray(np.asarray(inputs["mlstm_conv_w"], np.float32).transpose(0, 2, 1)),
        "mlstm_conv_b": np.ascontiguousarray(inputs["mlstm_conv_b"], np.float32),
        "mlstm_norm_w": np.ascontiguousarray(inputs["mlstm_norm_w"], np.float32),
        "nmixT": np.ascontiguousarray(np.asarray(inputs["norm_mix_w"], np.float32).reshape(DEPTH, 16, 128).transpose(0, 2, 1)),
        "nmlpT": np.ascontiguousarray(np.asarray(inputs["norm_mlp_w"], np.float32).reshape(DEPTH, 16, 128).transpose(0, 2, 1)),
        "nfinT": np.ascontiguousarray(np.asarray(inputs["final_norm_w"], np.float32).reshape(16, 128).T),
        "rel_bias": np.ascontiguousarray(rb),
        "mb_bias": np.ascontiguousarray(mb.transpose(2, 0, 1, 3)).astype(np.float32),
    }
    for k, v in cst.items():
        if not k.startswith("_"):
            shared[k] = v
    return shared


def kernel(**inputs):
    cst = _constants()
    shared = _host_inputs(inputs, cst)
    x = np.asarray(inputs["x"], np.float32)
    B = x.shape[0]
    per = B // NCORES
    nc = build_nc(n_seq=per, depth=DEPTH, gl=cst["_gl"])
    in_maps = []
    for i in range(NCORES):
        m = dict(shared)
        m["x"] = np.ascontiguousarray(x[i * per:(i + 1) * per].reshape(per * S, D))
        in_maps.append(m)
    res = run_bass_kernel_spmd(nc, in_maps, core_ids=list(range(NCORES)))
    outs = [np.asarray(r["out"], np.float32).reshape(per, S, D) for r in res.results]
    return np.concatenate(outs, axis=0)
```
